# Optimizing a Trainium2 kernel written in Bass

```python
import math
import jax, jax.numpy as jnp
from jax import lax
import numpy as np

D_MODEL = 2048
BATCH = 8
SEQ = 2048
DEPTH = 2

A_HEADS = 6
A_QK_DIM = 64
A_V_DIM = 2 * A_QK_DIM
B_HEADS = 12
B_KV_HEADS = 3
B_HEAD_DIM = 64
B_WINDOW = 128
C_HEADS = 4
C_QK_DIM = 64
C_V_DIM = 128
C_CHUNK = 128
Q_BLOCK = 128

A_WIDTH = A_HEADS * A_V_DIM
B_WIDTH = B_HEADS * B_HEAD_DIM
C_WIDTH = C_HEADS * C_V_DIM
MIX_WIDTH = A_WIDTH + B_WIDTH + C_WIDTH

PROJ_SIZES = [
    A_HEADS * 2 * A_QK_DIM,
    A_HEADS * 2 * A_QK_DIM,
    A_HEADS * A_V_DIM,
    B_HEADS * B_HEAD_DIM,
    B_KV_HEADS * B_HEAD_DIM,
    B_KV_HEADS * B_HEAD_DIM,
    C_HEADS * C_QK_DIM,
    C_HEADS * C_QK_DIM,
    C_HEADS * C_V_DIM,
    C_HEADS * C_V_DIM,
]
PROJ_WIDTH = int(sum(PROJ_SIZES))
SPLIT_POINTS = [int(v) for v in np.cumsum(PROJ_SIZES)[:-1]]

REL_BUCKETS = 32
REL_MAX_DIST = 128
N_BIAS_HEADS = A_HEADS + B_HEADS

D_FF = 5632
N_EXPERTS = 8
TOP_K = 2
EXPERT_FF = 5632
N_DENSE = (DEPTH + 1) // 2
N_MOE = DEPTH // 2

ALPHA = (2.0 * DEPTH) ** 0.25
BETA = (8.0 * DEPTH) ** -0.25
LN_EPS = 1e-5
NEG = -1e30

kernel_name = "hymba_style_diff_swa_retention_moe_deepnorm"

F32 = jnp.float32


def layer_norm(x, g, b):
    xf = x.astype(F32)
    mu = jnp.mean(xf, axis=-1, keepdims=True)
    xc = xf - mu
    var = jnp.mean(xc * xc, axis=-1, keepdims=True)
    return (xc * lax.rsqrt(var + LN_EPS) * g.astype(F32) + b.astype(F32)).astype(x.dtype)


def rel_bucket(dist):
    max_exact = REL_BUCKETS // 2
    d = jnp.maximum(dist, 0)
    ratio = jnp.maximum(d, 1).astype(F32) / max_exact
    large = max_exact + (jnp.log(ratio) / math.log(REL_MAX_DIST / max_exact)
                         * (REL_BUCKETS - max_exact)).astype(jnp.int32)
    large = jnp.minimum(large, REL_BUCKETS - 1)
    return jnp.where(d < max_exact, d, large)


def lambda_init(layer_idx):
    return 0.8 - 0.6 * math.exp(-0.3 * layer_idx)


def diff_attention(q, k, v, lam_params, lam_init, subln_g, bias_tab):
    Bsz, S = q.shape[0], q.shape[1]
    nblk = S // Q_BLOCK
    qf = q.astype(F32) * (A_QK_DIM ** -0.5)
    kf = k.astype(F32)
    vf = v.astype(F32)
    lp = lam_params.astype(F32)
    lam = jnp.exp(jnp.sum(lp[0] * lp[1])) - jnp.exp(jnp.sum(lp[2] * lp[3])) + lam_init
    qb = qf.reshape(Bsz, nblk, Q_BLOCK, A_HEADS, 2, A_QK_DIM).transpose(1, 0, 2, 3, 4, 5)
    k_pos = jnp.arange(S)
    tab = bias_tab.astype(F32)

    def block(args):
        qi, i = args
        q_pos = i * Q_BLOCK + jnp.arange(Q_BLOCK)
        dist = q_pos[:, None] - k_pos[None, :]
        bias = tab[rel_bucket(dist)].transpose(2, 0, 1)
        s = jnp.einsum('bqhmd,bkhmd->bhmqk', qi, kf) + bias[None, :, None]
        s = jnp.where(dist >= 0, s, NEG)
        p = jax.nn.softmax(s, axis=-1)
        a = p[:, :, 0] - lam * p[:, :, 1]
        return jnp.einsum('bhqk,bkhe->bqhe', a, vf)

    o = lax.map(block, (qb, jnp.arange(nblk)))
    o = o.transpose(1, 0, 2, 3, 4).reshape(Bsz, S, A_HEADS, A_V_DIM)
    o = o * lax.rsqrt(jnp.mean(o * o, axis=-1, keepdims=True) + LN_EPS) * subln_g.astype(F32)
    o = o * (1.0 - lam_init)
    return o.reshape(Bsz, S, A_WIDTH).astype(q.dtype)


def swa_sink_attention(q, k, v, sinks, bias_tab):
    Bsz, S = q.shape[0], q.shape[1]
    W = B_WINDOW
    G = B_HEADS // B_KV_HEADS
    nb = S // W
    qb = q.astype(F32).reshape(Bsz, nb, W, B_KV_HEADS, G, B_HEAD_DIM) * (B_HEAD_DIM ** -0.5)
    kb = k.astype(F32).reshape(Bsz, nb, W, B_KV_HEADS, B_HEAD_DIM)
    vb = v.astype(F32).reshape(Bsz, nb, W, B_KV_HEADS, B_HEAD_DIM)
    pad = ((0, 0), (1, 0), (0, 0), (0, 0), (0, 0))
    kk = jnp.concatenate([jnp.pad(kb, pad)[:, :-1], kb], axis=2)
    vv = jnp.concatenate([jnp.pad(vb, pad)[:, :-1], vb], axis=2)
    q_loc = jnp.arange(W)
    k_loc = jnp.arange(2 * W)
    dist = q_loc[:, None] + W - k_loc[None, :]
    band = (dist >= 0) & (dist < W)
    blk_ok = (jnp.arange(nb)[:, None] > 0) | (k_loc[None, :] >= W)
    mask = band[None] & blk_ok[:, None, :]
    bias = bias_tab.astype(F32)[rel_bucket(dist)]
    bias = bias.transpose(2, 0, 1).reshape(B_KV_HEADS, G, W, 2 * W)
    s = jnp.einsum('bnqhgd,bnkhd->bnhgqk', qb, kk) + bias[None, None]
    s = jnp.where(mask[None, :, None, None], s, NEG)
    sink = sinks.astype(F32).reshape(B_KV_HEADS, G)[None, None, :, :, None, None]
    m = jnp.maximum(jnp.max(s, axis=-1, keepdims=True), sink)
    e = jnp.exp(s - m)
    p = e / (jnp.sum(e, axis=-1, keepdims=True) + jnp.exp(sink - m))
    o = jnp.einsum('bnhgqk,bnkhd->bnqhgd', p, vv)
    return o.reshape(Bsz, S, B_WIDTH).astype(q.dtype)


def rotate_every_two(x):
    x1 = x[..., 0::2]
    x2 = x[..., 1::2]
    return jnp.stack([-x2, x1], axis=-1).reshape(x.shape)


def retention(q, k, v, g, sin, cos):
    Bsz, S = q.shape[0], q.shape[1]
    C = C_CHUNK
    nc = S // C
    sn = sin[None, :, None, :]
    cs = cos[None, :, None, :]
    qf = q.astype(F32)
    kf = k.astype(F32) * (C_QK_DIM ** -0.5)
    qf = qf * cs + rotate_every_two(qf) * sn
    kf = kf * cs + rotate_every_two(kf) * sn
    vf = v.astype(F32)
    log_g = jnp.log(1.0 - jnp.exp2(-5.0 - jnp.arange(C_HEADS, dtype=F32)))
    qc = qf.reshape(Bsz, nc, C, C_HEADS, C_QK_DIM)
    kc = kf.reshape(Bsz, nc, C, C_HEADS, C_QK_DIM)
    vc = vf.reshape(Bsz, nc, C, C_HEADS, C_V_DIM)
    pos = jnp.arange(C)
    rel = (pos[:, None] - pos[None, :]).astype(F32)
    decay = jnp.where((rel >= 0)[None], jnp.exp(jnp.maximum(rel, 0.0)[None] * log_g[:, None, None]), 0.0)
    inner = jnp.einsum('bnqhd,bnkhd->bnhqk', qc, kc) * decay[None, None]
    inner_o = jnp.einsum('bnhqk,bnkhe->bnqhe', inner, vc)
    zeta = jnp.exp((C - 1 - pos).astype(F32)[:, None] * log_g[None, :])
    xi = jnp.exp((pos + 1).astype(F32)[:, None] * log_g[None, :])
    kv = jnp.einsum('bnkhd,bnkhe,kh->nbhde', kc, vc, zeta)
    g_chunk = jnp.exp(C * log_g)[None, :, None, None]

    def step(state, kv_i):
        return state * g_chunk + kv_i, state

    init = jnp.zeros((Bsz, C_HEADS, C_QK_DIM, C_V_DIM), F32)
    _, prev = lax.scan(step, init, kv)
    cross = jnp.einsum('bnqhd,nbhde,qh->bnqhe', qc, prev, xi)
    o = (inner_o + cross).reshape(Bsz, S, C_HEADS, C_V_DIM)
    mu = jnp.mean(o, axis=-1, keepdims=True)
    oc = o - mu
    o = oc * lax.rsqrt(jnp.mean(oc * oc, axis=-1, keepdims=True) + LN_EPS)
    y = jax.nn.silu(g.astype(F32)) * o.reshape(Bsz, S, C_WIDTH)
    return y.astype(q.dtype)


def swiglu(x, w_gate, w_up, w_down):
    h = jax.nn.silu(x @ w_gate) * (x @ w_up)
    return h @ w_down


def moe_swiglu(x, w_router, w_gate, w_up, w_down):
    Bsz, S, D = x.shape
    xt = x.reshape(Bsz * S, D)
    logits = (xt @ w_router).astype(F32)
    top_v, top_i = lax.top_k(logits, TOP_K)
    top_w = jax.nn.softmax(top_v, axis=-1)
    gates = jnp.sum(jax.nn.one_hot(top_i, N_EXPERTS, dtype=F32) * top_w[..., None], axis=1)
    y = jnp.zeros((Bsz * S, D), F32)
    for e in range(N_EXPERTS):
        y = y + gates[:, e:e + 1] * swiglu(xt, w_gate[e], w_up[e], w_down[e]).astype(F32)
    return y.reshape(Bsz, S, D).astype(x.dtype)


def setup_inputs(seed: int = 0) -> dict:
    key = jax.random.key(seed)
    ks = jax.random.split(key, 20)
    nrm = jax.random.normal
    D = D_MODEL
    return {
        "x": nrm(ks[0], (BATCH, SEQ, D), F32),
        "w_in": nrm(ks[1], (DEPTH, D, PROJ_WIDTH), F32) * D ** -0.5,
        "rel_bias": nrm(ks[2], (REL_BUCKETS, N_BIAS_HEADS), F32) * 0.3,
        "a_lambda": nrm(ks[3], (DEPTH, 4, A_QK_DIM), F32) * 0.1,
        "a_subln_g": 1.0 + 0.02 * nrm(ks[4], (DEPTH, A_V_DIM), F32),
        "b_sinks": nrm(ks[5], (DEPTH, B_HEADS), F32) * 0.5,
        "w_out": nrm(ks[6], (DEPTH, MIX_WIDTH, D), F32) * (MIX_WIDTH ** -0.5) * BETA,
        "ln_mix_g": 1.0 + 0.02 * nrm(ks[7], (DEPTH, D), F32),
        "ln_mix_b": 0.02 * nrm(ks[8], (DEPTH, D), F32),
        "ln_ffn_g": 1.0 + 0.02 * nrm(ks[9], (DEPTH, D), F32),
        "ln_ffn_b": 0.02 * nrm(ks[10], (DEPTH, D), F32),
        "dense_w_gate": nrm(ks[11], (N_DENSE, D, D_FF), F32) * D ** -0.5,
        "dense_w_up": nrm(ks[12], (N_DENSE, D, D_FF), F32) * D ** -0.5,
        "dense_w_down": nrm(ks[13], (N_DENSE, D_FF, D), F32) * (D_FF ** -0.5) * BETA,
        "moe_router": nrm(ks[14], (N_MOE, D, N_EXPERTS), F32) * D ** -0.5,
        "moe_w_gate": nrm(ks[15], (N_MOE, N_EXPERTS, D, EXPERT_FF), F32) * D ** -0.5,
        "moe_w_up": nrm(ks[16], (N_MOE, N_EXPERTS, D, EXPERT_FF), F32) * D ** -0.5,
        "moe_w_down": nrm(ks[17], (N_MOE, N_EXPERTS, EXPERT_FF, D), F32) * (EXPERT_FF ** -0.5) * BETA,
    }


def reference(x, w_in, rel_bias, a_lambda, a_subln_g, b_sinks, w_out, ln_mix_g, ln_mix_b,
              ln_ffn_g, ln_ffn_b, dense_w_gate, dense_w_up, dense_w_down, moe_router,
              moe_w_gate, moe_w_up, moe_w_down):
    Bsz, S, _ = x.shape
    ang = jnp.repeat(1.0 / (10000.0 ** jnp.linspace(0.0, 1.0, C_QK_DIM // 2, dtype=F32)), 2)
    ang = jnp.arange(S, dtype=F32)[:, None] * ang[None, :]
    sin, cos = jnp.sin(ang), jnp.cos(ang)
    bias_a = rel_bias[:, :A_HEADS]
    bias_b = rel_bias[:, A_HEADS:]
    for l in range(DEPTH):
        proj = jnp.einsum('bsd,dp->bsp', x, w_in[l])
        aq, ak, av, bq, bk, bv, cq, ck, cv, cg = jnp.split(proj, SPLIT_POINTS, axis=-1)
        ya = diff_attention(aq.reshape(Bsz, S, A_HEADS, 2, A_QK_DIM),
                            ak.reshape(Bsz, S, A_HEADS, 2, A_QK_DIM),
                            av.reshape(Bsz, S, A_HEADS, A_V_DIM),
                            a_lambda[l], lambda_init(l), a_subln_g[l], bias_a)
        yb = swa_sink_attention(bq.reshape(Bsz, S, B_HEADS, B_HEAD_DIM),
                                bk.reshape(Bsz, S, B_KV_HEADS, B_HEAD_DIM),
                                bv.reshape(Bsz, S, B_KV_HEADS, B_HEAD_DIM),
                                b_sinks[l], bias_b)
        yc = retention(cq.reshape(Bsz, S, C_HEADS, C_QK_DIM),
                       ck.reshape(Bsz, S, C_HEADS, C_QK_DIM),
                       cv.reshape(Bsz, S, C_HEADS, C_V_DIM), cg, sin, cos)
        mix = jnp.einsum('bsm,md->bsd', jnp.concatenate([ya, yb, yc], axis=-1), w_out[l])
        x = layer_norm(ALPHA * x + mix, ln_mix_g[l], ln_mix_b[l])
        if l % 2 == 0:
            j = l // 2
            f = swiglu(x, dense_w_gate[j], dense_w_up[j], dense_w_down[j])
        else:
            j = l // 2
            f = moe_swiglu(x, moe_router[j], moe_w_gate[j], moe_w_up[j], moe_w_down[j])
        x = layer_norm(ALPHA * x + f, ln_ffn_g[l], ln_ffn_b[l])
    return x
```

```python
import os
import math
from contextlib import ExitStack
import numpy as np
import ml_dtypes
import concourse.bass as bass
import concourse.mybir as mybir
from concourse.bass_utils import run_bass_kernel_spmd

F32 = mybir.dt.float32
BF16 = mybir.dt.bfloat16
ALU = mybir.AluOpType
AF = mybir.ActivationFunctionType

S = 2048
D = 2048
DEPTH = 2
PROJ = 4992
DFF = 5632
NFB = DFF // 128
NE = 8
ALPHA = (2.0 * DEPTH) ** 0.25
EPS = 1e-5
NEGM = -30000.0
O_AQ, O_AK, O_AV, O_BQ, O_BK, O_BV, O_CQ, O_CK, O_CV, O_CG = 0, 768, 1536, 2304, 3072, 3264, 3456, 3712, 3968, 4480


def lambda_init(l):
    return 0.8 - 0.6 * math.exp(-0.3 * l)


class Sem:
    def __init__(self, h, idx):
        self.h = h
        self.idx = idx
        self.total = 0


class Buf:
    __slots__ = ("name", "w", "r", "sem")

    def __init__(self, name=""):
        self.name = name
        self.w = None
        self.r = {}
        self.sem = None


class Eng:
    def __init__(self, name, eng, sem, is_pe=False):
        self.name = name
        self.eng = eng
        self.sem = sem
        self.seen = {}
        self.is_pe = is_pe

    def wait(self, tok):
        if tok is None:
            return
        s, v = tok
        if self.is_pe and s is self.sem:
            return
        if self.seen.get(s.idx, 0) >= v:
            return
        self.eng.wait_ge(s.h, v)
        self.seen[s.idx] = v


class K:
    def __init__(self, nc, stack):
        self.nc = nc
        self.stack = stack
        self.nsem = 0
        self.all_sems = []
        self.PE = Eng("pe", nc.tensor, self.new_sem("pe"), is_pe=True)
        self.ACT = Eng("act", nc.scalar, self.new_sem("act"))
        self.DVE = Eng("dve", nc.vector, self.new_sem("dve"))
        self.POOL = Eng("pool", nc.gpsimd, self.new_sem("pool"))
        self.SP = Eng("sp", nc.sync, self.new_sem("sp"))
        self.engs = [self.PE, self.ACT, self.DVE, self.POOL, self.SP]
        self.free_dma = []
        self.stage_bufs = []

    def new_sem(self, name):
        h = self.stack.enter_context(self.nc.semaphore(f"s_{name}_{self.nsem}"))
        s = Sem(h, self.nsem)
        self.nsem += 1
        self.all_sems.append(s)
        return s

    def _deps(self, E, reads, writes):
        for b in reads:
            E.wait(b.w)
        for b in writes:
            E.wait(b.w)
            for t in b.r.values():
                E.wait(t)

    def _mark(self, tok, reads, writes):
        s = tok[0]
        for b in reads:
            b.r[s.idx] = tok
        for b in writes:
            b.w = tok
            b.r = {}

    def op(self, E, fn, reads=(), writes=()):
        self._deps(E, reads, writes)
        inst = fn()
        E.sem.total += 1
        inst.then_inc(E.sem.h, 1)
        tok = (E.sem, E.sem.total)
        self._mark(tok, reads, writes)
        return tok

    def mm(self, fns, reads=(), writes=()):
        E = self.PE
        self._deps(E, reads, writes)
        inst = None
        for f in fns:
            inst = f()
        E.sem.total += 1
        inst.then_inc(E.sem.h, 1)
        tok = (E.sem, E.sem.total)
        self._mark(tok, reads, writes)
        return tok

    def dma(self, Q, pairs, sem_buf, reads=(), writes=()):
        self._deps(Q, reads, writes)
        if sem_buf.sem is None:
            if self.free_dma:
                sem_buf.sem = self.free_dma.pop()
            else:
                sem_buf.sem = self.new_sem("dma")
            self.stage_bufs.append(sem_buf)
        s = sem_buf.sem
        for (o, i) in pairs:
            inst = Q.eng.dma_start(out=o, in_=i)
            s.total += 16
            inst.then_inc(s.h, 16)
        tok = (s, s.total)
        self._mark(tok, reads, writes)
        return tok

    def barrier(self):
        for E in self.engs:
            for s in self.all_sems:
                if s.total > 0:
                    E.wait((s, s.total))
        for b in self.stage_bufs:
            self.free_dma.append(b.sem)
            b.sem = None
        self.stage_bufs = []


def _rel_bucket_np(dist):
    d = np.maximum(dist, 0)
    ratio = np.maximum(d, 1).astype(np.float32) / np.float32(16)
    large = 16 + (np.log(ratio).astype(np.float32) / np.float32(math.log(128 / 16)) * np.float32(16)).astype(np.int32)
    large = np.minimum(large, 31)
    return np.where(d < 16, d, large)


def make_consts():
    bf = ml_dtypes.bfloat16
    c = {}
    c["c_identf"] = np.eye(128, dtype=np.float32)
    c["c_identb"] = np.eye(128, dtype=np.float32).astype(bf)
    ob = np.zeros((128, 3, 128), np.float32)
    ob[:, 0, :] = 1.0
    ob[:, 1, :64] = 1.0
    ob[:, 2, 64:] = 1.0
    c["c_onesb"] = ob.astype(bf)
    R = np.zeros((64, 64), np.float32)
    for i in range(32):
        R[2 * i + 1, 2 * i] = -1.0
        R[2 * i, 2 * i + 1] = 1.0
    R128 = np.zeros((128, 128), np.float32)
    R128[:64, :64] = R
    R128[64:, 64:] = R
    c["c_rot"] = R128.astype(bf)
    kk = np.arange(128)[:, None]
    jj = np.arange(256)[None, :]
    mk = np.zeros((128, 2, 256), np.float32)
    mk[:, 0, :] = np.where(jj >= kk, 0.0, NEGM)
    mk[:, 1, :] = np.where((jj - kk >= 0) & (jj - kk < 128), 0.0, NEGM)
    c["c_mask"] = mk
    m = np.arange(384)
    dist = m - 127
    bk = _rel_bucket_np(dist)
    oh = np.zeros((32, 384), np.float32)
    for i in range(384):
        if dist[i] >= 0:
            oh[bk[i], i] = 1.0
    c["c_ohb"] = oh
    hh = np.arange(4, dtype=np.float32)
    log_g = np.log(1.0 - np.exp2(-5.0 - hh)).astype(np.float64)
    pos = np.arange(128)
    dec = np.zeros((128, 4, 128), np.float32)
    for h in range(4):
        rel = pos[None, :] - pos[:, None]
        dec[:, h, :] = np.where(rel >= 0, np.exp(np.maximum(rel, 0) * log_g[h]), 0.0)
    c["c_decay"] = dec
    zeta = np.exp((127 - pos)[:, None] * log_g[None, :]).astype(np.float32)
    c["c_zeta"] = zeta
    xi = np.exp((pos + 1)[:, None] * log_g[None, :])
    ang = np.repeat(1.0 / (10000.0 ** np.linspace(0.0, 1.0, 32, dtype=np.float32)), 2).astype(np.float32)
    ang = np.arange(S, dtype=np.float32)[:, None] * ang[None, :]
    sin, cos = np.sin(ang).astype(np.float32), np.cos(ang).astype(np.float32)
    tab = np.zeros((128, 6, S), np.float32)
    for p in range(128):
        d = p % 64
        tab[p, 0, :] = cos[:, d]
        tab[p, 1, :] = sin[:, d]
        for blk in range(2):
            h = 2 * blk + p // 64
            xs = xi[np.arange(S) % 128, h]
            tab[p, 2 + 2 * blk, :] = cos[:, d] * xs
            tab[p, 3 + 2 * blk, :] = sin[:, d] * xs
    c["c_tab"] = tab
    c["c_gchunk"] = np.exp(128 * log_g).astype(np.float64)
    sel = np.zeros((8, 8, 128), np.float32)
    for e in range(8):
        sel[e, e, :] = 1.0
    c["c_sel"] = sel.astype(bf)
    return c


def build_program(dbg=False, nlayers=DEPTH, stop=None):
    nc = bass.Bass("TRN2", target_bir_lowering=False)
    need_dense = (stop is None) or nlayers > 1
    need_moe = (stop is None and nlayers > 1)
    need_router = nlayers > 1
    consts = make_consts()
    gchunk = consts["c_gchunk"]

    def din(name, shape, dt=F32):
        return nc.dram_tensor(name, list(shape), dt, kind="ExternalInput")

    x_in = din("x", [S, D])
    w_in = din("w_in", [DEPTH, D, PROJ])
    rel_bias = din("rel_bias", [32, 18])
    a_lambda = din("a_lambda", [DEPTH, 4, 64])
    a_subln_g = din("a_subln_g", [DEPTH, 128])
    b_sinks = din("b_sinks", [DEPTH, 12])
    w_out = din("w_out", [DEPTH, D, D])
    ln_mix_g = din("ln_mix_g", [DEPTH, D])
    ln_mix_b = din("ln_mix_b", [DEPTH, D])
    ln_ffn_g = din("ln_ffn_g", [DEPTH, D])
    ln_ffn_b = din("ln_ffn_b", [DEPTH, D])
    if need_dense:
        dense_w_gate = din("dense_w_gate", [1, D, DFF])
        dense_w_up = din("dense_w_up", [1, D, DFF])
        dense_w_down = din("dense_w_down", [1, DFF, D])
    if need_router:
        moe_router = din("moe_router", [1, D, NE])
    if need_moe:
        moe_w_gate = din("moe_w_gate", [1, NE, D, DFF])
        moe_w_up = din("moe_w_up", [1, NE, D, DFF])
        moe_w_down = din("moe_w_down", [1, NE, DFF, D])
    c_identf = din("c_identf", [128, 128])
    c_identb = din("c_identb", [128, 128], BF16)
    c_onesb = din("c_onesb", [128, 3, 128], BF16)
    c_rot = din("c_rot", [128, 128], BF16)
    c_mask = din("c_mask", [128, 2, 256])
    c_ohb = din("c_ohb", [32, 384])
    c_decay = din("c_decay", [128, 4, 128])
    c_zeta = din("c_zeta", [128, 4])
    c_tab = din("c_tab", [128, 6, S])
    c_sel = din("c_sel", [8, 8, 128], BF16)

    skind = "ExternalOutput" if dbg else "Internal"
    y_out = nc.dram_tensor("y", [S, D], F32, kind="ExternalOutput")
    yT_d = nc.dram_tensor("yT_d", [D, S], BF16, kind=skind)
    x1_d = nc.dram_tensor("x1_d", [S, D], F32, kind=skind)
    x1T_d = nc.dram_tensor("x1T_d", [D, S], BF16, kind=skind)
    x2_d = nc.dram_tensor("x2_d", [S, D], F32, kind=skind)
    z_d = nc.dram_tensor("z_d", [18, 128, 384], F32, kind=skind)

    stack = ExitStack()
    stack.enter_context(nc.allow_low_precision("bf16 matmul operands with fp32 accumulation"))
    stack.enter_context(nc.allow_non_contiguous_dma(reason="tiny parameter loads"))
    k = K(nc, stack)
    PE, ACT, DVE, POOL, SP = k.PE, k.ACT, k.DVE, k.POOL, k.SP

    uid = [0]

    def sb(name, shape, dt, st=None):
        uid[0] += 1
        return (st or stack).enter_context(nc.sbuf_tensor(f"{name}_{uid[0]}", list(shape), dt))

    def ps(name, shape, dt, st):
        uid[0] += 1
        return st.enter_context(nc.psum_tensor(f"{name}_{uid[0]}", list(shape), dt))

    identf = sb("identf", [128, 128], F32)
    identb = sb("identb", [128, 128], BF16)
    onesb = sb("onesb", [128, 3, 128], BF16)
    rotb = sb("rotb", [128, 128], BF16)
    TA = sb("TA", [128, 6, 256], BF16)
    TB = sb("TB", [128, 12, 256], BF16)
    cA = sb("cA", [128, 6], F32)
    selT = sb("selT", [8, 8, 128], BF16)
    gTh = sb("gTh", [8, S], BF16)
    gTl = sb("gTl", [8, S], BF16)
    b_const = Buf("const")
    b_gT = Buf("gT")
    epsc = sb("epsc", [128, 2], F32)
    k.op(DVE, lambda: nc.vector.memset(epsc[:, 0:1], EPS), writes=[b_const])
    k.op(DVE, lambda: nc.vector.memset(epsc[:, 1:2], 128.0 * EPS), writes=[b_const])

    k.dma(SP, [(identf[:], c_identf.ap()), (identb[:], c_identb.ap()), (onesb[:], c_onesb.ap()),
               (rotb[:], c_rot.ap()), (selT[:], c_sel.ap())], b_const, writes=[b_const])

    with ExitStack() as st:
        tabs = sb("tabs", [32, 18], F32, st)
        ohb = sb("ohb", [32, 384], F32, st)
        ones32 = sb("ones32", [32, 128], BF16, st)
        oht = sb("oht", [32, 2, 384], BF16, st)
        frep = sb("frep", [128, 18, 384], F32, st)
        traw = sb("traw", [128, 18, 256], F32, st)
        mask = sb("mask", [128, 2, 256], F32, st)
        pf = [ps(f"pf{i}", [128, 512], F32, st) for i in range(2)]
        b_tabs, b_frep, b_traw, b_mask = Buf(), Buf(), Buf(), Buf()
        b_oht = [Buf(), Buf()]
        b_pf = [Buf(), Buf()]
        b_ones32 = Buf()
        k.dma(SP, [(tabs[:], rel_bias.ap()), (ohb[:], c_ohb.ap())], b_tabs, writes=[b_tabs])
        k.dma(SP, [(mask[:], c_mask.ap())], b_mask, writes=[b_mask])
        k.op(DVE, lambda: nc.vector.memset(ones32[:], 1.0), writes=[b_ones32])
        for h in range(18):
            i = h % 2
            k.op(DVE, lambda: nc.vector.tensor_scalar(out=oht[:, i, :], in0=ohb[:], scalar1=tabs[:, h:h + 1], scalar2=None,
                                                      op0=ALU.mult), reads=[b_tabs], writes=[b_oht[i]])
            k.mm([lambda: nc.tensor.matmul(pf[i][:, 0:384], lhsT=ones32[:], rhs=oht[:, i, :], start=True, stop=True)],
                 reads=[b_oht[i], b_ones32], writes=[b_pf[i]])
            k.op(ACT, lambda: nc.scalar.copy(out=frep[:, h, :], in_=pf[i][:, 0:384]), reads=[b_pf[i]], writes=[b_frep])
        k.dma(SP, [(z_d.ap().rearrange("h k m -> k h m"), frep[:])], b_frep, reads=[b_frep])
        k.barrier()
        skew = bass.AP(z_d, 127, [[383, 128], [128 * 384, 18], [1, 256]])
        k.dma(SP, [(traw[:], skew)], b_traw, writes=[b_traw])
        k.op(DVE, lambda: nc.vector.tensor_copy(out=cA[:], in_=frep[:, 0:6, 382]), reads=[b_frep], writes=[b_const])
        for h in range(6):
            k.op(DVE, lambda: nc.vector.scalar_tensor_tensor(out=TA[:, h, :], in0=traw[:, h, :], scalar=cA[:, h:h + 1],
                                                             in1=mask[:, 0, :], op0=ALU.subtract, op1=ALU.add),
                 reads=[b_traw, b_mask, b_const], writes=[b_const])
        for h in range(12):
            k.op(DVE, lambda: nc.vector.tensor_tensor(out=TB[:, h, :], in0=traw[:, 6 + h, :], in1=mask[:, 1, :], op=ALU.add),
                 reads=[b_traw, b_mask], writes=[b_const])
        k.barrier()

    def layer_norm_tile(st_name, r, b_r, stats, mv, sc2, b_small, gb, bb, b_gb, out_t, b_out):
        for j in range(4):
            k.op(DVE, lambda: nc.vector.bn_stats(out=stats[:, j, :], in_=r[:, j * 512:(j + 1) * 512]),
                 reads=[b_r], writes=[b_small])
        k.op(DVE, lambda: nc.vector.bn_aggr(out=mv[:], in_=stats[:]), reads=[b_small], writes=[b_small])
        k.op(ACT, lambda: nc.scalar.activation(out=sc2[:, 0:1], in_=mv[:, 1:2], func=AF.Ln, bias=epsc[:, 0:1], scale=1.0),
             reads=[b_small, b_const], writes=[b_small])
        k.op(ACT, lambda: nc.scalar.activation(out=sc2[:, 0:1], in_=sc2[:, 0:1], func=AF.Exp, scale=-0.5),
             reads=[], writes=[b_small])
        k.op(DVE, lambda: nc.vector.scalar_tensor_tensor(out=sc2[:, 1:2], in0=mv[:, 0:1], scalar=-1.0, in1=sc2[:, 0:1],
                                                         op0=ALU.mult, op1=ALU.mult), reads=[b_small], writes=[b_small])
        k.op(ACT, lambda: nc.scalar.activation(out=out_t, in_=r[:], func=AF.Identity, bias=sc2[:, 1:2], scale=sc2[:, 0:1]),
             reads=[b_r, b_small], writes=[b_out])
        k.op(DVE, lambda: nc.vector.tensor_tensor(out=out_t, in0=out_t, in1=gb[:], op=ALU.mult),
             reads=[b_gb], writes=[b_out])
        k.op(DVE, lambda: nc.vector.tensor_tensor(out=out_t, in0=out_t, in1=bb[:], op=ALU.add),
             reads=[b_gb], writes=[b_out])

    def bcast_row(handle, off, n):
        return bass.AP(handle, off, [[0, 128], [1, n]])

    for l in range(nlayers):
        x_src = x_in if l == 0 else x2_d
        x_dst = y_out if l == nlayers - 1 else x2_d

        with ExitStack() as st:
            xT = sb("xT", [128, 16, S], BF16, st)
            b_xT = [Buf() for _ in range(16)]
            WB = [sb(f"wb{i}", [128, 16, 256], BF16, st) for i in range(2)]
            b_WB = [Buf(), Buf()]
            wb_i = [0]
            PS = [ps(f"psA{i}", [128, 512], F32, st) for i in range(8)]
            b_PS = [Buf() for _ in range(8)]

            with ExitStack() as st2:
                xin = [sb(f"xin{i}", [128, D], F32, st2) for i in range(2)]
                b_xin = [Buf(), Buf()]
                for tt in range(16):
                    i = tt % 2
                    k.dma(SP, [(xin[i][:], x_src.ap()[tt * 128:(tt + 1) * 128, :])], b_xin[i], writes=[b_xin[i]])
                    for cg in range(4):
                        pb = (tt * 4 + cg) % 8
                        k.mm([(lambda c=c: nc.tensor.transpose(PS[pb][:, (c % 4) * 128:(c % 4 + 1) * 128],
                                                               xin[i][:, c * 128:(c + 1) * 128], identf[:]))
                              for c in range(cg * 4, cg * 4 + 4)],
                             reads=[b_xin[i], b_const], writes=[b_PS[pb]])
                        E = ACT if cg % 2 == 0 else DVE
                        src = PS[pb][:].rearrange("p (c t) -> p c t", c=4)
                        dst = xT[:, cg * 4:cg * 4 + 4, tt * 128:(tt + 1) * 128]
                        if E is ACT:
                            k.op(ACT, lambda: nc.scalar.copy(out=dst, in_=src), reads=[b_PS[pb]], writes=[b_xT[tt]])
                        else:
                            k.op(DVE, lambda: nc.vector.tensor_copy(out=dst, in_=src), reads=[b_PS[pb]], writes=[b_xT[tt]])
                k.barrier()

            ps_rr = [0]

            def next_ps():
                i = ps_rr[0] % 8
                ps_rr[0] += 1
                return i

            def load_w(col_pairs, ncols):
                i = wb_i[0] % 2
                wb_i[0] += 1
                wsrc = w_in.ap()[l].rearrange("(c p) f -> p c f", p=128)
                pairs = [(WB[i][:, :, dc:dc + n], wsrc[:, :, sc:sc + n]) for (dc, sc, n) in col_pairs]
                k.dma(POOL, pairs, b_WB[i], writes=[b_WB[i]])
                return WB[i], b_WB[i]

            def proj_fm(col_pairs, nblk, scale, dsts, b_dsts):
                wt, b_wt = load_w(col_pairs, nblk * 128)
                for j in range(nblk):
                    for tb in range(4):
                        pb = next_ps()
                        k.mm([(lambda c=c: nc.tensor.matmul(PS[pb][:], lhsT=wt[:, c, j * 128:(j + 1) * 128],
                                                            rhs=xT[:, c, tb * 512:(tb + 1) * 512],
                                                            start=(c == 0), stop=(c == 15))) for c in range(16)],
                             reads=[b_wt] + b_xT[tb * 4:tb * 4 + 4], writes=[b_PS[pb]])
                        k.op(ACT, lambda: nc.scalar.activation(out=dsts[j][:, tb * 512:(tb + 1) * 512], in_=PS[pb][:],
                                                               func=AF.Copy, scale=float(scale)),
                             reads=[b_PS[pb]], writes=[b_dsts[j]])

            def proj_tm(col0, ncols, evac):
                wt, b_wt = load_w([(0, col0, ncols)], ncols)
                for tt in range(16):
                    pb = next_ps()
                    k.mm([(lambda c=c: nc.tensor.matmul(PS[pb][:, 0:ncols], lhsT=xT[:, c, tt * 128:(tt + 1) * 128],
                                                        rhs=wt[:, c, 0:ncols], start=(c == 0), stop=(c == 15)))
                          for c in range(16)],
                         reads=[b_wt, b_xT[tt]], writes=[b_PS[pb]])
                    evac(tt, PS[pb], b_PS[pb])

            def proj_fm_cols(col0, nblk, scale, dsts, b_dsts):
                for j0 in range(0, nblk, 2):
                    nb = min(2, nblk - j0)
                    proj_fm([(0, col0 + j0 * 128, nb * 128)], nb, scale, dsts[j0:j0 + nb], b_dsts[j0:j0 + nb])

            def proj_tm_cols(col0, ncols, mk_evac):
                for c0 in range(0, ncols, 256):
                    ncl = min(256, ncols - c0)
                    proj_tm(col0 + c0, ncl, mk_evac(c0, ncl))

            lam3 = sb("lam3", [128, 4, 64], F32, st)
            lamt = sb("lamt", [128, 8], F32, st)
            gsub = sb("gsub", [128, 2], F32, st)
            esink = sb("esink", [128, 6], F32, st)
            b_par = Buf()
            k.dma(SP, [(lam3[:].rearrange("p a b -> p (a b)"), bcast_row(a_lambda, l * 256, 256)),
                       (gsub[:, 0:1], bass.AP(a_subln_g, l * 128, [[1, 128], [1, 1]])),
                       (esink[0:64, :], bass.AP(b_sinks, l * 12, [[0, 64], [2, 6]])),
                       (esink[64:128, :], bass.AP(b_sinks, l * 12 + 1, [[0, 64], [2, 6]]))],
                  b_par, writes=[b_par])
            for a in range(2):
                k.op(DVE, lambda: nc.vector.tensor_tensor(out=lam3[:, 2 * a, :], in0=lam3[:, 2 * a, :], in1=lam3[:, 2 * a + 1, :],
                                                          op=ALU.mult), reads=[b_par], writes=[b_par])
                k.op(DVE, lambda: nc.vector.reduce_sum(out=lamt[:, a:a + 1], in_=lam3[:, 2 * a, :], axis=mybir.AxisListType.X),
                     reads=[b_par], writes=[b_par])
            k.op(ACT, lambda: nc.scalar.activation(out=lamt[:, 2:4], in_=lamt[:, 0:2], func=AF.Exp), reads=[b_par], writes=[b_par])
            k.op(DVE, lambda: nc.vector.tensor_tensor(out=lamt[:, 4:5], in0=lamt[:, 3:4], in1=lamt[:, 2:3], op=ALU.subtract),
                 reads=[b_par], writes=[b_par])
            k.op(DVE, lambda: nc.vector.tensor_scalar(out=lamt[:, 4:5], in0=lamt[:, 4:5], scalar1=-lambda_init(l), scalar2=None,
                                                      op0=ALU.add), reads=[b_par], writes=[b_par])
            k.op(DVE, lambda: nc.vector.tensor_scalar(out=gsub[:, 1:2], in0=gsub[:, 0:1],
                                                      scalar1=float(math.sqrt(128.0) * (1.0 - lambda_init(l))), scalar2=None,
                                                      op0=ALU.mult), reads=[b_par], writes=[b_par])
            k.op(ACT, lambda: nc.scalar.activation(out=esink[:], in_=esink[:], func=AF.Exp), reads=[b_par], writes=[b_par])
            neglam = lamt[:, 4:5]

            with ExitStack() as st2:
                Cqf = [sb(f"Cqf{j}", [128, S], BF16, st2) for j in range(2)]
                Cqx = [sb(f"Cqx{j}", [128, S], BF16, st2) for j in range(2)]
                Ckf = [sb(f"Ckf{j}", [128, S], BF16, st2) for j in range(2)]
                Ckt = sb("Ckt", [128, 16, 256], BF16, st2)
                Cv = sb("Cv", [128, 16, 512], BF16, st2)
                Csg = sb("Csg", [128, 16, 512], BF16, st2)
                decay = sb("decay", [128, 4, 128], F32, st2)
                zeta = sb("zeta", [128, 4], F32, st2)
                b_Cqf, b_Cqx, b_Ckf = [Buf(), Buf()], [Buf(), Buf()], [Buf(), Buf()]
                b_Ckt, b_Cv, b_Csg = Buf(), Buf(), Buf()
                b_cc = Buf()
                k.dma(SP, [(decay[:], c_decay.ap()), (zeta[:], c_zeta.ap())], b_cc, writes=[b_cc])

                def mk_evac_cv(c0, ncl):
                    def ev(tt, p, b_p):
                        k.op(ACT, lambda: nc.scalar.copy(out=Cv[:, tt, c0:c0 + ncl], in_=p[:, 0:ncl]), reads=[b_p], writes=[b_Cv])
                    return ev
                proj_tm_cols(O_CV, 512, mk_evac_cv)

                def mk_evac_cg(c0, ncl):
                    def ev(tt, p, b_p):
                        k.op(ACT, lambda: nc.scalar.activation(out=Csg[:, tt, c0:c0 + ncl], in_=p[:, 0:ncl], func=AF.Silu),
                             reads=[b_p], writes=[b_Csg])
                    return ev
                proj_tm_cols(O_CG, 512, mk_evac_cg)

                with ExitStack() as st3:
                    CqT = [sb(f"CqT{j}", [128, S], BF16, st3) for j in range(2)]
                    CkT = [sb(f"CkT{j}", [128, S], BF16, st3) for j in range(2)]
                    tabt = [sb(f"tabt{i}", [128, 6, 512], F32, st3) for i in range(1)]
                    tmp1 = [sb(f"rtmp{i}", [128, 512], F32, st3) for i in range(2)]
                    b_CqT, b_CkT = [Buf(), Buf()], [Buf(), Buf()]
                    b_tabt = [Buf()]
                    b_tmp1 = [Buf(), Buf()]
                    proj_fm_cols(O_CQ, 2, 1.0, CqT, b_CqT)
                    proj_fm_cols(O_CK, 2, 0.125, CkT, b_CkT)
                    for tb in range(4):
                        ti = 0
                        k.dma(SP, [(tabt[ti][:], c_tab.ap()[:, :, tb * 512:(tb + 1) * 512])], b_tabt[ti], writes=[b_tabt[ti]])
                        for j in range(2):
                            for (src, b_src, outs) in ((CqT[j], b_CqT[j], [(Cqf[j], b_Cqf[j], 0, 1), (Cqx[j], b_Cqx[j], 2 + 2 * j, 3 + 2 * j)]),
                                                       (CkT[j], b_CkT[j], [(Ckf[j], b_Ckf[j], 0, 1)])):
                                pb = next_ps()
                                sl = slice(tb * 512, (tb + 1) * 512)
                                k.mm([lambda: nc.tensor.matmul(PS[pb][:], lhsT=rotb[:], rhs=src[:, sl], start=True, stop=True)],
                                     reads=[b_src, b_const], writes=[b_PS[pb]])
                                for (dst, b_dst, ic, isn) in outs:
                                    t1 = tmp1[0]
                                    t2 = tmp1[1]
                                    k.op(DVE, lambda: nc.vector.tensor_tensor(out=t1[:], in0=src[:, sl], in1=tabt[ti][:, ic, :], op=ALU.mult),
                                         reads=[b_src, b_tabt[ti]], writes=[b_tmp1[0]])
                                    k.op(DVE, lambda: nc.vector.tensor_tensor(out=t2[:], in0=PS[pb][:], in1=tabt[ti][:, isn, :], op=ALU.mult),
                                         reads=[b_PS[pb], b_tabt[ti]], writes=[b_tmp1[1]])
                                    k.op(DVE, lambda: nc.vector.tensor_tensor(out=dst[:, sl], in0=t1[:], in1=t2[:], op=ALU.add),
                                         reads=[b_tmp1[0], b_tmp1[1]], writes=[b_dst])
                    k.barrier()
                PSb = [PS[i][:].bitcast(BF16) if hasattr(PS[i][:], "bitcast") else None for i in range(8)]
                for tt in range(16):
                    pb = next_ps()
                    pview = PSb[pb]
                    k.mm([(lambda j=j: nc.tensor.transpose(pview[:, j * 128:(j + 1) * 128], Ckf[j][:, tt * 128:(tt + 1) * 128], identb[:]))
                          for j in range(2)], reads=[b_Ckf[0], b_Ckf[1], b_const], writes=[b_PS[pb]])
                    for h in range(4):
                        k.op(DVE, lambda: nc.vector.tensor_scalar(out=Ckt[:, tt, h * 64:(h + 1) * 64], in0=pview[:, h * 64:(h + 1) * 64],
                                                                  scalar1=zeta[:, h:h + 1], scalar2=None, op0=ALU.mult),
                             reads=[b_PS[pb], b_cc], writes=[b_Ckt])

                ST = sb("ST", [128, 2, 128], F32, st2)
                STb = [sb(f"STb{i}", [128, 2, 128], BF16, st2) for i in range(2)]
                iT = [sb(f"iT{i}", [128, 128], BF16, st2) for i in range(4)]
                ycm = [sb(f"ycm{i}", [128, 512], BF16, st2) for i in range(2)]
                ycT = [sb(f"ycT{i}", [128, 4, 128], BF16, st2) for i in range(2)]
                cst = sb("cst", [128, 4, 6], F32, st2)
                cmv = sb("cmv", [128, 4, 2], F32, st2)
                crs = sb("crs", [128, 4], F32, st2)
                ctmp = sb("ctmp", [128, 512], F32, st2)
                b_ST, b_STb, b_iT = Buf(), [Buf(), Buf()], [Buf() for _ in range(4)]
                b_ycm, b_ycT, b_cs, b_ctmp = [Buf(), Buf()], [Buf(), Buf()], Buf(), Buf()
                k.op(DVE, lambda: nc.vector.memset(ST[:], 0.0), writes=[b_ST])
                for n in range(16):
                    cs = slice(n * 128, (n + 1) * 128)
                    po = next_ps()
                    for h in range(4):
                        blk, p0 = h // 2, 64 * (h % 2)
                        pi = next_ps()
                        if pi == po:
                            pi = next_ps()
                        k.mm([lambda: nc.tensor.matmul(PS[pi][:, 0:128], lhsT=Ckf[blk][p0:p0 + 64, cs], rhs=Cqf[blk][p0:p0 + 64, cs],
                                                       start=True, stop=True)],
                             reads=[b_Ckf[blk], b_Cqf[blk]], writes=[b_PS[pi]])
                        it = (n * 4 + h) % 4
                        k.op(DVE, lambda: nc.vector.tensor_tensor(out=iT[it][:], in0=PS[pi][:, 0:128], in1=decay[:, h, :], op=ALU.mult),
                             reads=[b_PS[pi], b_cc], writes=[b_iT[it]])
                        fns = [lambda: nc.tensor.matmul(PS[po][:, h * 128:(h + 1) * 128], lhsT=iT[it][:], rhs=Cv[:, n, h * 128:(h + 1) * 128],
                                                        start=True, stop=(n == 0))]
                        rds = [b_iT[it], b_Cv]
                        if n > 0:
                            sbf = STb[n % 2]
                            fns.append(lambda: nc.tensor.matmul(PS[po][:, h * 128:(h + 1) * 128], lhsT=Cqx[blk][p0:p0 + 64, cs],
                                                                rhs=sbf[p0:p0 + 64, blk, :], start=False, stop=True))
                            rds += [b_Cqx[blk], b_STb[n % 2]]
                        k.mm(fns, reads=rds, writes=[b_PS[po]])
                    if n < 15:
                        for blk in range(2):
                            pk = next_ps()
                            if pk == po:
                                pk = next_ps()
                            k.mm([lambda: nc.tensor.matmul(PS[pk][:, 0:256], lhsT=Ckt[:, n, blk * 128:(blk + 1) * 128],
                                                           rhs=Cv[:, n, blk * 256:(blk + 1) * 256], start=True, stop=True)],
                                 reads=[b_Ckt, b_Cv], writes=[b_PS[pk]])
                            for par in range(2):
                                h = 2 * blk + par
                                pr = slice(64 * par, 64 * par + 64)
                                k.op(DVE, lambda: nc.vector.scalar_tensor_tensor(out=ST[pr, blk, :], in0=ST[pr, blk, :],
                                                                                 scalar=float(gchunk[h]),
                                                                                 in1=PS[pk][pr, par * 128:(par + 1) * 128],
                                                                                 op0=ALU.mult, op1=ALU.add),
                                     reads=[b_PS[pk]], writes=[b_ST])
                        nb = (n + 1) % 2
                        k.op(ACT, lambda: nc.scalar.copy(out=STb[nb][:], in_=ST[:]), reads=[b_ST], writes=[b_STb[nb]])
                    yi = n % 2
                    for h in range(4):
                        k.op(DVE, lambda: nc.vector.bn_stats(out=cst[:, h, :], in_=PS[po][:, h * 128:(h + 1) * 128]),
                             reads=[b_PS[po]], writes=[b_cs])
                        k.op(DVE, lambda: nc.vector.bn_aggr(out=cmv[:, h, :], in_=cst[:, h, :]), reads=[b_cs], writes=[b_cs])
                    k.op(ACT, lambda: nc.scalar.activation(out=crs[:], in_=cmv[:, :, 1], func=AF.Ln, bias=epsc[:, 0:1], scale=1.0),
                         reads=[b_cs, b_const], writes=[b_cs])
                    k.op(ACT, lambda: nc.scalar.activation(out=crs[:], in_=crs[:], func=AF.Exp, scale=-0.5),
                         reads=[], writes=[b_cs])
                    for h in range(4):
                        hs = slice(h * 128, (h + 1) * 128)
                        k.op(DVE, lambda: nc.vector.tensor_scalar(out=ctmp[:, hs], in0=PS[po][:, hs], scalar1=cmv[:, h, 0:1],
                                                                  scalar2=crs[:, h:h + 1], op0=ALU.subtract, op1=ALU.mult),
                             reads=[b_PS[po], b_cs], writes=[b_ctmp])
                    k.op(DVE, lambda: nc.vector.tensor_tensor(out=ycm[yi][:], in0=ctmp[:], in1=Csg[:, n, :], op=ALU.mult),
                         reads=[b_ctmp, b_Csg], writes=[b_ycm[yi]])
                    pt = next_ps()
                    pview = PSb[pt]
                    k.mm([(lambda h=h: nc.tensor.transpose(pview[:, h * 128:(h + 1) * 128], ycm[yi][:, h * 128:(h + 1) * 128], identb[:]))
                          for h in range(4)], reads=[b_ycm[yi], b_const], writes=[b_PS[pt]])
                    k.op(ACT, lambda: nc.scalar.copy(out=ycT[yi][:].rearrange("p h q -> p (h q)"), in_=pview[:, 0:512]),
                         reads=[b_PS[pt]], writes=[b_ycT[yi]])
                    k.dma(SP, [(yT_d.ap()[1536:2048, cs].rearrange("(h e) q -> e h q", e=128), ycT[yi][:])], b_ycT[yi],
                          reads=[b_ycT[yi]])
                k.barrier()
            if stop == "C" and l == nlayers - 1:
                break

            with ExitStack() as st2:
                AqT = [sb(f"AqT{j}", [128, S], BF16, st2) for j in range(6)]
                AkT = [sb(f"AkT{j}", [128, S], BF16, st2) for j in range(6)]
                Av = sb("Av", [128, 16, 768], BF16, st2)
                b_AqT = [Buf() for _ in range(6)]
                b_AkT = [Buf() for _ in range(6)]
                b_Av = Buf()
                proj_fm_cols(O_AQ, 6, 0.125, AqT, b_AqT)
                proj_fm_cols(O_AK, 6, 1.0, AkT, b_AkT)

                def mk_evac_av(c0, ncol):
                    def ev(tt, p, b_p):
                        k.op(ACT, lambda: nc.scalar.copy(out=Av[:, tt, c0:c0 + ncol], in_=p[:, 0:ncol]), reads=[b_p], writes=[b_Av])
                    return ev
                proj_tm_cols(O_AV, 768, mk_evac_av)

                PT = [sb(f"PT{i}", [128, 512], BF16, st2) for i in range(4)]
                b_PT = [Buf() for _ in range(4)]
                fa = [sb(f"fa{i}", [128, 512], F32, st2) for i in range(4)]
                b_fa = [Buf() for _ in range(4)]
                sqb = sb("sqb", [128, 512], BF16, st2)
                b_sq = Buf()
                yst = [sb(f"yst{i}", [128, 512], BF16, st2) for i in range(2)]
                b_yst = [Buf(), Buf()]
                step = [0]
                blkc = 0
                for h in range(6):
                    for qb in range(4):
                        nkt = 4 * qb + 4
                        pend = []
                        q0 = qb * 512

                        def emit_pv(item):
                            kt, m, c0, pti = item
                            Ob, Sb = 2 * m, 2 * m + 1
                            k.mm([lambda: nc.tensor.matmul(PS[Ob][:, c0:512], lhsT=Av[:, kt, h * 128:(h + 1) * 128], rhs=PT[pti][:, c0:512],
                                                           start=(kt == 0), stop=(kt == nkt - 1)),
                                  lambda: nc.tensor.matmul(PS[Sb][:, c0:512], lhsT=onesb[:, 0, :], rhs=PT[pti][:, c0:512],
                                                           start=(kt == 0), stop=(kt == nkt - 1))],
                                 reads=[b_Av, b_PT[pti], b_const], writes=[b_PS[Ob], b_PS[Sb]])

                        for kt in range(nkt):
                            j0 = q0 - 128 * kt
                            c0 = max(0, -j0)
                            for m in range(2):
                                sci = 4 + step[0] % 4
                                pti = step[0] % 4
                                step[0] += 1
                                pr = slice(64 * m, 64 * m + 64)
                                fns = [lambda: nc.tensor.matmul(PS[sci][:, c0:512], lhsT=AkT[h][pr, kt * 128:(kt + 1) * 128],
                                                                rhs=AqT[h][pr, q0 + c0:q0 + 512], start=True, stop=(j0 >= 256))]
                                if j0 < 256:
                                    jA = max(j0, 0)
                                    cA_, cB_ = jA - j0, min(512, 256 - j0)
                                    jB = cB_ + j0
                                    fns.append(lambda: nc.tensor.matmul(PS[sci][:, cA_:cB_], lhsT=identb[:], rhs=TA[:, h, jA:jB],
                                                                        start=False, stop=True))
                                k.mm(fns, reads=[b_AkT[h], b_AqT[h], b_const], writes=[b_PS[sci]])
                                k.op(ACT, lambda: nc.scalar.activation(out=PT[pti][:, c0:512], in_=PS[sci][:, c0:512], func=AF.Exp,
                                                                       bias=cA[:, h:h + 1], scale=1.0),
                                     reads=[b_PS[sci], b_const], writes=[b_PT[pti]])
                                pend.append((kt, m, c0, pti))
                                if len(pend) > 2:
                                    emit_pv(pend.pop(0))
                        while pend:
                            emit_pv(pend.pop(0))
                        k.op(DVE, lambda: nc.vector.reciprocal(out=fa[0][:], in_=PS[1][:]), reads=[b_PS[1]], writes=[b_fa[0]])
                        k.op(DVE, lambda: nc.vector.tensor_tensor(out=fa[0][:], in0=fa[0][:], in1=PS[0][:], op=ALU.mult),
                             reads=[b_PS[0]], writes=[b_fa[0]])
                        k.op(DVE, lambda: nc.vector.reciprocal(out=fa[1][:], in_=PS[3][:]), reads=[b_PS[3]], writes=[b_fa[1]])
                        k.op(DVE, lambda: nc.vector.tensor_tensor(out=fa[1][:], in0=fa[1][:], in1=PS[2][:], op=ALU.mult),
                             reads=[b_PS[2]], writes=[b_fa[1]])
                        k.op(DVE, lambda: nc.vector.scalar_tensor_tensor(out=fa[2][:], in0=fa[1][:], scalar=neglam, in1=fa[0][:],
                                                                         op0=ALU.mult, op1=ALU.add),
                             reads=[b_fa[0], b_fa[1], b_par], writes=[b_fa[2]])
                        k.op(DVE, lambda: nc.vector.tensor_tensor(out=sqb[:], in0=fa[2][:], in1=fa[2][:], op=ALU.mult),
                             reads=[b_fa[2]], writes=[b_sq])
                        sci = 4 + step[0] % 4
                        step[0] += 1
                        k.mm([lambda: nc.tensor.matmul(PS[sci][:], lhsT=onesb[:, 0, :], rhs=sqb[:], start=True, stop=True)],
                             reads=[b_sq, b_const], writes=[b_PS[sci]])
                        k.op(ACT, lambda: nc.scalar.activation(out=fa[3][:], in_=PS[sci][:], func=AF.Ln, bias=epsc[:, 1:2], scale=1.0),
                             reads=[b_PS[sci], b_const], writes=[b_fa[3]])
                        k.op(ACT, lambda: nc.scalar.activation(out=fa[3][:], in_=fa[3][:], func=AF.Exp, scale=-0.5),
                             reads=[], writes=[b_fa[3]])
                        yi = blkc % 2
                        blkc += 1
                        k.op(DVE, lambda: nc.vector.scalar_tensor_tensor(out=yst[yi][:], in0=fa[2][:], scalar=gsub[:, 1:2], in1=fa[3][:],
                                                                         op0=ALU.mult, op1=ALU.mult),
                             reads=[b_fa[2], b_fa[3], b_par], writes=[b_yst[yi]])
                        k.dma(SP, [(yT_d.ap()[h * 128:(h + 1) * 128, q0:q0 + 512], yst[yi][:])], b_yst[yi], reads=[b_yst[yi]])
                k.barrier()
            if stop == "A" and l == nlayers - 1:
                break

            with ExitStack() as st2:
                BqT = [sb(f"BqT{j}", [128, S], BF16, st2) for j in range(6)]
                BkT = [sb(f"BkT{j}", [128, S], BF16, st2) for j in range(3)]
                Bvp = sb("Bvp", [128, 16, 3, 2, 128], BF16, st2)
                b_BqT = [Buf() for _ in range(6)]
                b_BkT = [Buf() for _ in range(3)]
                b_Bvp = Buf()
                k.op(DVE, lambda: nc.vector.memset(Bvp[:].rearrange("p a b c d -> p (a b c d)"), 0.0), writes=[b_Bvp])
                proj_fm_cols(O_BQ, 6, 0.125, BqT, b_BqT)
                proj_fm([(0, O_BK, 64), (64, O_BK, 64), (128, O_BK + 64, 64), (192, O_BK + 64, 64)], 2, 1.0, BkT[0:2], b_BkT[0:2])
                proj_fm([(0, O_BK + 128, 64), (64, O_BK + 128, 64)], 1, 1.0, BkT[2:3], b_BkT[2:3])

                def evac_bv(tt, p, b_p):
                    src = p[:, 0:192].rearrange("p (g e) -> p g e", g=3)
                    k.op(ACT, lambda: nc.scalar.copy(out=Bvp[:, tt, :, 0, 0:64], in_=src), reads=[b_p], writes=[b_Bvp])
                    k.op(DVE, lambda: nc.vector.tensor_copy(out=Bvp[:, tt, :, 1, 64:128], in_=src), reads=[b_p], writes=[b_Bvp])
                proj_tm(O_BV, 192, evac_bv)

                PT = [sb(f"PTb{i}", [128, 256], BF16, st2) for i in range(4)]
                b_PT = [Buf() for _ in range(4)]
                fb_ = [sb(f"fb{i}", [128, 512], F32, st2) for i in range(2)]
                b_fb = [Buf(), Buf()]
                yst = [sb(f"ystb{i}", [128, 512], BF16, st2) for i in range(2)]
                b_yst = [Buf(), Buf()]
                step = [0]
                blkc = 0
                for i in range(6):
                    g = i // 2
                    for qb in range(4):
                        q0 = qb * 512
                        kts = list(range(max(0, 4 * qb - 1), 4 * qb + 4))
                        for kt in kts:
                            cs_ = max(0, 128 * kt - q0)
                            ce_ = min(512, 128 * kt + 256 - q0)
                            N = ce_ - cs_
                            jA = q0 + cs_ - 128 * kt
                            for par in range(2):
                                pr = slice(64 * par, 64 * par + 64)
                                sci = 4 + step[0] % 4
                                pti = step[0] % 4
                                step[0] += 1
                                hb = 2 * i + par
                                k.mm([lambda: nc.tensor.matmul(PS[sci][:, 0:N], lhsT=BkT[g][pr, kt * 128:(kt + 1) * 128],
                                                               rhs=BqT[i][pr, q0 + cs_:q0 + ce_], start=True, stop=False),
                                      lambda: nc.tensor.matmul(PS[sci][:, 0:N], lhsT=identb[:], rhs=TB[:, hb, jA:jA + N],
                                                               start=False, stop=True)],
                                     reads=[b_BkT[g], b_BqT[i], b_const], writes=[b_PS[sci]])
                                k.op(ACT, lambda: nc.scalar.activation(out=PT[pti][:, 0:N], in_=PS[sci][:, 0:N], func=AF.Exp),
                                     reads=[b_PS[sci]], writes=[b_PT[pti]])
                                fns = []
                                wr = set()
                                for sub in range(N // 128):
                                    cc = cs_ + sub * 128
                                    qt = (q0 + cc) // 128
                                    qtl = cc // 128
                                    bank = qtl % 2
                                    col = (qtl // 2) * 128
                                    first = (kt == max(0, qt - 1)) and par == 0
                                    last = (kt == qt) and par == 1
                                    fns.append(lambda bank=bank, col=col, sub=sub, first=first, last=last:
                                               nc.tensor.matmul(PS[bank][:, col:col + 128], lhsT=Bvp[:, kt, g, par, :],
                                                                rhs=PT[pti][:, sub * 128:(sub + 1) * 128], start=first, stop=last))
                                    fns.append(lambda bank=bank, col=col, sub=sub, first=first, last=last:
                                               nc.tensor.matmul(PS[2 + bank][:, col:col + 128], lhsT=onesb[:, 1 + par, :],
                                                                rhs=PT[pti][:, sub * 128:(sub + 1) * 128], start=first, stop=last))
                                    wr.add(bank)
                                    wr.add(2 + bank)
                                k.mm(fns, reads=[b_Bvp, b_PT[pti], b_const], writes=[b_PS[w] for w in sorted(wr)])
                        yi = blkc % 2
                        blkc += 1
                        for bank in range(2):
                            dstv = fb_[0][:].rearrange("p (a b c) -> p a b c", a=2, b=2)[:, :, bank, :]
                            k.op(DVE, lambda: nc.vector.tensor_scalar(out=dstv, in0=PS[2 + bank][:, 0:256].rearrange("p (a c) -> p a c", a=2),
                                                                      scalar1=esink[:, i:i + 1], scalar2=None, op0=ALU.add),
                                 reads=[b_PS[2 + bank], b_par], writes=[b_fb[0]])
                        k.op(DVE, lambda: nc.vector.reciprocal(out=fb_[1][:], in_=fb_[0][:]), reads=[b_fb[0]], writes=[b_fb[1]])
                        for bank in range(2):
                            dstv = yst[yi][:].rearrange("p (a b c) -> p a b c", a=2, b=2)[:, :, bank, :]
                            rv = fb_[1][:].rearrange("p (a b c) -> p a b c", a=2, b=2)[:, :, bank, :]
                            k.op(DVE, lambda: nc.vector.tensor_tensor(out=dstv, in0=PS[bank][:, 0:256].rearrange("p (a c) -> p a c", a=2),
                                                                      in1=rv, op=ALU.mult),
                                 reads=[b_PS[bank], b_fb[1]], writes=[b_yst[yi]])
                        k.dma(SP, [(yT_d.ap()[768 + i * 128:768 + (i + 1) * 128, q0:q0 + 512], yst[yi][:])], b_yst[yi],
                              reads=[b_yst[yi]])
                k.barrier()
        if stop == "B" and l == nlayers - 1:
            break

        is_moe = (l % 2 == 1)
        with ExitStack() as st:
            Wo = sb("Wo", [128, 16, D], BF16, st)
            b_Wo = Buf()
            wsrc = w_out.ap()[l].rearrange("(c p) f -> p c f", p=128)
            k.dma(POOL, [(Wo[:, :, j * 512:(j + 1) * 512], wsrc[:, :, j * 512:(j + 1) * 512]) for j in range(4)], b_Wo, writes=[b_Wo])
            gb = sb("gb", [128, D], F32, st)
            bb = sb("bb", [128, D], F32, st)
            b_gb = Buf()
            k.dma(SP, [(gb[:], bcast_row(ln_mix_g, l * D, D)), (bb[:], bcast_row(ln_mix_b, l * D, D))], b_gb, writes=[b_gb])
            yt = [sb(f"yt{i}", [128, 16, 128], BF16, st) for i in range(2)]
            xr = [sb(f"xr{i}", [128, D], F32, st) for i in range(2)]
            rr = [sb(f"rr{i}", [128, D], F32, st) for i in range(2)]
            x1 = [sb(f"x1{i}", [128, D], F32, st) for i in range(2)]
            x1Tb = [sb(f"x1Tb{i}", [128, 16, 128], BF16, st) for i in range(2)]
            stats = sb("stats", [128, 4, 6], F32, st)
            mv = sb("mv", [128, 2], F32, st)
            sc2 = sb("sc2", [128, 2], F32, st)
            b_yt, b_xr, b_rr, b_x1, b_x1Tb = [Buf(), Buf()], [Buf(), Buf()], [Buf(), Buf()], [Buf(), Buf()], [Buf(), Buf()]
            b_small = Buf()
            PS = [ps(f"psB{i}", [128, 512], F32, st) for i in range(8)]
            b_PS = [Buf() for _ in range(8)]
            if is_moe:
                Wr = sb("Wr", [128, 16, NE], F32, st)
                Wrh = sb("Wrh", [128, 16, NE], BF16, st)
                Wrl = sb("Wrl", [128, 16, NE], BF16, st)
                x1Tf = sb("x1Tl", [128, 16, 128], BF16, st)
                gl = sb("gl", [128, 6, NE], F32, st)
                gsm = sb("gsm", [128, 4], F32, st)
                b_Wr, b_x1Tf, b_gl = Buf(), Buf(), Buf()
                k.dma(SP, [(Wr[:], moe_router.ap()[0].rearrange("(c p) e -> p c e", p=128))], b_Wr, writes=[b_Wr])
                k.op(DVE, lambda: nc.vector.tensor_copy(out=Wrh[:], in_=Wr[:]), reads=[b_Wr], writes=[b_Wr])
                k.op(DVE, lambda: nc.vector.tensor_tensor(out=Wrl[:], in0=Wr[:], in1=Wrh[:], op=ALU.subtract), reads=[b_Wr], writes=[b_Wr])
            ysrc = yT_d.ap().rearrange("(c p) t -> p c t", p=128)
            x1Tdst = x1T_d.ap().rearrange("(c p) t -> p c t", p=128)
            for tt in range(16):
                i = tt % 2
                ts_ = slice(tt * 128, (tt + 1) * 128)
                k.dma(SP, [(yt[i][:], ysrc[:, :, ts_])], b_yt[i], writes=[b_yt[i]])
                k.dma(SP, [(xr[i][:], x_src.ap()[ts_, :])], b_xr[i], writes=[b_xr[i]])
                for cg in range(4):
                    k.mm([(lambda c=c: nc.tensor.matmul(PS[cg][:], lhsT=yt[i][:, c, :], rhs=Wo[:, c, cg * 512:(cg + 1) * 512],
                                                        start=(c == 0), stop=(c == 15))) for c in range(16)],
                         reads=[b_yt[i], b_Wo], writes=[b_PS[cg]])
                    k.op(DVE, lambda: nc.vector.scalar_tensor_tensor(out=rr[i][:, cg * 512:(cg + 1) * 512], in0=xr[i][:, cg * 512:(cg + 1) * 512],
                                                                     scalar=float(ALPHA), in1=PS[cg][:], op0=ALU.mult, op1=ALU.add),
                         reads=[b_xr[i], b_PS[cg]], writes=[b_rr[i]])
                layer_norm_tile("mix", rr[i], b_rr[i], stats, mv, sc2, b_small, gb, bb, b_gb, x1[i][:], b_x1[i])
                k.dma(SP, [(x1_d.ap()[ts_, :], x1[i][:])], b_x1[i], reads=[b_x1[i]])
                for cg in range(4):
                    pb = 4 + cg
                    k.mm([(lambda c=c: nc.tensor.transpose(PS[pb][:, (c % 4) * 128:(c % 4 + 1) * 128],
                                                           x1[i][:, c * 128:(c + 1) * 128], identf[:]))
                          for c in range(cg * 4, cg * 4 + 4)], reads=[b_x1[i], b_const], writes=[b_PS[pb]])
                    src = PS[pb][:].rearrange("p (c t) -> p c t", c=4)
                    k.op(ACT, lambda: nc.scalar.copy(out=x1Tb[i][:, cg * 4:cg * 4 + 4, :], in_=src), reads=[b_PS[pb]], writes=[b_x1Tb[i]])
                    if is_moe:
                        k.op(DVE, lambda: nc.vector.tensor_tensor(out=x1Tf[:, cg * 4:cg * 4 + 4, :], in0=src, in1=x1Tb[i][:, cg * 4:cg * 4 + 4, :],
                                                                  op=ALU.subtract), reads=[b_PS[pb], b_x1Tb[i]], writes=[b_x1Tf])
                k.dma(SP, [(x1Tdst[:, :, ts_], x1Tb[i][:])], b_x1Tb[i], reads=[b_x1Tb[i]])
                if is_moe:
                    rfn = []
                    for (xa, wa) in ((x1Tb[i], Wrh), (x1Tf, Wrh), (x1Tb[i], Wrl)):
                        for c in range(16):
                            rfn.append(lambda c=c, xa=xa, wa=wa: nc.tensor.matmul(PS[0][:, 0:NE], lhsT=xa[:, c, :], rhs=wa[:, c, :],
                                                                                  start=(len(rfn_i) == 0), stop=(len(rfn_i) == 47)))
                    rfn_i = []

                    def _run(f):
                        r = f()
                        rfn_i.append(1)
                        return r
                    k.mm([(lambda f=f: _run(f)) for f in rfn], reads=[b_x1Tf, b_x1Tb[i], b_Wr], writes=[b_PS[0]])
                    L, L2, SELm, Wg_, G = gl[:, 0, :], gl[:, 1, :], gl[:, 2, :], gl[:, 3, :], gl[:, 4, :]
                    AX = mybir.AxisListType.X
                    ops = [
                        (DVE, lambda: nc.vector.tensor_copy(out=L, in_=PS[0][:, 0:NE]), [b_PS[0]]),
                        (DVE, lambda: nc.vector.reduce_max(out=gsm[:, 0:1], in_=L, axis=AX), []),
                        (DVE, lambda: nc.vector.tensor_scalar(out=L2, in0=L, scalar1=gsm[:, 0:1], scalar2=-1e30, op0=ALU.is_equal, op1=ALU.mult), []),
                        (DVE, lambda: nc.vector.tensor_tensor(out=L2, in0=L2, in1=L, op=ALU.add), []),
                        (DVE, lambda: nc.vector.reduce_max(out=gsm[:, 1:2], in_=L2, axis=AX), []),
                        (DVE, lambda: nc.vector.tensor_scalar(out=SELm, in0=L, scalar1=gsm[:, 1:2], scalar2=None, op0=ALU.is_ge), []),
                        (DVE, lambda: nc.vector.tensor_scalar(out=gsm[:, 2:3], in0=gsm[:, 0:1], scalar1=-1.0, scalar2=None, op0=ALU.mult), []),
                        (ACT, lambda: nc.scalar.activation(out=Wg_, in_=L, func=AF.Exp, bias=gsm[:, 2:3], scale=1.0), []),
                        (DVE, lambda: nc.vector.tensor_tensor(out=Wg_, in0=Wg_, in1=SELm, op=ALU.mult), []),
                        (DVE, lambda: nc.vector.reduce_sum(out=gsm[:, 3:4], in_=Wg_, axis=AX), []),
                        (DVE, lambda: nc.vector.reciprocal(out=gsm[:, 3:4], in_=gsm[:, 3:4]), []),
                        (DVE, lambda: nc.vector.tensor_scalar(out=G, in0=Wg_, scalar1=gsm[:, 3:4], scalar2=None, op0=ALU.mult), []),
                    ]
                    for (E, f, rd) in ops:
                        k.op(E, f, reads=rd + [b_gl], writes=[b_gl])
                    k.mm([lambda: nc.tensor.transpose(PS[1][0:NE, 0:128], G, identf[:])], reads=[b_gl, b_const], writes=[b_PS[1]])
                    k.op(ACT, lambda: nc.scalar.copy(out=gTh[:, ts_], in_=PS[1][0:NE, 0:128]), reads=[b_PS[1]], writes=[b_gT])
                    k.op(DVE, lambda: nc.vector.tensor_tensor(out=gTl[:, ts_], in0=PS[1][0:NE, 0:128], in1=gTh[:, ts_], op=ALU.subtract),
                         reads=[b_PS[1], b_gT], writes=[b_gT])
            k.barrier()
        if stop == "O" and l == nlayers - 1:
            break

        with ExitStack() as st:
            xb = sb("xb", [128, 16, 512], BF16, st)
            hT = sb("hT", [128, NFB, 512], BF16, st)
            acc = sb("acc", [128, 16, 512], F32, st)
            WG = [sb(f"wg{i}", [128, 16, 256], BF16, st) for i in range(2)]
            WU = [sb(f"wu{i}", [128, 16, 256], BF16, st) for i in range(2)]
            WD = [sb(f"wd{i}", [128, NFB // 2, 128], BF16, st) for i in range(3)]
            sgt = [sb(f"sgt{i}", [128, 512], BF16, st) for i in range(2)]
            gb = sb("gb2", [128, D], F32, st)
            bb = sb("bb2", [128, D], F32, st)
            xr = sb("xr2", [128, D], F32, st)
            rr = sb("rr2", [128, D], F32, st)
            stats = sb("stats2", [128, 4, 6], F32, st)
            mv = sb("mv2", [128, 2], F32, st)
            sc2 = sb("sc22", [128, 2], F32, st)
            b_xb, b_hT, b_acc = Buf(), [Buf() for _ in range(NFB)], [Buf() for _ in range(16)]
            b_WG, b_WU, b_WD, b_sgt = [Buf(), Buf()], [Buf(), Buf()], [Buf(), Buf(), Buf()], [Buf(), Buf()]
            b_gb, b_xr, b_rr, b_small = Buf(), Buf(), Buf(), Buf()
            if is_moe:
                gtb = sb("gtb", [128, 512], F32, st)
                mtmp = sb("mtmp", [128, 512], F32, st)
                b_gtb, b_mtmp = Buf(), Buf()
            PS = [ps(f"psC{i}", [128, 512], F32, st) for i in range(8)]
            b_PS = [Buf() for _ in range(8)]
            k.dma(SP, [(gb[:], bcast_row(ln_ffn_g, l * D, D)), (bb[:], bcast_row(ln_ffn_b, l * D, D))], b_gb, writes=[b_gb])
            x1Tsrc = x1T_d.ap().rearrange("(c p) t -> p c t", p=128)
            wcnt = [0, 0]
            pcnt = [0]
            nexp = NE if is_moe else 1
            for tb in range(4):
                k.dma(SP, [(xb[:], x1Tsrc[:, :, tb * 512:(tb + 1) * 512])], b_xb, writes=[b_xb])
                for e in range(nexp):
                    if is_moe:
                        wg_src = moe_w_gate.ap()[0, e].rearrange("(c p) f -> p c f", p=128)
                        wu_src = moe_w_up.ap()[0, e].rearrange("(c p) f -> p c f", p=128)
                        wd_src = moe_w_down.ap()[0, e].rearrange("(c p) n -> p c n", p=128)
                        k.mm([lambda: nc.tensor.matmul(PS[7][:], lhsT=selT[:, e, :], rhs=gTh[:, tb * 512:(tb + 1) * 512], start=True, stop=False),
                              lambda: nc.tensor.matmul(PS[7][:], lhsT=selT[:, e, :], rhs=gTl[:, tb * 512:(tb + 1) * 512], start=False, stop=True)],
                             reads=[b_gT, b_const], writes=[b_PS[7]])
                        k.op(ACT, lambda: nc.scalar.copy(out=gtb[:], in_=PS[7][:]), reads=[b_PS[7]], writes=[b_gtb])
                    else:
                        wg_src = dense_w_gate.ap()[0].rearrange("(c p) f -> p c f", p=128)
                        wu_src = dense_w_up.ap()[0].rearrange("(c p) f -> p c f", p=128)
                        wd_src = dense_w_down.ap()[0].rearrange("(c p) n -> p c n", p=128)
                    for fg in range(NFB // 2):
                        wi = wcnt[0] % 2
                        wcnt[0] += 1
                        k.dma(POOL, [(WG[wi][:], wg_src[:, :, fg * 256:(fg + 1) * 256])], b_WG[wi], writes=[b_WG[wi]])
                        k.dma(POOL, [(WU[wi][:], wu_src[:, :, fg * 256:(fg + 1) * 256])], b_WU[wi], writes=[b_WU[wi]])
                        for j in range(2):
                            fbk = fg * 2 + j
                            pg = (pcnt[0] * 2) % 6
                            pu = pg + 1
                            pcnt[0] += 1
                            k.mm([(lambda c=c: nc.tensor.matmul(PS[pg][:], lhsT=WG[wi][:, c, j * 128:(j + 1) * 128], rhs=xb[:, c, :],
                                                                start=(c == 0), stop=(c == 15))) for c in range(16)],
                                 reads=[b_WG[wi], b_xb], writes=[b_PS[pg]])
                            k.mm([(lambda c=c: nc.tensor.matmul(PS[pu][:], lhsT=WU[wi][:, c, j * 128:(j + 1) * 128], rhs=xb[:, c, :],
                                                                start=(c == 0), stop=(c == 15))) for c in range(16)],
                                 reads=[b_WU[wi], b_xb], writes=[b_PS[pu]])
                            si = fbk % 2
                            k.op(ACT, lambda: nc.scalar.activation(out=sgt[si][:], in_=PS[pg][:], func=AF.Silu), reads=[b_PS[pg]], writes=[b_sgt[si]])
                            k.op(DVE, lambda: nc.vector.tensor_tensor(out=hT[:, fbk, :], in0=sgt[si][:], in1=PS[pu][:], op=ALU.mult),
                                 reads=[b_sgt[si], b_PS[pu]], writes=[b_hT[fbk]])
                    for dc in range(16):
                        wis = []
                        for hf in range(2):
                            wi = wcnt[1] % 3
                            wcnt[1] += 1
                            wis.append(wi)
                            k.dma(POOL, [(WD[wi][:], wd_src[:, hf * 22:(hf + 1) * 22, dc * 128:(dc + 1) * 128])], b_WD[wi], writes=[b_WD[wi]])
                        po = 6 + dc % 2 if not is_moe else 6
                        k.mm([(lambda f=f: nc.tensor.matmul(PS[po][:], lhsT=WD[wis[f // 22]][:, f % 22, :], rhs=hT[:, f, :],
                                                            start=(f == 0), stop=(f == NFB - 1)))
                              for f in range(NFB)], reads=[b_WD[wis[0]], b_WD[wis[1]]] + b_hT, writes=[b_PS[po]])
                        if not is_moe:
                            k.op(ACT, lambda: nc.scalar.copy(out=acc[:, dc, :], in_=PS[po][:]), reads=[b_PS[po]], writes=[b_acc[dc]])
                        elif e == 0:
                            k.op(DVE, lambda: nc.vector.tensor_tensor(out=acc[:, dc, :], in0=PS[po][:], in1=gtb[:], op=ALU.mult),
                                 reads=[b_PS[po], b_gtb], writes=[b_acc[dc]])
                        else:
                            k.op(DVE, lambda: nc.vector.tensor_tensor(out=mtmp[:], in0=PS[po][:], in1=gtb[:], op=ALU.mult),
                                 reads=[b_PS[po], b_gtb], writes=[b_mtmp])
                            k.op(DVE, lambda: nc.vector.tensor_tensor(out=acc[:, dc, :], in0=acc[:, dc, :], in1=mtmp[:], op=ALU.add),
                                 reads=[b_mtmp], writes=[b_acc[dc]])
                for t4 in range(4):
                    tt = tb * 4 + t4
                    ts_ = slice(tt * 128, (tt + 1) * 128)
                    k.dma(SP, [(xr[:], x1_d.ap()[ts_, :])], b_xr, writes=[b_xr])
                    for cg in range(4):
                        pb = cg
                        k.mm([(lambda c=c: nc.tensor.transpose(PS[pb][:, (c % 4) * 128:(c % 4 + 1) * 128],
                                                               acc[:, c, t4 * 128:(t4 + 1) * 128], identf[:]))
                              for c in range(cg * 4, cg * 4 + 4)], reads=b_acc[cg * 4:cg * 4 + 4] + [b_const], writes=[b_PS[pb]])
                        k.op(DVE, lambda: nc.vector.scalar_tensor_tensor(out=rr[:, cg * 512:(cg + 1) * 512], in0=xr[:, cg * 512:(cg + 1) * 512],
                                                                         scalar=float(ALPHA), in1=PS[pb][:], op0=ALU.mult, op1=ALU.add),
                             reads=[b_xr, b_PS[pb]], writes=[b_rr])
                    layer_norm_tile("ffn", rr, b_rr, stats, mv, sc2, b_small, gb, bb, b_gb, rr[:], b_rr)
                    k.dma(SP, [(x_dst.ap()[ts_, :], rr[:])], b_rr, reads=[b_rr])
            k.barrier()

    k.barrier()
    stack.close()
    return nc, consts


_CACHE = {}


def kernel(**inputs):
    n = 8
    if "prog" not in _CACHE:
        _CACHE["prog"] = build_program()
    nc, consts = _CACHE["prog"]
    shared = {kk: np.ascontiguousarray(v) for kk, v in inputs.items() if kk != "x"}
    for kk, v in consts.items():
        if kk == "c_gchunk":
            continue
        shared[kk] = v
    x = np.ascontiguousarray(inputs["x"])
    in_maps = []
    for b in range(n):
        m = dict(shared)
        m["x"] = x[b]
        in_maps.append(m)
    res = run_bass_kernel_spmd(nc, in_maps, core_ids=list(range(n)))
    return np.stack([np.asarray(r["y"]) for r in res.results], axis=0).astype(np.float32)
```

```python
import os
import math
from contextlib import ExitStack
import numpy as np
import ml_dtypes
import concourse.bass as bass
import concourse.mybir as mybir
from concourse.bass_utils import run_bass_kernel_spmd

F32 = mybir.dt.float32
BF16 = mybir.dt.bfloat16
ALU = mybir.AluOpType
AF = mybir.ActivationFunctionType

S = 2048
D = 2048
DEPTH = 2
PROJ = 4992
DFF = 5632
NFB = DFF // 128
NE = 8
ALPHA = (2.0 * DEPTH) ** 0.25
EPS = 1e-5
NEGM = -30000.0
O_AQ, O_AK, O_AV, O_BQ, O_BK, O_BV, O_CQ, O_CK, O_CV, O_CG = 0, 768, 1536, 2304, 3072, 3264, 3456, 3712, 3968, 4480


def lambda_init(l):
    return 0.8 - 0.6 * math.exp(-0.3 * l)


class Sem:
    def __init__(self, h, idx):
        self.h = h
        self.idx = idx
        self.total = 0


class Buf:
    __slots__ = ("name", "w", "r", "sem")

    def __init__(self, name=""):
        self.name = name
        self.w = None
        self.r = {}
        self.sem = None


class Eng:
    def __init__(self, name, eng, sem, is_pe=False):
        self.name = name
        self.eng = eng
        self.sem = sem
        self.seen = {}
        self.is_pe = is_pe

    def wait(self, tok):
        if tok is None:
            return
        s, v, ep = tok
        if ep != EPOCH[0]:
            return
        if self.is_pe and s is self.sem:
            return
        if self.seen.get(s.idx, 0) >= v:
            return
        self.eng.wait_ge(s.h, v)
        self.seen[s.idx] = v


EPOCH = [0]


class K:
    def __init__(self, nc, stack):
        EPOCH[0] = 0
        self.nc = nc
        self.stack = stack
        self.nsem = 0
        self.all_sems = []
        self.PE = Eng("pe", nc.tensor, self.new_sem("pe"), is_pe=True)
        self.ACT = Eng("act", nc.scalar, self.new_sem("act"))
        self.DVE = Eng("dve", nc.vector, self.new_sem("dve"))
        self.POOL = Eng("pool", nc.gpsimd, self.new_sem("pool"))
        self.SP = Eng("sp", nc.sync, self.new_sem("sp"))
        self.engs = [self.PE, self.ACT, self.DVE, self.POOL, self.SP]
        self.free_dma = []
        self.stage_bufs = []

    def new_sem(self, name):
        h = self.stack.enter_context(self.nc.semaphore(f"s_{name}_{self.nsem}"))
        s = Sem(h, self.nsem)
        self.nsem += 1
        self.all_sems.append(s)
        return s

    def _deps(self, E, reads, writes):
        for b in reads:
            E.wait(b.w)
        for b in writes:
            E.wait(b.w)
            for t in b.r.values():
                E.wait(t)

    def _mark(self, tok, reads, writes):
        s = tok[0]
        for b in reads:
            b.r[s.idx] = tok
        for b in writes:
            b.w = tok
            b.r = {}

    def op(self, E, fn, reads=(), writes=()):
        self._deps(E, reads, writes)
        inst = fn()
        E.sem.total += 1
        inst.then_inc(E.sem.h, 1)
        tok = (E.sem, E.sem.total, EPOCH[0])
        self._mark(tok, reads, writes)
        return tok

    def mm(self, fns, reads=(), writes=()):
        E = self.PE
        self._deps(E, reads, writes)
        inst = None
        for f in fns:
            inst = f()
        E.sem.total += 1
        inst.then_inc(E.sem.h, 1)
        tok = (E.sem, E.sem.total, EPOCH[0])
        self._mark(tok, reads, writes)
        return tok

    def dma(self, Q, pairs, sem_buf, reads=(), writes=()):
        self._deps(Q, reads, writes)
        if sem_buf.sem is None:
            if self.free_dma:
                sem_buf.sem = self.free_dma.pop()
            else:
                sem_buf.sem = self.new_sem("dma")
            self.stage_bufs.append(sem_buf)
        s = sem_buf.sem
        s.queue = Q
        for pr in pairs:
            if callable(pr):
                inst = pr()
            else:
                inst = Q.eng.dma_start(out=pr[0], in_=pr[1])
            s.total += 16
            inst.then_inc(s.h, 16)
        tok = (s, s.total, EPOCH[0])
        self._mark(tok, reads, writes)
        return tok

    def snapshot(self):
        return {s_.idx: s_.total for s_ in self.all_sems}

    def compensate(self, snap, dummy_out, dummy_in):
        eng_of = {E.sem.idx: E for E in self.engs}
        for s_ in self.all_sems:
            d = s_.total - snap.get(s_.idx, 0)
            if d <= 0:
                continue
            if s_.idx in eng_of:
                eng_of[s_.idx].eng.sem_inc(s_.h, d)
            else:
                s_.queue.eng.dma_start(out=dummy_out, in_=dummy_in).then_inc(s_.h, d)

    def reset(self):
        return

    def _reset(self):
        if not hasattr(self, "bp"):
            self.bp = [(self.new_sem("hbB"), self.new_sem("hbC")) for _ in range(3)]
            self.bp_ids = {x.idx for p in self.bp for x in p}
            self.hbn = 0
        pair = self.bp[self.hbn % 3]
        nxt = self.bp[(self.hbn + 1) % 3]
        self.hbn += 1
        for E in self.engs:
            E.eng.sem_inc(pair[0].h, 1)
        sp = self.SP.eng
        sp.wait_ge(pair[0].h, len(self.engs))
        for s_ in self.all_sems:
            if s_.idx in self.bp_ids:
                continue
            sp.sem_clear(s_.h)
            s_.total = 0
        sp.sem_clear(nxt[0].h)
        sp.sem_clear(nxt[1].h)
        sp.sem_inc(pair[1].h, 1)
        for E in self.engs:
            E.eng.wait_ge(pair[1].h, 1)
            E.seen = {}
        EPOCH[0] += 1

    def barrier(self):
        for E in self.engs:
            for s in self.all_sems:
                if s.total > 0:
                    E.wait((s, s.total, EPOCH[0]))
        for b in self.stage_bufs:
            self.free_dma.append(b.sem)
            b.sem = None
        self.stage_bufs = []


def _rel_bucket_np(dist):
    d = np.maximum(dist, 0)
    ratio = np.maximum(d, 1).astype(np.float32) / np.float32(16)
    large = 16 + (np.log(ratio).astype(np.float32) / np.float32(math.log(128 / 16)) * np.float32(16)).astype(np.int32)
    large = np.minimum(large, 31)
    return np.where(d < 16, d, large)


def make_consts():
    bf = ml_dtypes.bfloat16
    c = {}
    c["c_identf"] = np.eye(128, dtype=np.float32)
    c["c_identb"] = np.eye(128, dtype=np.float32).astype(bf)
    ob = np.zeros((128, 3, 128), np.float32)
    ob[:, 0, :] = 1.0
    ob[:, 1, :64] = 1.0
    ob[:, 2, 64:] = 1.0
    c["c_onesb"] = ob.astype(bf)
    R = np.zeros((64, 64), np.float32)
    for i in range(32):
        R[2 * i + 1, 2 * i] = -1.0
        R[2 * i, 2 * i + 1] = 1.0
    R128 = np.zeros((128, 128), np.float32)
    R128[:64, :64] = R
    R128[64:, 64:] = R
    c["c_rot"] = R128.astype(bf)
    kk = np.arange(128)[:, None]
    jj = np.arange(256)[None, :]
    mk = np.zeros((128, 2, 256), np.float32)
    mk[:, 0, :] = np.where(jj >= kk, 0.0, NEGM)
    mk[:, 1, :] = np.where((jj - kk >= 0) & (jj - kk < 128), 0.0, NEGM)
    c["c_mask"] = mk
    m = np.arange(384)
    dist = m - 127
    bk = _rel_bucket_np(dist)
    oh = np.zeros((32, 384), np.float32)
    for i in range(384):
        if dist[i] >= 0:
            oh[bk[i], i] = 1.0
    c["c_ohb"] = oh
    hh = np.arange(4, dtype=np.float32)
    log_g = np.log(1.0 - np.exp2(-5.0 - hh)).astype(np.float64)
    pos = np.arange(128)
    dec = np.zeros((128, 4, 128), np.float32)
    for h in range(4):
        rel = pos[None, :] - pos[:, None]
        dec[:, h, :] = np.where(rel >= 0, np.exp(np.maximum(rel, 0) * log_g[h]), 0.0)
    c["c_decay"] = dec
    zeta = np.exp((127 - pos)[:, None] * log_g[None, :]).astype(np.float32)
    c["c_zeta"] = zeta
    xi = np.exp((pos + 1)[:, None] * log_g[None, :])
    ang = np.repeat(1.0 / (10000.0 ** np.linspace(0.0, 1.0, 32, dtype=np.float32)), 2).astype(np.float32)
    ang = np.arange(S, dtype=np.float32)[:, None] * ang[None, :]
    sin, cos = np.sin(ang).astype(np.float32), np.cos(ang).astype(np.float32)
    tab = np.zeros((128, 6, S), np.float32)
    for p in range(128):
        d = p % 64
        tab[p, 0, :] = cos[:, d]
        tab[p, 1, :] = sin[:, d]
        for blk in range(2):
            h = 2 * blk + p // 64
            xs = xi[np.arange(S) % 128, h]
            tab[p, 2 + 2 * blk, :] = cos[:, d] * xs
            tab[p, 3 + 2 * blk, :] = sin[:, d] * xs
    c["c_tab"] = tab
    c["c_gchunk"] = np.exp(128 * log_g).astype(np.float64)
    tt_ = np.arange(128)
    c["c_ust"] = (tt_[:, None] < tt_[None, :]).astype(np.float32).astype(bf)
    c["c_ebase"] = np.broadcast_to((np.arange(8, dtype=np.float32) * 2048.0)[None, :], (128, 8)).copy()
    return c


def build_program(dbg=False, nlayers=DEPTH, stop=None):
    nc = bass.Bass("TRN2", target_bir_lowering=False)
    need_dense = (stop is None) or nlayers > 1
    need_moe = (stop is None and nlayers > 1)
    need_router = nlayers > 1
    consts = make_consts()
    gchunk = consts["c_gchunk"]

    def din(name, shape, dt=F32):
        return nc.dram_tensor(name, list(shape), dt, kind="ExternalInput")

    x_in = din("x", [S, D])
    w_in = din("w_in", [DEPTH, D, PROJ])
    rel_bias = din("rel_bias", [32, 18])
    a_lambda = din("a_lambda", [DEPTH, 4, 64])
    a_subln_g = din("a_subln_g", [DEPTH, 128])
    b_sinks = din("b_sinks", [DEPTH, 12])
    w_out = din("w_out", [DEPTH, D, D])
    ln_mix_g = din("ln_mix_g", [DEPTH, D])
    ln_mix_b = din("ln_mix_b", [DEPTH, D])
    ln_ffn_g = din("ln_ffn_g", [DEPTH, D])
    ln_ffn_b = din("ln_ffn_b", [DEPTH, D])
    if need_dense:
        dense_w_gate = din("dense_w_gate", [1, D, DFF])
        dense_w_up = din("dense_w_up", [1, D, DFF])
        dense_w_down = din("dense_w_down", [1, DFF, D])
    if need_router:
        moe_router = din("moe_router", [1, D, NE])
    if need_moe:
        moe_w_gate = din("moe_w_gate", [1, NE, D, DFF])
        moe_w_up = din("moe_w_up", [1, NE, D, DFF])
        moe_w_down = din("moe_w_down", [1, NE, DFF, D])
    c_identf = din("c_identf", [128, 128])
    c_identb = din("c_identb", [128, 128], BF16)
    c_onesb = din("c_onesb", [128, 3, 128], BF16)
    c_rot = din("c_rot", [128, 128], BF16)
    c_mask = din("c_mask", [128, 2, 256])
    c_ohb = din("c_ohb", [32, 384])
    c_decay = din("c_decay", [128, 4, 128])
    c_zeta = din("c_zeta", [128, 4])
    c_tab = din("c_tab", [128, 6, S])
    c_ust = din("c_ust", [128, 128], BF16)
    c_ebase = din("c_ebase", [128, 8])

    skind = "ExternalOutput" if dbg else "Internal"
    y_out = nc.dram_tensor("y", [S, D], F32, kind="ExternalOutput")
    yT_d = nc.dram_tensor("yT_d", [D, S], BF16, kind=skind)
    x1_d = nc.dram_tensor("x1_d", [S, D], F32, kind=skind)
    x1T_d = nc.dram_tensor("x1T_d", [D, S], BF16, kind=skind)
    x2_d = nc.dram_tensor("x2_d", [S, D], F32, kind=skind)
    z_d = nc.dram_tensor("z_d", [18, 128, 384], F32, kind=skind)
    xg_d = nc.dram_tensor("xg_d", [NE * S, D], BF16, kind="Internal")
    ye_d = nc.dram_tensor("ye_d", [NE * S, D], F32, kind="Internal")
    cnt_d = nc.dram_tensor("cnt_d", [1, NE], mybir.dt.int32, kind=skind)
    if dbg:
        idx_dbg = nc.dram_tensor("idx_dbg", [128, 16, 2], mybir.dt.int32, kind="ExternalOutput")
        gsel_dbg = nc.dram_tensor("gsel_dbg", [128, 16, 2], F32, kind="ExternalOutput")

    stack = ExitStack()
    stack.enter_context(nc.allow_low_precision("bf16 matmul operands with fp32 accumulation"))
    stack.enter_context(nc.allow_non_contiguous_dma(reason="tiny parameter loads"))
    k = K(nc, stack)
    PE, ACT, DVE, POOL, SP = k.PE, k.ACT, k.DVE, k.POOL, k.SP

    uid = [0]

    def sb(name, shape, dt, st=None):
        uid[0] += 1
        return (st or stack).enter_context(nc.sbuf_tensor(f"{name}_{uid[0]}", list(shape), dt))

    def ps(name, shape, dt, st):
        uid[0] += 1
        return st.enter_context(nc.psum_tensor(f"{name}_{uid[0]}", list(shape), dt))

    identf = sb("identf", [128, 128], F32)
    identb = sb("identb", [128, 128], BF16)
    onesb = sb("onesb", [128, 3, 128], BF16)
    rotb = sb("rotb", [128, 128], BF16)
    TA = sb("TA", [128, 6, 256], BF16)
    TB = sb("TB", [128, 12, 256], BF16)
    cA = sb("cA", [128, 6], F32)
    ust = sb("ust", [128, 128], BF16)
    ebase = sb("ebase", [128, 8], F32)
    idxs = sb("idxs", [128, 16, 2], mybir.dt.int32)
    gsel = sb("gsel", [128, 16, 2], F32)
    carry = sb("carry", [128, 8], F32)
    b_const = Buf("const")
    b_gT = Buf("gT")
    epsc = sb("epsc", [128, 2], F32)
    k.op(DVE, lambda: nc.vector.memset(epsc[:, 0:1], EPS), writes=[b_const])
    k.op(DVE, lambda: nc.vector.memset(epsc[:, 1:2], 128.0 * EPS), writes=[b_const])

    k.dma(SP, [(identf[:], c_identf.ap()), (identb[:], c_identb.ap()), (onesb[:], c_onesb.ap()),
               (rotb[:], c_rot.ap()), (ust[:], c_ust.ap()), (ebase[:], c_ebase.ap())], b_const, writes=[b_const])

    with ExitStack() as st:
        tabs = sb("tabs", [32, 18], F32, st)
        ohb = sb("ohb", [32, 384], F32, st)
        ones32 = sb("ones32", [32, 128], BF16, st)
        oht = sb("oht", [32, 2, 384], BF16, st)
        frep = sb("frep", [128, 18, 384], F32, st)
        traw = sb("traw", [128, 18, 256], F32, st)
        mask = sb("mask", [128, 2, 256], F32, st)
        pf = [ps(f"pf{i}", [128, 512], F32, st) for i in range(2)]
        b_tabs, b_frep, b_traw, b_mask = Buf(), Buf(), Buf(), Buf()
        b_oht = [Buf(), Buf()]
        b_pf = [Buf(), Buf()]
        b_ones32 = Buf()
        k.dma(SP, [(tabs[:], rel_bias.ap()), (ohb[:], c_ohb.ap())], b_tabs, writes=[b_tabs])
        k.dma(SP, [(mask[:], c_mask.ap())], b_mask, writes=[b_mask])
        k.op(DVE, lambda: nc.vector.memset(ones32[:], 1.0), writes=[b_ones32])
        for h in range(18):
            i = h % 2
            k.op(DVE, lambda: nc.vector.tensor_scalar(out=oht[:, i, :], in0=ohb[:], scalar1=tabs[:, h:h + 1], scalar2=None,
                                                      op0=ALU.mult), reads=[b_tabs], writes=[b_oht[i]])
            k.mm([lambda: nc.tensor.matmul(pf[i][:, 0:384], lhsT=ones32[:], rhs=oht[:, i, :], start=True, stop=True)],
                 reads=[b_oht[i], b_ones32], writes=[b_pf[i]])
            k.op(ACT, lambda: nc.scalar.copy(out=frep[:, h, :], in_=pf[i][:, 0:384]), reads=[b_pf[i]], writes=[b_frep])
        k.dma(SP, [(z_d.ap().rearrange("h k m -> k h m"), frep[:])], b_frep, reads=[b_frep])
        k.barrier()
        skew = bass.AP(z_d, 127, [[383, 128], [128 * 384, 18], [1, 256]])
        k.dma(SP, [(traw[:], skew)], b_traw, writes=[b_traw])
        k.op(DVE, lambda: nc.vector.tensor_copy(out=cA[:], in_=frep[:, 0:6, 382]), reads=[b_frep], writes=[b_const])
        for h in range(6):
            k.op(DVE, lambda: nc.vector.scalar_tensor_tensor(out=TA[:, h, :], in0=traw[:, h, :], scalar=cA[:, h:h + 1],
                                                             in1=mask[:, 0, :], op0=ALU.subtract, op1=ALU.add),
                 reads=[b_traw, b_mask, b_const], writes=[b_const])
        for h in range(12):
            k.op(DVE, lambda: nc.vector.tensor_tensor(out=TB[:, h, :], in0=traw[:, 6 + h, :], in1=mask[:, 1, :], op=ALU.add),
                 reads=[b_traw, b_mask], writes=[b_const])
        k.barrier()

    def layer_norm_tile(st_name, r, b_r, stats, mv, sc2, b_small, gb, bb, b_gb, out_t, b_out):
        for j in range(4):
            k.op(DVE, lambda: nc.vector.bn_stats(out=stats[:, j, :], in_=r[:, j * 512:(j + 1) * 512]),
                 reads=[b_r], writes=[b_small])
        k.op(DVE, lambda: nc.vector.bn_aggr(out=mv[:], in_=stats[:]), reads=[b_small], writes=[b_small])
        k.op(ACT, lambda: nc.scalar.activation(out=sc2[:, 0:1], in_=mv[:, 1:2], func=AF.Ln, bias=epsc[:, 0:1], scale=1.0),
             reads=[b_small, b_const], writes=[b_small])
        k.op(ACT, lambda: nc.scalar.activation(out=sc2[:, 0:1], in_=sc2[:, 0:1], func=AF.Exp, scale=-0.5),
             reads=[], writes=[b_small])
        k.op(DVE, lambda: nc.vector.scalar_tensor_tensor(out=sc2[:, 1:2], in0=mv[:, 0:1], scalar=-1.0, in1=sc2[:, 0:1],
                                                         op0=ALU.mult, op1=ALU.mult), reads=[b_small], writes=[b_small])
        k.op(ACT, lambda: nc.scalar.activation(out=out_t, in_=r[:], func=AF.Identity, bias=sc2[:, 1:2], scale=sc2[:, 0:1]),
             reads=[b_r, b_small], writes=[b_out])
        k.op(DVE, lambda: nc.vector.tensor_tensor(out=out_t, in0=out_t, in1=gb[:], op=ALU.mult),
             reads=[b_gb], writes=[b_out])
        k.op(DVE, lambda: nc.vector.tensor_tensor(out=out_t, in0=out_t, in1=bb[:], op=ALU.add),
             reads=[b_gb], writes=[b_out])

    def bcast_row(handle, off, n):
        return bass.AP(handle, off, [[0, 128], [1, n]])

    for l in range(nlayers):
        x_src = x_in if l == 0 else x2_d
        x_dst = y_out if l == nlayers - 1 else x2_d

        with ExitStack() as st:
            xT = sb("xT", [128, 16, S], BF16, st)
            b_xT = [Buf() for _ in range(16)]
            WB = [sb(f"wb{i}", [128, 16, 256], BF16, st) for i in range(2)]
            b_WB = [Buf(), Buf()]
            wb_i = [0]
            PS = [ps(f"psA{i}", [128, 512], F32, st) for i in range(8)]
            b_PS = [Buf() for _ in range(8)]

            with ExitStack() as st2:
                xin = [sb(f"xin{i}", [128, D], F32, st2) for i in range(2)]
                b_xin = [Buf(), Buf()]
                for tt in range(16):
                    i = tt % 2
                    k.dma(SP, [(xin[i][:], x_src.ap()[tt * 128:(tt + 1) * 128, :])], b_xin[i], writes=[b_xin[i]])
                    for cg in range(4):
                        pb = (tt * 4 + cg) % 8
                        k.mm([(lambda c=c: nc.tensor.transpose(PS[pb][:, (c % 4) * 128:(c % 4 + 1) * 128],
                                                               xin[i][:, c * 128:(c + 1) * 128], identf[:]))
                              for c in range(cg * 4, cg * 4 + 4)],
                             reads=[b_xin[i], b_const], writes=[b_PS[pb]])
                        E = ACT if cg % 2 == 0 else DVE
                        src = PS[pb][:].rearrange("p (c t) -> p c t", c=4)
                        dst = xT[:, cg * 4:cg * 4 + 4, tt * 128:(tt + 1) * 128]
                        if E is ACT:
                            k.op(ACT, lambda: nc.scalar.copy(out=dst, in_=src), reads=[b_PS[pb]], writes=[b_xT[tt]])
                        else:
                            k.op(DVE, lambda: nc.vector.tensor_copy(out=dst, in_=src), reads=[b_PS[pb]], writes=[b_xT[tt]])
                k.barrier()

            ps_rr = [0]

            def next_ps():
                i = ps_rr[0] % 8
                ps_rr[0] += 1
                return i

            def load_w(col_pairs, ncols):
                i = wb_i[0] % 2
                wb_i[0] += 1
                wsrc = w_in.ap()[l].rearrange("(c p) f -> p c f", p=128)
                pairs = [(WB[i][:, :, dc:dc + n], wsrc[:, :, sc:sc + n]) for (dc, sc, n) in col_pairs]
                k.dma(POOL, pairs, b_WB[i], writes=[b_WB[i]])
                return WB[i], b_WB[i]

            def proj_fm(col_pairs, nblk, scale, dsts, b_dsts):
                wt, b_wt = load_w(col_pairs, nblk * 128)
                for j in range(nblk):
                    for tb in range(4):
                        pb = next_ps()
                        k.mm([(lambda c=c: nc.tensor.matmul(PS[pb][:], lhsT=wt[:, c, j * 128:(j + 1) * 128],
                                                            rhs=xT[:, c, tb * 512:(tb + 1) * 512],
                                                            start=(c == 0), stop=(c == 15))) for c in range(16)],
                             reads=[b_wt] + b_xT[tb * 4:tb * 4 + 4], writes=[b_PS[pb]])
                        k.op(ACT, lambda: nc.scalar.activation(out=dsts[j][:, tb * 512:(tb + 1) * 512], in_=PS[pb][:],
                                                               func=AF.Copy, scale=float(scale)),
                             reads=[b_PS[pb]], writes=[b_dsts[j]])

            def proj_tm(col0, ncols, evac):
                wt, b_wt = load_w([(0, col0, ncols)], ncols)
                for tt in range(16):
                    pb = next_ps()
                    k.mm([(lambda c=c: nc.tensor.matmul(PS[pb][:, 0:ncols], lhsT=xT[:, c, tt * 128:(tt + 1) * 128],
                                                        rhs=wt[:, c, 0:ncols], start=(c == 0), stop=(c == 15)))
                          for c in range(16)],
                         reads=[b_wt, b_xT[tt]], writes=[b_PS[pb]])
                    evac(tt, PS[pb], b_PS[pb])

            def proj_fm_cols(col0, nblk, scale, dsts, b_dsts):
                for j0 in range(0, nblk, 2):
                    nb = min(2, nblk - j0)
                    proj_fm([(0, col0 + j0 * 128, nb * 128)], nb, scale, dsts[j0:j0 + nb], b_dsts[j0:j0 + nb])

            def proj_tm_cols(col0, ncols, mk_evac):
                for c0 in range(0, ncols, 256):
                    ncl = min(256, ncols - c0)
                    proj_tm(col0 + c0, ncl, mk_evac(c0, ncl))

            lam3 = sb("lam3", [128, 4, 64], F32, st)
            lamt = sb("lamt", [128, 8], F32, st)
            gsub = sb("gsub", [128, 2], F32, st)
            esink = sb("esink", [128, 6], F32, st)
            b_par = Buf()
            k.dma(SP, [(lam3[:].rearrange("p a b -> p (a b)"), bcast_row(a_lambda, l * 256, 256)),
                       (gsub[:, 0:1], bass.AP(a_subln_g, l * 128, [[1, 128], [1, 1]])),
                       (esink[0:64, :], bass.AP(b_sinks, l * 12, [[0, 64], [2, 6]])),
                       (esink[64:128, :], bass.AP(b_sinks, l * 12 + 1, [[0, 64], [2, 6]]))],
                  b_par, writes=[b_par])
            for a in range(2):
                k.op(DVE, lambda: nc.vector.tensor_tensor(out=lam3[:, 2 * a, :], in0=lam3[:, 2 * a, :], in1=lam3[:, 2 * a + 1, :],
                                                          op=ALU.mult), reads=[b_par], writes=[b_par])
                k.op(DVE, lambda: nc.vector.reduce_sum(out=lamt[:, a:a + 1], in_=lam3[:, 2 * a, :], axis=mybir.AxisListType.X),
                     reads=[b_par], writes=[b_par])
            k.op(ACT, lambda: nc.scalar.activation(out=lamt[:, 2:4], in_=lamt[:, 0:2], func=AF.Exp), reads=[b_par], writes=[b_par])
            k.op(DVE, lambda: nc.vector.tensor_tensor(out=lamt[:, 4:5], in0=lamt[:, 3:4], in1=lamt[:, 2:3], op=ALU.subtract),
                 reads=[b_par], writes=[b_par])
            k.op(DVE, lambda: nc.vector.tensor_scalar(out=lamt[:, 4:5], in0=lamt[:, 4:5], scalar1=-lambda_init(l), scalar2=None,
                                                      op0=ALU.add), reads=[b_par], writes=[b_par])
            k.op(DVE, lambda: nc.vector.tensor_scalar(out=gsub[:, 1:2], in0=gsub[:, 0:1],
                                                      scalar1=float(math.sqrt(128.0) * (1.0 - lambda_init(l))), scalar2=None,
                                                      op0=ALU.mult), reads=[b_par], writes=[b_par])
            k.op(ACT, lambda: nc.scalar.activation(out=esink[:], in_=esink[:], func=AF.Exp), reads=[b_par], writes=[b_par])
            neglam = lamt[:, 4:5]

            with ExitStack() as st2:
                Cqf = [sb(f"Cqf{j}", [128, S], BF16, st2) for j in range(2)]
                Cqx = [sb(f"Cqx{j}", [128, S], BF16, st2) for j in range(2)]
                Ckf = [sb(f"Ckf{j}", [128, S], BF16, st2) for j in range(2)]
                Ckt = sb("Ckt", [128, 16, 256], BF16, st2)
                Cv = sb("Cv", [128, 16, 512], BF16, st2)
                Csg = sb("Csg", [128, 16, 512], BF16, st2)
                decay = sb("decay", [128, 4, 128], F32, st2)
                zeta = sb("zeta", [128, 4], F32, st2)
                b_Cqf, b_Cqx, b_Ckf = [Buf(), Buf()], [Buf(), Buf()], [Buf(), Buf()]
                b_Ckt, b_Cv, b_Csg = Buf(), Buf(), Buf()
                b_cc = Buf()
                k.dma(SP, [(decay[:], c_decay.ap()), (zeta[:], c_zeta.ap())], b_cc, writes=[b_cc])

                def mk_evac_cv(c0, ncl):
                    def ev(tt, p, b_p):
                        k.op(ACT, lambda: nc.scalar.copy(out=Cv[:, tt, c0:c0 + ncl], in_=p[:, 0:ncl]), reads=[b_p], writes=[b_Cv])
                    return ev
                proj_tm_cols(O_CV, 512, mk_evac_cv)

                def mk_evac_cg(c0, ncl):
                    def ev(tt, p, b_p):
                        k.op(ACT, lambda: nc.scalar.activation(out=Csg[:, tt, c0:c0 + ncl], in_=p[:, 0:ncl], func=AF.Silu),
                             reads=[b_p], writes=[b_Csg])
                    return ev
                proj_tm_cols(O_CG, 512, mk_evac_cg)

                with ExitStack() as st3:
                    CqT = [sb(f"CqT{j}", [128, S], BF16, st3) for j in range(2)]
                    CkT = [sb(f"CkT{j}", [128, S], BF16, st3) for j in range(2)]
                    tabt = [sb(f"tabt{i}", [128, 6, 512], F32, st3) for i in range(1)]
                    tmp1 = [sb(f"rtmp{i}", [128, 512], F32, st3) for i in range(2)]
                    b_CqT, b_CkT = [Buf(), Buf()], [Buf(), Buf()]
                    b_tabt = [Buf()]
                    b_tmp1 = [Buf(), Buf()]
                    proj_fm_cols(O_CQ, 2, 1.0, CqT, b_CqT)
                    proj_fm_cols(O_CK, 2, 0.125, CkT, b_CkT)
                    for tb in range(4):
                        ti = 0
                        k.dma(SP, [(tabt[ti][:], c_tab.ap()[:, :, tb * 512:(tb + 1) * 512])], b_tabt[ti], writes=[b_tabt[ti]])
                        for j in range(2):
                            for (src, b_src, outs) in ((CqT[j], b_CqT[j], [(Cqf[j], b_Cqf[j], 0, 1), (Cqx[j], b_Cqx[j], 2 + 2 * j, 3 + 2 * j)]),
                                                       (CkT[j], b_CkT[j], [(Ckf[j], b_Ckf[j], 0, 1)])):
                                pb = next_ps()
                                sl = slice(tb * 512, (tb + 1) * 512)
                                k.mm([lambda: nc.tensor.matmul(PS[pb][:], lhsT=rotb[:], rhs=src[:, sl], start=True, stop=True)],
                                     reads=[b_src, b_const], writes=[b_PS[pb]])
                                for (dst, b_dst, ic, isn) in outs:
                                    t1 = tmp1[0]
                                    t2 = tmp1[1]
                                    k.op(DVE, lambda: nc.vector.tensor_tensor(out=t1[:], in0=src[:, sl], in1=tabt[ti][:, ic, :], op=ALU.mult),
                                         reads=[b_src, b_tabt[ti]], writes=[b_tmp1[0]])
                                    k.op(DVE, lambda: nc.vector.tensor_tensor(out=t2[:], in0=PS[pb][:], in1=tabt[ti][:, isn, :], op=ALU.mult),
                                         reads=[b_PS[pb], b_tabt[ti]], writes=[b_tmp1[1]])
                                    k.op(DVE, lambda: nc.vector.tensor_tensor(out=dst[:, sl], in0=t1[:], in1=t2[:], op=ALU.add),
                                         reads=[b_tmp1[0], b_tmp1[1]], writes=[b_dst])
                    k.barrier()
                PSb = [PS[i][:].bitcast(BF16) if hasattr(PS[i][:], "bitcast") else None for i in range(8)]
                for tt in range(16):
                    pb = next_ps()
                    pview = PSb[pb]
                    k.mm([(lambda j=j: nc.tensor.transpose(pview[:, j * 128:(j + 1) * 128], Ckf[j][:, tt * 128:(tt + 1) * 128], identb[:]))
                          for j in range(2)], reads=[b_Ckf[0], b_Ckf[1], b_const], writes=[b_PS[pb]])
                    for h in range(4):
                        k.op(DVE, lambda: nc.vector.tensor_scalar(out=Ckt[:, tt, h * 64:(h + 1) * 64], in0=pview[:, h * 64:(h + 1) * 64],
                                                                  scalar1=zeta[:, h:h + 1], scalar2=None, op0=ALU.mult),
                             reads=[b_PS[pb], b_cc], writes=[b_Ckt])

                ST = sb("ST", [128, 2, 128], F32, st2)
                STb = [sb(f"STb{i}", [128, 2, 128], BF16, st2) for i in range(2)]
                iT = [sb(f"iT{i}", [128, 128], BF16, st2) for i in range(4)]
                ycm = [sb(f"ycm{i}", [128, 512], BF16, st2) for i in range(2)]
                ycT = [sb(f"ycT{i}", [128, 4, 128], BF16, st2) for i in range(2)]
                cst = sb("cst", [128, 4, 6], F32, st2)
                cmv = sb("cmv", [128, 4, 2], F32, st2)
                crs = sb("crs", [128, 4], F32, st2)
                ctmp = sb("ctmp", [128, 512], F32, st2)
                b_ST, b_STb, b_iT = Buf(), [Buf(), Buf()], [Buf() for _ in range(4)]
                b_ycm, b_ycT, b_cs, b_ctmp = [Buf(), Buf()], [Buf(), Buf()], Buf(), Buf()
                k.op(DVE, lambda: nc.vector.memset(ST[:], 0.0), writes=[b_ST])
                for n in range(16):
                    cs = slice(n * 128, (n + 1) * 128)
                    po = next_ps()
                    for h in range(4):
                        blk, p0 = h // 2, 64 * (h % 2)
                        pi = next_ps()
                        if pi == po:
                            pi = next_ps()
                        k.mm([lambda: nc.tensor.matmul(PS[pi][:, 0:128], lhsT=Ckf[blk][p0:p0 + 64, cs], rhs=Cqf[blk][p0:p0 + 64, cs],
                                                       start=True, stop=True)],
                             reads=[b_Ckf[blk], b_Cqf[blk]], writes=[b_PS[pi]])
                        it = (n * 4 + h) % 4
                        k.op(DVE, lambda: nc.vector.tensor_tensor(out=iT[it][:], in0=PS[pi][:, 0:128], in1=decay[:, h, :], op=ALU.mult),
                             reads=[b_PS[pi], b_cc], writes=[b_iT[it]])
                        fns = [lambda: nc.tensor.matmul(PS[po][:, h * 128:(h + 1) * 128], lhsT=iT[it][:], rhs=Cv[:, n, h * 128:(h + 1) * 128],
                                                        start=True, stop=(n == 0))]
                        rds = [b_iT[it], b_Cv]
                        if n > 0:
                            sbf = STb[n % 2]
                            fns.append(lambda: nc.tensor.matmul(PS[po][:, h * 128:(h + 1) * 128], lhsT=Cqx[blk][p0:p0 + 64, cs],
                                                                rhs=sbf[p0:p0 + 64, blk, :], start=False, stop=True))
                            rds += [b_Cqx[blk], b_STb[n % 2]]
                        k.mm(fns, reads=rds, writes=[b_PS[po]])
                    if n < 15:
                        for blk in range(2):
                            pk = next_ps()
                            if pk == po:
                                pk = next_ps()
                            k.mm([lambda: nc.tensor.matmul(PS[pk][:, 0:256], lhsT=Ckt[:, n, blk * 128:(blk + 1) * 128],
                                                           rhs=Cv[:, n, blk * 256:(blk + 1) * 256], start=True, stop=True)],
                                 reads=[b_Ckt, b_Cv], writes=[b_PS[pk]])
                            for par in range(2):
                                h = 2 * blk + par
                                pr = slice(64 * par, 64 * par + 64)
                                k.op(DVE, lambda: nc.vector.scalar_tensor_tensor(out=ST[pr, blk, :], in0=ST[pr, blk, :],
                                                                                 scalar=float(gchunk[h]),
                                                                                 in1=PS[pk][pr, par * 128:(par + 1) * 128],
                                                                                 op0=ALU.mult, op1=ALU.add),
                                     reads=[b_PS[pk]], writes=[b_ST])
                        nb = (n + 1) % 2
                        k.op(ACT, lambda: nc.scalar.copy(out=STb[nb][:], in_=ST[:]), reads=[b_ST], writes=[b_STb[nb]])
                    yi = n % 2
                    for h in range(4):
                        k.op(DVE, lambda: nc.vector.bn_stats(out=cst[:, h, :], in_=PS[po][:, h * 128:(h + 1) * 128]),
                             reads=[b_PS[po]], writes=[b_cs])
                        k.op(DVE, lambda: nc.vector.bn_aggr(out=cmv[:, h, :], in_=cst[:, h, :]), reads=[b_cs], writes=[b_cs])
                    k.op(ACT, lambda: nc.scalar.activation(out=crs[:], in_=cmv[:, :, 1], func=AF.Ln, bias=epsc[:, 0:1], scale=1.0),
                         reads=[b_cs, b_const], writes=[b_cs])
                    k.op(ACT, lambda: nc.scalar.activation(out=crs[:], in_=crs[:], func=AF.Exp, scale=-0.5),
                         reads=[], writes=[b_cs])
                    for h in range(4):
                        hs = slice(h * 128, (h + 1) * 128)
                        k.op(DVE, lambda: nc.vector.tensor_scalar(out=ctmp[:, hs], in0=PS[po][:, hs], scalar1=cmv[:, h, 0:1],
                                                                  scalar2=crs[:, h:h + 1], op0=ALU.subtract, op1=ALU.mult),
                             reads=[b_PS[po], b_cs], writes=[b_ctmp])
                    k.op(DVE, lambda: nc.vector.tensor_tensor(out=ycm[yi][:], in0=ctmp[:], in1=Csg[:, n, :], op=ALU.mult),
                         reads=[b_ctmp, b_Csg], writes=[b_ycm[yi]])
                    pt = next_ps()
                    pview = PSb[pt]
                    k.mm([(lambda h=h: nc.tensor.transpose(pview[:, h * 128:(h + 1) * 128], ycm[yi][:, h * 128:(h + 1) * 128], identb[:]))
                          for h in range(4)], reads=[b_ycm[yi], b_const], writes=[b_PS[pt]])
                    k.op(ACT, lambda: nc.scalar.copy(out=ycT[yi][:].rearrange("p h q -> p (h q)"), in_=pview[:, 0:512]),
                         reads=[b_PS[pt]], writes=[b_ycT[yi]])
                    k.dma(SP, [(yT_d.ap()[1536:2048, cs].rearrange("(h e) q -> e h q", e=128), ycT[yi][:])], b_ycT[yi],
                          reads=[b_ycT[yi]])
                k.barrier()
            if stop == "C" and l == nlayers - 1:
                break

            with ExitStack() as st2:
                AqT = [sb(f"AqT{j}", [128, S], BF16, st2) for j in range(6)]
                AkT = [sb(f"AkT{j}", [128, S], BF16, st2) for j in range(6)]
                Av = sb("Av", [128, 16, 768], BF16, st2)
                b_AqT = [Buf() for _ in range(6)]
                b_AkT = [Buf() for _ in range(6)]
                b_Av = Buf()
                proj_fm_cols(O_AQ, 6, 0.125, AqT, b_AqT)
                proj_fm_cols(O_AK, 6, 1.0, AkT, b_AkT)

                def mk_evac_av(c0, ncol):
                    def ev(tt, p, b_p):
                        k.op(ACT, lambda: nc.scalar.copy(out=Av[:, tt, c0:c0 + ncol], in_=p[:, 0:ncol]), reads=[b_p], writes=[b_Av])
                    return ev
                proj_tm_cols(O_AV, 768, mk_evac_av)

                PT = [sb(f"PT{i}", [128, 512], BF16, st2) for i in range(4)]
                b_PT = [Buf() for _ in range(4)]
                fa = [sb(f"fa{i}", [128, 512], F32, st2) for i in range(4)]
                b_fa = [Buf() for _ in range(4)]
                sqb = sb("sqb", [128, 512], BF16, st2)
                b_sq = Buf()
                yst = [sb(f"yst{i}", [128, 512], BF16, st2) for i in range(2)]
                b_yst = [Buf(), Buf()]
                step = [0]
                blkc = 0
                for h in range(6):
                    for qb in range(4):
                        nkt = 4 * qb + 4
                        pend = []
                        q0 = qb * 512

                        def emit_pv(item):
                            kt, m, c0, pti = item
                            Ob, Sb = 2 * m, 2 * m + 1
                            k.mm([lambda: nc.tensor.matmul(PS[Ob][:, c0:512], lhsT=Av[:, kt, h * 128:(h + 1) * 128], rhs=PT[pti][:, c0:512],
                                                           start=(kt == 0), stop=(kt == nkt - 1)),
                                  lambda: nc.tensor.matmul(PS[Sb][:, c0:512], lhsT=onesb[:, 0, :], rhs=PT[pti][:, c0:512],
                                                           start=(kt == 0), stop=(kt == nkt - 1))],
                                 reads=[b_Av, b_PT[pti], b_const], writes=[b_PS[Ob], b_PS[Sb]])

                        for kt in range(nkt):
                            j0 = q0 - 128 * kt
                            c0 = max(0, -j0)
                            for m in range(2):
                                sci = 4 + step[0] % 4
                                pti = step[0] % 4
                                step[0] += 1
                                pr = slice(64 * m, 64 * m + 64)
                                fns = [lambda: nc.tensor.matmul(PS[sci][:, c0:512], lhsT=AkT[h][pr, kt * 128:(kt + 1) * 128],
                                                                rhs=AqT[h][pr, q0 + c0:q0 + 512], start=True, stop=(j0 >= 256))]
                                if j0 < 256:
                                    jA = max(j0, 0)
                                    cA_, cB_ = jA - j0, min(512, 256 - j0)
                                    jB = cB_ + j0
                                    fns.append(lambda: nc.tensor.matmul(PS[sci][:, cA_:cB_], lhsT=identb[:], rhs=TA[:, h, jA:jB],
                                                                        start=False, stop=True))
                                k.mm(fns, reads=[b_AkT[h], b_AqT[h], b_const], writes=[b_PS[sci]])
                                k.op(ACT, lambda: nc.scalar.activation(out=PT[pti][:, c0:512], in_=PS[sci][:, c0:512], func=AF.Exp,
                                                                       bias=cA[:, h:h + 1], scale=1.0),
                                     reads=[b_PS[sci], b_const], writes=[b_PT[pti]])
                                pend.append((kt, m, c0, pti))
                                if len(pend) > 2:
                                    emit_pv(pend.pop(0))
                        while pend:
                            emit_pv(pend.pop(0))
                        k.op(DVE, lambda: nc.vector.reciprocal(out=fa[0][:], in_=PS[1][:]), reads=[b_PS[1]], writes=[b_fa[0]])
                        k.op(DVE, lambda: nc.vector.tensor_tensor(out=fa[0][:], in0=fa[0][:], in1=PS[0][:], op=ALU.mult),
                             reads=[b_PS[0]], writes=[b_fa[0]])
                        k.op(DVE, lambda: nc.vector.reciprocal(out=fa[1][:], in_=PS[3][:]), reads=[b_PS[3]], writes=[b_fa[1]])
                        k.op(DVE, lambda: nc.vector.tensor_tensor(out=fa[1][:], in0=fa[1][:], in1=PS[2][:], op=ALU.mult),
                             reads=[b_PS[2]], writes=[b_fa[1]])
                        k.op(DVE, lambda: nc.vector.scalar_tensor_tensor(out=fa[2][:], in0=fa[1][:], scalar=neglam, in1=fa[0][:],
                                                                         op0=ALU.mult, op1=ALU.add),
                             reads=[b_fa[0], b_fa[1], b_par], writes=[b_fa[2]])
                        k.op(DVE, lambda: nc.vector.tensor_tensor(out=sqb[:], in0=fa[2][:], in1=fa[2][:], op=ALU.mult),
                             reads=[b_fa[2]], writes=[b_sq])
                        sci = 4 + step[0] % 4
                        step[0] += 1
                        k.mm([lambda: nc.tensor.matmul(PS[sci][:], lhsT=onesb[:, 0, :], rhs=sqb[:], start=True, stop=True)],
                             reads=[b_sq, b_const], writes=[b_PS[sci]])
                        k.op(ACT, lambda: nc.scalar.activation(out=fa[3][:], in_=PS[sci][:], func=AF.Ln, bias=epsc[:, 1:2], scale=1.0),
                             reads=[b_PS[sci], b_const], writes=[b_fa[3]])
                        k.op(ACT, lambda: nc.scalar.activation(out=fa[3][:], in_=fa[3][:], func=AF.Exp, scale=-0.5),
                             reads=[], writes=[b_fa[3]])
                        yi = blkc % 2
                        blkc += 1
                        k.op(DVE, lambda: nc.vector.scalar_tensor_tensor(out=yst[yi][:], in0=fa[2][:], scalar=gsub[:, 1:2], in1=fa[3][:],
                                                                         op0=ALU.mult, op1=ALU.mult),
                             reads=[b_fa[2], b_fa[3], b_par], writes=[b_yst[yi]])
                        k.dma(SP, [(yT_d.ap()[h * 128:(h + 1) * 128, q0:q0 + 512], yst[yi][:])], b_yst[yi], reads=[b_yst[yi]])
                k.barrier()
            if stop == "A" and l == nlayers - 1:
                break

            with ExitStack() as st2:
                BqT = [sb(f"BqT{j}", [128, S], BF16, st2) for j in range(6)]
                BkT = [sb(f"BkT{j}", [128, S], BF16, st2) for j in range(3)]
                Bvp = sb("Bvp", [128, 16, 3, 2, 128], BF16, st2)
                b_BqT = [Buf() for _ in range(6)]
                b_BkT = [Buf() for _ in range(3)]
                b_Bvp = Buf()
                k.op(DVE, lambda: nc.vector.memset(Bvp[:].rearrange("p a b c d -> p (a b c d)"), 0.0), writes=[b_Bvp])
                proj_fm_cols(O_BQ, 6, 0.125, BqT, b_BqT)
                proj_fm([(0, O_BK, 64), (64, O_BK, 64), (128, O_BK + 64, 64), (192, O_BK + 64, 64)], 2, 1.0, BkT[0:2], b_BkT[0:2])
                proj_fm([(0, O_BK + 128, 64), (64, O_BK + 128, 64)], 1, 1.0, BkT[2:3], b_BkT[2:3])

                def evac_bv(tt, p, b_p):
                    src = p[:, 0:192].rearrange("p (g e) -> p g e", g=3)
                    k.op(ACT, lambda: nc.scalar.copy(out=Bvp[:, tt, :, 0, 0:64], in_=src), reads=[b_p], writes=[b_Bvp])
                    k.op(DVE, lambda: nc.vector.tensor_copy(out=Bvp[:, tt, :, 1, 64:128], in_=src), reads=[b_p], writes=[b_Bvp])
                proj_tm(O_BV, 192, evac_bv)

                PT = [sb(f"PTb{i}", [128, 256], BF16, st2) for i in range(4)]
                b_PT = [Buf() for _ in range(4)]
                fb_ = [sb(f"fb{i}", [128, 512], F32, st2) for i in range(2)]
                b_fb = [Buf(), Buf()]
                yst = [sb(f"ystb{i}", [128, 512], BF16, st2) for i in range(2)]
                b_yst = [Buf(), Buf()]
                step = [0]
                blkc = 0
                for i in range(6):
                    g = i // 2
                    for qb in range(4):
                        q0 = qb * 512
                        kts = list(range(max(0, 4 * qb - 1), 4 * qb + 4))
                        for kt in kts:
                            cs_ = max(0, 128 * kt - q0)
                            ce_ = min(512, 128 * kt + 256 - q0)
                            N = ce_ - cs_
                            jA = q0 + cs_ - 128 * kt
                            for par in range(2):
                                pr = slice(64 * par, 64 * par + 64)
                                sci = 4 + step[0] % 4
                                pti = step[0] % 4
                                step[0] += 1
                                hb = 2 * i + par
                                k.mm([lambda: nc.tensor.matmul(PS[sci][:, 0:N], lhsT=BkT[g][pr, kt * 128:(kt + 1) * 128],
                                                               rhs=BqT[i][pr, q0 + cs_:q0 + ce_], start=True, stop=False),
                                      lambda: nc.tensor.matmul(PS[sci][:, 0:N], lhsT=identb[:], rhs=TB[:, hb, jA:jA + N],
                                                               start=False, stop=True)],
                                     reads=[b_BkT[g], b_BqT[i], b_const], writes=[b_PS[sci]])
                                k.op(ACT, lambda: nc.scalar.activation(out=PT[pti][:, 0:N], in_=PS[sci][:, 0:N], func=AF.Exp),
                                     reads=[b_PS[sci]], writes=[b_PT[pti]])
                                fns = []
                                wr = set()
                                for sub in range(N // 128):
                                    cc = cs_ + sub * 128
                                    qt = (q0 + cc) // 128
                                    qtl = cc // 128
                                    bank = qtl % 2
                                    col = (qtl // 2) * 128
                                    first = (kt == max(0, qt - 1)) and par == 0
                                    last = (kt == qt) and par == 1
                                    fns.append(lambda bank=bank, col=col, sub=sub, first=first, last=last:
                                               nc.tensor.matmul(PS[bank][:, col:col + 128], lhsT=Bvp[:, kt, g, par, :],
                                                                rhs=PT[pti][:, sub * 128:(sub + 1) * 128], start=first, stop=last))
                                    fns.append(lambda bank=bank, col=col, sub=sub, first=first, last=last:
                                               nc.tensor.matmul(PS[2 + bank][:, col:col + 128], lhsT=onesb[:, 1 + par, :],
                                                                rhs=PT[pti][:, sub * 128:(sub + 1) * 128], start=first, stop=last))
                                    wr.add(bank)
                                    wr.add(2 + bank)
                                k.mm(fns, reads=[b_Bvp, b_PT[pti], b_const], writes=[b_PS[w] for w in sorted(wr)])
                        yi = blkc % 2
                        blkc += 1
                        for bank in range(2):
                            dstv = fb_[0][:].rearrange("p (a b c) -> p a b c", a=2, b=2)[:, :, bank, :]
                            k.op(DVE, lambda: nc.vector.tensor_scalar(out=dstv, in0=PS[2 + bank][:, 0:256].rearrange("p (a c) -> p a c", a=2),
                                                                      scalar1=esink[:, i:i + 1], scalar2=None, op0=ALU.add),
                                 reads=[b_PS[2 + bank], b_par], writes=[b_fb[0]])
                        k.op(DVE, lambda: nc.vector.reciprocal(out=fb_[1][:], in_=fb_[0][:]), reads=[b_fb[0]], writes=[b_fb[1]])
                        for bank in range(2):
                            dstv = yst[yi][:].rearrange("p (a b c) -> p a b c", a=2, b=2)[:, :, bank, :]
                            rv = fb_[1][:].rearrange("p (a b c) -> p a b c", a=2, b=2)[:, :, bank, :]
                            k.op(DVE, lambda: nc.vector.tensor_tensor(out=dstv, in0=PS[bank][:, 0:256].rearrange("p (a c) -> p a c", a=2),
                                                                      in1=rv, op=ALU.mult),
                                 reads=[b_PS[bank], b_fb[1]], writes=[b_yst[yi]])
                        k.dma(SP, [(yT_d.ap()[768 + i * 128:768 + (i + 1) * 128, q0:q0 + 512], yst[yi][:])], b_yst[yi],
                              reads=[b_yst[yi]])
                k.barrier()
        if stop == "B" and l == nlayers - 1:
            break

        is_moe = (l % 2 == 1)
        with ExitStack() as st:
            Wo = sb("Wo", [128, 16, D], BF16, st)
            b_Wo = Buf()
            wsrc = w_out.ap()[l].rearrange("(c p) f -> p c f", p=128)
            k.dma(POOL, [(Wo[:, :, j * 512:(j + 1) * 512], wsrc[:, :, j * 512:(j + 1) * 512]) for j in range(4)], b_Wo, writes=[b_Wo])
            gb = sb("gb", [128, D], F32, st)
            bb = sb("bb", [128, D], F32, st)
            b_gb = Buf()
            k.dma(SP, [(gb[:], bcast_row(ln_mix_g, l * D, D)), (bb[:], bcast_row(ln_mix_b, l * D, D))], b_gb, writes=[b_gb])
            yt = [sb(f"yt{i}", [128, 16, 128], BF16, st) for i in range(2)]
            xr = [sb(f"xr{i}", [128, D], F32, st) for i in range(2)]
            rr = [sb(f"rr{i}", [128, D], F32, st) for i in range(2)]
            x1 = [sb(f"x1{i}", [128, D], F32, st) for i in range(2)]
            x1Tb = [sb(f"x1Tb{i}", [128, 16, 128], BF16, st) for i in range(2)]
            stats = sb("stats", [128, 4, 6], F32, st)
            mv = sb("mv", [128, 2], F32, st)
            sc2 = sb("sc2", [128, 2], F32, st)
            b_yt, b_xr, b_rr, b_x1, b_x1Tb = [Buf(), Buf()], [Buf(), Buf()], [Buf(), Buf()], [Buf(), Buf()], [Buf(), Buf()]
            b_small = Buf()
            PS = [ps(f"psB{i}", [128, 512], F32, st) for i in range(8)]
            b_PS = [Buf() for _ in range(8)]
            if is_moe:
                Wr = sb("Wr", [128, 16, NE], F32, st)
                Wrh = sb("Wrh", [128, 16, NE], BF16, st)
                Wrl = sb("Wrl", [128, 16, NE], BF16, st)
                x1Tf = sb("x1Tl", [128, 16, 128], BF16, st)
                gl = sb("gl", [128, 6, NE], F32, st)
                gsm = sb("gsm", [128, 4], F32, st)
                SELb = sb("SELb", [128, NE], BF16, st)
                rk = sb("rk", [128, NE], F32, st)
                tmp8 = sb("tmp8", [128, NE], F32, st)
                sfl = sb("sfl", [128, 2], F32, st)
                cnti = sb("cnti", [128, NE], mybir.dt.int32, st)
                x1bf = [sb(f"x1bf{i}", [128, D], BF16, st) for i in range(2)]
                b_x1bf = [Buf(), Buf()]
                b_idx = Buf()
                b_Wr, b_x1Tf, b_gl = Buf(), Buf(), Buf()
                k.op(DVE, lambda: nc.vector.memset(carry[:], 0.0), writes=[b_gT])
                k.dma(SP, [(Wr[:], moe_router.ap()[0].rearrange("(c p) e -> p c e", p=128))], b_Wr, writes=[b_Wr])
                k.op(DVE, lambda: nc.vector.tensor_copy(out=Wrh[:], in_=Wr[:]), reads=[b_Wr], writes=[b_Wr])
                k.op(DVE, lambda: nc.vector.tensor_tensor(out=Wrl[:], in0=Wr[:], in1=Wrh[:], op=ALU.subtract), reads=[b_Wr], writes=[b_Wr])
            ysrc = yT_d.ap().rearrange("(c p) t -> p c t", p=128)
            x1Tdst = x1T_d.ap().rearrange("(c p) t -> p c t", p=128)
            for tt in range(16):
                i = tt % 2
                ts_ = slice(tt * 128, (tt + 1) * 128)
                k.dma(SP, [(yt[i][:], ysrc[:, :, ts_])], b_yt[i], writes=[b_yt[i]])
                k.dma(SP, [(xr[i][:], x_src.ap()[ts_, :])], b_xr[i], writes=[b_xr[i]])
                for cg in range(4):
                    k.mm([(lambda c=c: nc.tensor.matmul(PS[cg][:], lhsT=yt[i][:, c, :], rhs=Wo[:, c, cg * 512:(cg + 1) * 512],
                                                        start=(c == 0), stop=(c == 15))) for c in range(16)],
                         reads=[b_yt[i], b_Wo], writes=[b_PS[cg]])
                    k.op(DVE, lambda: nc.vector.scalar_tensor_tensor(out=rr[i][:, cg * 512:(cg + 1) * 512], in0=xr[i][:, cg * 512:(cg + 1) * 512],
                                                                     scalar=float(ALPHA), in1=PS[cg][:], op0=ALU.mult, op1=ALU.add),
                         reads=[b_xr[i], b_PS[cg]], writes=[b_rr[i]])
                layer_norm_tile("mix", rr[i], b_rr[i], stats, mv, sc2, b_small, gb, bb, b_gb, x1[i][:], b_x1[i])
                k.dma(SP, [(x1_d.ap()[ts_, :], x1[i][:])], b_x1[i], reads=[b_x1[i]])
                for cg in range(4):
                    pb = 4 + cg
                    k.mm([(lambda c=c: nc.tensor.transpose(PS[pb][:, (c % 4) * 128:(c % 4 + 1) * 128],
                                                           x1[i][:, c * 128:(c + 1) * 128], identf[:]))
                          for c in range(cg * 4, cg * 4 + 4)], reads=[b_x1[i], b_const], writes=[b_PS[pb]])
                    src = PS[pb][:].rearrange("p (c t) -> p c t", c=4)
                    k.op(ACT, lambda: nc.scalar.copy(out=x1Tb[i][:, cg * 4:cg * 4 + 4, :], in_=src), reads=[b_PS[pb]], writes=[b_x1Tb[i]])
                    if is_moe:
                        k.op(DVE, lambda: nc.vector.tensor_tensor(out=x1Tf[:, cg * 4:cg * 4 + 4, :], in0=src, in1=x1Tb[i][:, cg * 4:cg * 4 + 4, :],
                                                                  op=ALU.subtract), reads=[b_PS[pb], b_x1Tb[i]], writes=[b_x1Tf])
                k.dma(SP, [(x1Tdst[:, :, ts_], x1Tb[i][:])], b_x1Tb[i], reads=[b_x1Tb[i]])
                if is_moe:
                    rfn = []
                    for (xa, wa) in ((x1Tb[i], Wrh), (x1Tf, Wrh), (x1Tb[i], Wrl)):
                        for c in range(16):
                            rfn.append(lambda c=c, xa=xa, wa=wa: nc.tensor.matmul(PS[0][:, 0:NE], lhsT=xa[:, c, :], rhs=wa[:, c, :],
                                                                                  start=(len(rfn_i) == 0), stop=(len(rfn_i) == 47)))
                    rfn_i = []

                    def _run(f):
                        r = f()
                        rfn_i.append(1)
                        return r
                    k.mm([(lambda f=f: _run(f)) for f in rfn], reads=[b_x1Tf, b_x1Tb[i], b_Wr], writes=[b_PS[0]])
                    L, L2, SELm, Wg_, G = gl[:, 0, :], gl[:, 1, :], gl[:, 2, :], gl[:, 3, :], gl[:, 4, :]
                    AX = mybir.AxisListType.X
                    ops = [
                        (DVE, lambda: nc.vector.tensor_copy(out=L, in_=PS[0][:, 0:NE]), [b_PS[0]]),
                        (DVE, lambda: nc.vector.reduce_max(out=gsm[:, 0:1], in_=L, axis=AX), []),
                        (DVE, lambda: nc.vector.tensor_scalar(out=L2, in0=L, scalar1=gsm[:, 0:1], scalar2=-1e30, op0=ALU.is_equal, op1=ALU.mult), []),
                        (DVE, lambda: nc.vector.tensor_tensor(out=L2, in0=L2, in1=L, op=ALU.add), []),
                        (DVE, lambda: nc.vector.reduce_max(out=gsm[:, 1:2], in_=L2, axis=AX), []),
                        (DVE, lambda: nc.vector.tensor_scalar(out=SELm, in0=L, scalar1=gsm[:, 1:2], scalar2=None, op0=ALU.is_ge), []),
                        (DVE, lambda: nc.vector.tensor_scalar(out=gsm[:, 2:3], in0=gsm[:, 0:1], scalar1=-1.0, scalar2=None, op0=ALU.mult), []),
                        (ACT, lambda: nc.scalar.activation(out=Wg_, in_=L, func=AF.Exp, bias=gsm[:, 2:3], scale=1.0), []),
                        (DVE, lambda: nc.vector.tensor_tensor(out=Wg_, in0=Wg_, in1=SELm, op=ALU.mult), []),
                        (DVE, lambda: nc.vector.reduce_sum(out=gsm[:, 3:4], in_=Wg_, axis=AX), []),
                        (DVE, lambda: nc.vector.reciprocal(out=gsm[:, 3:4], in_=gsm[:, 3:4]), []),
                        (DVE, lambda: nc.vector.tensor_scalar(out=G, in0=Wg_, scalar1=gsm[:, 3:4], scalar2=None, op0=ALU.mult), []),
                    ]
                    for (E, f, rd) in ops:
                        k.op(E, f, reads=rd + [b_gl], writes=[b_gl])
                    eq1, eq2 = gl[:, 5, :], gl[:, 1, :]
                    for f in (lambda: nc.vector.tensor_scalar(out=eq1, in0=L, scalar1=gsm[:, 0:1], scalar2=None, op0=ALU.is_equal),
                              lambda: nc.vector.tensor_tensor(out=eq2, in0=SELm, in1=eq1, op=ALU.subtract),
                              lambda: nc.vector.tensor_copy(out=SELb[:], in_=SELm)):
                        k.op(DVE, f, reads=[b_gl], writes=[b_gl])
                    k.mm([lambda: nc.tensor.matmul(PS[1][:, 0:NE], lhsT=ust[:], rhs=SELb[:], start=True, stop=True),
                          lambda: nc.tensor.matmul(PS[1][:, NE:2 * NE], lhsT=onesb[:, 0, :], rhs=SELb[:], start=True, stop=True)],
                         reads=[b_gl, b_const], writes=[b_PS[1]])
                    for f in (lambda: nc.vector.tensor_tensor(out=rk[:], in0=PS[1][:, 0:NE], in1=carry[:], op=ALU.add),
                              lambda: nc.vector.tensor_tensor(out=rk[:], in0=rk[:], in1=ebase[:], op=ALU.add),
                              lambda: nc.vector.tensor_tensor(out=carry[:], in0=carry[:], in1=PS[1][:, NE:2 * NE], op=ALU.add),
                              lambda: nc.vector.tensor_tensor(out=tmp8[:], in0=eq1, in1=rk[:], op=ALU.mult),
                              lambda: nc.vector.reduce_sum(out=sfl[:, 0:1], in_=tmp8[:], axis=AX),
                              lambda: nc.vector.tensor_tensor(out=tmp8[:], in0=eq2, in1=rk[:], op=ALU.mult),
                              lambda: nc.vector.reduce_sum(out=sfl[:, 1:2], in_=tmp8[:], axis=AX),
                              lambda: nc.vector.tensor_tensor(out=tmp8[:], in0=eq1, in1=G, op=ALU.mult),
                              lambda: nc.vector.reduce_sum(out=gsel[:, tt, 0:1], in_=tmp8[:], axis=AX),
                              lambda: nc.vector.tensor_tensor(out=tmp8[:], in0=eq2, in1=G, op=ALU.mult),
                              lambda: nc.vector.reduce_sum(out=gsel[:, tt, 1:2], in_=tmp8[:], axis=AX)):
                        k.op(DVE, f, reads=[b_gl, b_PS[1], b_const], writes=[b_gl, b_gT])
                    k.op(DVE, lambda: nc.vector.tensor_copy(out=idxs[:, tt, :], in_=sfl[:]), reads=[b_gl], writes=[b_idx])
                    k.op(ACT, lambda: nc.scalar.copy(out=x1bf[i][:], in_=x1[i][:]), reads=[b_x1[i]], writes=[b_x1bf[i]])
                    k.dma(POOL, [(lambda kk_=kk_: nc.gpsimd.indirect_dma_start(
                        out=xg_d.ap(), out_offset=bass.IndirectOffsetOnAxis(idxs[:, tt, kk_:kk_ + 1], 0),
                        in_=x1bf[i][:], in_offset=None)) for kk_ in range(2)],
                          b_x1bf[i], reads=[b_x1bf[i], b_idx])
            if is_moe:
                k.op(DVE, lambda: nc.vector.tensor_copy(out=cnti[:], in_=carry[:]), reads=[b_gT, b_gl], writes=[b_gT])
                k.dma(SP, [(cnt_d.ap(), cnti[0:1, :])], b_gT, reads=[b_gT])
                if dbg:
                    k.dma(SP, [(idx_dbg.ap(), idxs[:]), (gsel_dbg.ap(), gsel[:])], b_gT, reads=[b_gT, b_idx])
            k.barrier()
        if stop == "O" and l == nlayers - 1:
            break

        def ffn_unit(xb, b_xb, hT, b_hT, WG, WU, WD, b_WG, b_WU, b_WD, sgt, b_sgt, PS, b_PS, wcnt, pcnt,
                     wg_src, wu_src, wd_src, down_evac, po_banks):
            for fg in range(NFB // 2):
                wi = wcnt[0] % 2
                wcnt[0] += 1
                k.dma(POOL, [(WG[wi][:], wg_src[:, :, fg * 256:(fg + 1) * 256])], b_WG[wi], writes=[b_WG[wi]])
                k.dma(POOL, [(WU[wi][:], wu_src[:, :, fg * 256:(fg + 1) * 256])], b_WU[wi], writes=[b_WU[wi]])
                for j in range(2):
                    fbk = fg * 2 + j
                    pg = 2 + (pcnt[0] * 2) % 6 if len(po_banks) == 2 and po_banks[0] < 2 else (pcnt[0] * 2) % 6
                    pu = pg + 1
                    pcnt[0] += 1
                    k.mm([(lambda c=c: nc.tensor.matmul(PS[pg][:], lhsT=WG[wi][:, c, j * 128:(j + 1) * 128], rhs=xb[:, c, :],
                                                        start=(c == 0), stop=(c == 15))) for c in range(16)],
                         reads=[b_WG[wi], b_xb], writes=[b_PS[pg]])
                    k.mm([(lambda c=c: nc.tensor.matmul(PS[pu][:], lhsT=WU[wi][:, c, j * 128:(j + 1) * 128], rhs=xb[:, c, :],
                                                        start=(c == 0), stop=(c == 15))) for c in range(16)],
                         reads=[b_WU[wi], b_xb], writes=[b_PS[pu]])
                    si = fbk % 2
                    k.op(ACT, lambda: nc.scalar.activation(out=sgt[si][:], in_=PS[pg][:], func=AF.Silu), reads=[b_PS[pg]], writes=[b_sgt[si]])
                    k.op(DVE, lambda: nc.vector.tensor_tensor(out=hT[:, fbk, :], in0=sgt[si][:], in1=PS[pu][:], op=ALU.mult),
                         reads=[b_sgt[si], b_PS[pu]], writes=[b_hT[fbk]])
            for dc in range(16):
                wis = []
                for hf in range(2):
                    wi = wcnt[1] % 3
                    wcnt[1] += 1
                    wis.append(wi)
                    k.dma(POOL, [(WD[wi][:], wd_src[:, hf * 22:(hf + 1) * 22, dc * 128:(dc + 1) * 128])], b_WD[wi], writes=[b_WD[wi]])
                po = po_banks[dc % len(po_banks)]
                k.mm([(lambda f=f: nc.tensor.matmul(PS[po][:], lhsT=WD[wis[f // 22]][:, f % 22, :], rhs=hT[:, f, :],
                                                    start=(f == 0), stop=(f == NFB - 1)))
                      for f in range(NFB)], reads=[b_WD[wis[0]], b_WD[wis[1]]] + b_hT, writes=[b_PS[po]])
                down_evac(dc, PS[po], b_PS[po])

        if not is_moe:
            with ExitStack() as st:
                xb = sb("xb", [128, 16, 512], BF16, st)
                hT = sb("hT", [128, NFB, 512], BF16, st)
                acc = sb("acc", [128, 16, 512], F32, st)
                WG = [sb(f"wg{i}", [128, 16, 256], BF16, st) for i in range(2)]
                WU = [sb(f"wu{i}", [128, 16, 256], BF16, st) for i in range(2)]
                WD = [sb(f"wd{i}", [128, NFB // 2, 128], BF16, st) for i in range(3)]
                sgt = [sb(f"sgt{i}", [128, 512], BF16, st) for i in range(2)]
                gb = sb("gb2", [128, D], F32, st)
                bb = sb("bb2", [128, D], F32, st)
                xr = sb("xr2", [128, D], F32, st)
                rr = sb("rr2", [128, D], F32, st)
                stats = sb("stats2", [128, 4, 6], F32, st)
                mv = sb("mv2", [128, 2], F32, st)
                sc2 = sb("sc22", [128, 2], F32, st)
                b_xb, b_hT, b_acc = Buf(), [Buf() for _ in range(NFB)], [Buf() for _ in range(16)]
                b_WG, b_WU, b_WD, b_sgt = [Buf(), Buf()], [Buf(), Buf()], [Buf(), Buf(), Buf()], [Buf(), Buf()]
                b_gb, b_xr, b_rr, b_small = Buf(), Buf(), Buf(), Buf()
                PS = [ps(f"psC{i}", [128, 512], F32, st) for i in range(8)]
                b_PS = [Buf() for _ in range(8)]
                k.dma(SP, [(gb[:], bcast_row(ln_ffn_g, l * D, D)), (bb[:], bcast_row(ln_ffn_b, l * D, D))], b_gb, writes=[b_gb])
                x1Tsrc = x1T_d.ap().rearrange("(c p) t -> p c t", p=128)
                wcnt = [0, 0]
                pcnt = [0]
                wg_src = dense_w_gate.ap()[0].rearrange("(c p) f -> p c f", p=128)
                wu_src = dense_w_up.ap()[0].rearrange("(c p) f -> p c f", p=128)
                wd_src = dense_w_down.ap()[0].rearrange("(c p) n -> p c n", p=128)
                for tb in range(4):
                    k.dma(SP, [(xb[:], x1Tsrc[:, :, tb * 512:(tb + 1) * 512])], b_xb, writes=[b_xb])

                    def evac_dense(dc, p, b_p):
                        k.op(ACT, lambda: nc.scalar.copy(out=acc[:, dc, :], in_=p[:]), reads=[b_p], writes=[b_acc[dc]])
                    ffn_unit(xb, b_xb, hT, b_hT, WG, WU, WD, b_WG, b_WU, b_WD, sgt, b_sgt, PS, b_PS, wcnt, pcnt,
                             wg_src, wu_src, wd_src, evac_dense, [6, 7])
                    for t4 in range(4):
                        tt = tb * 4 + t4
                        ts_ = slice(tt * 128, (tt + 1) * 128)
                        k.dma(SP, [(xr[:], x1_d.ap()[ts_, :])], b_xr, writes=[b_xr])
                        for cg in range(4):
                            pb = cg
                            k.mm([(lambda c=c: nc.tensor.transpose(PS[pb][:, (c % 4) * 128:(c % 4 + 1) * 128],
                                                                   acc[:, c, t4 * 128:(t4 + 1) * 128], identf[:]))
                                  for c in range(cg * 4, cg * 4 + 4)], reads=b_acc[cg * 4:cg * 4 + 4] + [b_const], writes=[b_PS[pb]])
                            k.op(DVE, lambda: nc.vector.scalar_tensor_tensor(out=rr[:, cg * 512:(cg + 1) * 512], in0=xr[:, cg * 512:(cg + 1) * 512],
                                                                             scalar=float(ALPHA), in1=PS[pb][:], op0=ALU.mult, op1=ALU.add),
                                 reads=[b_xr, b_PS[pb]], writes=[b_rr])
                        layer_norm_tile("ffn", rr, b_rr, stats, mv, sc2, b_small, gb, bb, b_gb, rr[:], b_rr)
                        k.dma(SP, [(x_dst.ap()[ts_, :], rr[:])], b_rr, reads=[b_rr])
                k.barrier()
        else:
            with ExitStack() as st:
                xgr = sb("xgr", [128, 4, D], BF16, st)
                xb = sb("xbm", [128, 16, 512], BF16, st)
                hT = sb("hTm", [128, NFB, 512], BF16, st)
                WG = [sb(f"wgm{i}", [128, 16, 256], BF16, st) for i in range(2)]
                WU = [sb(f"wum{i}", [128, 16, 256], BF16, st) for i in range(2)]
                WD = [sb(f"wdm{i}", [128, NFB // 2, 128], BF16, st) for i in range(3)]
                sgt = [sb(f"sgtm{i}", [128, 512], BF16, st) for i in range(2)]
                ysb = [sb(f"ysb{i}", [128, 512], F32, st) for i in range(2)]
                yes = sb("yes", [128, 4, D], F32, st)
                PS = [ps(f"psM{i}", [128, 512], F32, st) for i in range(8)]
                dscr = sb("dscr", [1, NE], mybir.dt.int32, st)
                cregs = [nc.alloc_registers(f"cnt{l}_{e}", mybir.ALL_ENGINES) for e in range(NE)]
                for e in range(NE):
                    for r in cregs[e]:
                        nc.reg_load(r, cnt_d.ap()[0:1, e:e + 1])
                k.barrier()
                k.reset()
                for e in range(NE):
                    wg_src = moe_w_gate.ap()[0, e].rearrange("(c p) f -> p c f", p=128)
                    wu_src = moe_w_up.ap()[0, e].rearrange("(c p) f -> p c f", p=128)
                    wd_src = moe_w_down.ap()[0, e].rearrange("(c p) n -> p c n", p=128)
                    moe_mode = os.environ.get("MOE_MODE", "if")
                    for j in range(4 if moe_mode == "if" else (0 if moe_mode == "none" else 1)):
                        snap_ = k.snapshot()
                        with (nc.If_cmp(cregs[e], 512 * j, "IS_GT") if moe_mode == "if" else ExitStack()):
                            b_xgr, b_xb, b_hT = Buf(), Buf(), [Buf() for _ in range(NFB)]
                            b_WG, b_WU, b_WD, b_sgt = [Buf(), Buf()], [Buf(), Buf()], [Buf(), Buf(), Buf()], [Buf(), Buf()]
                            b_ysb, b_yes = [Buf(), Buf()], Buf()
                            b_PS = [Buf() for _ in range(8)]
                            base = e * S + 512 * j
                            k.dma(SP, [(xgr[:], xg_d.ap()[base:base + 512, :].rearrange("(s p) d -> p s d", p=128))], b_xgr, writes=[b_xgr])
                            for cp in range(8):
                                pb = cp % 8
                                pv = PS[pb][:].bitcast(BF16)
                                fns = []
                                for cc in range(2):
                                    for s_ in range(4):
                                        fns.append(lambda cc=cc, s_=s_: nc.tensor.transpose(
                                            pv[:, cc * 512 + s_ * 128:cc * 512 + (s_ + 1) * 128],
                                            xgr[:, s_, (cp * 2 + cc) * 128:(cp * 2 + cc + 1) * 128], identb[:]))
                                k.mm(fns, reads=[b_xgr, b_const], writes=[b_PS[pb]])
                                srcv = pv[:, 0:1024].rearrange("p (c t) -> p c t", c=2)
                                if cp % 2 == 0:
                                    k.op(ACT, lambda: nc.scalar.copy(out=xb[:, cp * 2:cp * 2 + 2, :], in_=srcv), reads=[b_PS[pb]], writes=[b_xb])
                                else:
                                    k.op(DVE, lambda: nc.vector.tensor_copy(out=xb[:, cp * 2:cp * 2 + 2, :], in_=srcv), reads=[b_PS[pb]], writes=[b_xb])

                            def evac_moe(dc, p, b_p):
                                si = dc % 2
                                pt = 2 + dc % 2
                                k.op(ACT, lambda: nc.scalar.copy(out=ysb[si][:], in_=p[:]), reads=[b_p], writes=[b_ysb[si]])
                                k.mm([(lambda s_=s_: nc.tensor.transpose(PS[pt][:, s_ * 128:(s_ + 1) * 128], ysb[si][:, s_ * 128:(s_ + 1) * 128], identf[:]))
                                      for s_ in range(4)], reads=[b_ysb[si], b_const], writes=[b_PS[pt]])
                                k.op(DVE, lambda: nc.vector.tensor_copy(out=yes[:, :, dc * 128:(dc + 1) * 128],
                                                                        in_=PS[pt][:].rearrange("p (s c) -> p s c", s=4)),
                                     reads=[b_PS[pt]], writes=[b_yes])
                            ffn_unit(xb, b_xb, hT, b_hT, WG, WU, WD, b_WG, b_WU, b_WD, sgt, b_sgt, PS, b_PS, [0, 0], [0],
                                     wg_src, wu_src, wd_src, evac_moe, [0, 1])
                            k.dma(SP, [(ye_d.ap()[base:base + 512, :].rearrange("(s p) d -> p s d", p=128), yes[:])], b_yes, reads=[b_yes])
                            k.barrier()
                        if moe_mode == "if":
                            with nc.Else():
                                k.compensate(snap_, dscr[0:1, :], cnt_d.ap())
            with ExitStack() as st:
                gb = sb("gb3", [128, D], F32, st)
                bb = sb("bb3", [128, D], F32, st)
                r1 = [sb(f"r1_{i}", [128, D], F32, st) for i in range(2)]
                r2 = [sb(f"r2_{i}", [128, D], F32, st) for i in range(2)]
                xr = [sb(f"xr3_{i}", [128, D], F32, st) for i in range(2)]
                stats = sb("stats3", [128, 4, 6], F32, st)
                mv = sb("mv3", [128, 2], F32, st)
                sc2 = sb("sc23", [128, 2], F32, st)
                b_gb, b_small = Buf(), Buf()
                b_r1, b_r2, b_xr = [Buf(), Buf()], [Buf(), Buf()], [Buf(), Buf()]
                k.dma(SP, [(gb[:], bcast_row(ln_ffn_g, l * D, D)), (bb[:], bcast_row(ln_ffn_b, l * D, D))], b_gb, writes=[b_gb])
                for tt in range(16):
                    i = tt % 2
                    ts_ = slice(tt * 128, (tt + 1) * 128)
                    k.dma(POOL, [lambda: nc.gpsimd.indirect_dma_start(out=r1[i][:], out_offset=None, in_=ye_d.ap(),
                                                                      in_offset=bass.IndirectOffsetOnAxis(idxs[:, tt, 0:1], 0))],
                          b_r1[i], writes=[b_r1[i]])
                    k.dma(POOL, [lambda: nc.gpsimd.indirect_dma_start(out=r2[i][:], out_offset=None, in_=ye_d.ap(),
                                                                      in_offset=bass.IndirectOffsetOnAxis(idxs[:, tt, 1:2], 0))],
                          b_r2[i], writes=[b_r2[i]])
                    k.dma(SP, [(xr[i][:], x1_d.ap()[ts_, :])], b_xr[i], writes=[b_xr[i]])
                    k.op(DVE, lambda: nc.vector.tensor_scalar(out=r1[i][:], in0=r1[i][:], scalar1=gsel[:, tt, 0:1], scalar2=None, op0=ALU.mult),
                         reads=[], writes=[b_r1[i]])
                    k.op(DVE, lambda: nc.vector.scalar_tensor_tensor(out=r1[i][:], in0=r2[i][:], scalar=gsel[:, tt, 1:2], in1=r1[i][:],
                                                                     op0=ALU.mult, op1=ALU.add), reads=[b_r2[i]], writes=[b_r1[i]])
                    k.op(DVE, lambda: nc.vector.scalar_tensor_tensor(out=r1[i][:], in0=xr[i][:], scalar=float(ALPHA), in1=r1[i][:],
                                                                     op0=ALU.mult, op1=ALU.add), reads=[b_xr[i]], writes=[b_r1[i]])
                    layer_norm_tile("ffn", r1[i], b_r1[i], stats, mv, sc2, b_small, gb, bb, b_gb, r1[i][:], b_r1[i])
                    k.dma(SP, [(x_dst.ap()[ts_, :], r1[i][:])], b_r1[i], reads=[b_r1[i]])
                k.barrier()

    k.barrier()
    stack.close()
    return nc, consts


_CACHE = {}


def kernel(**inputs):
    n = 8
    if "prog" not in _CACHE:
        _CACHE["prog"] = build_program()
    nc, consts = _CACHE["prog"]
    shared = {kk: np.ascontiguousarray(v) for kk, v in inputs.items() if kk != "x"}
    for kk, v in consts.items():
        if kk == "c_gchunk":
            continue
        shared[kk] = v
    x = np.ascontiguousarray(inputs["x"])
    in_maps = []
    for b in range(n):
        m = dict(shared)
        m["x"] = x[b]
        in_maps.append(m)
    res = run_bass_kernel_spmd(nc, in_maps, core_ids=list(range(n)))
    return np.stack([np.asarray(r["y"]) for r in res.results], axis=0).astype(np.float32)
```

```python
import os
import math
from contextlib import ExitStack
import numpy as np
import ml_dtypes
import concourse.bass as bass
import concourse.mybir as mybir
from concourse.bass_utils import run_bass_kernel_spmd

F32 = mybir.dt.float32
BF16 = mybir.dt.bfloat16
ALU = mybir.AluOpType
AF = mybir.ActivationFunctionType

S = 2048
D = 2048
DEPTH = 2
PROJ = 4992
DFF = 5632
NFB = DFF // 128
NE = 8
ALPHA = (2.0 * DEPTH) ** 0.25
EPS = 1e-5
NEGM = -30000.0
O_AQ, O_AK, O_AV, O_BQ, O_BK, O_BV, O_CQ, O_CK, O_CV, O_CG = 0, 768, 1536, 2304, 3072, 3264, 3456, 3712, 3968, 4480


def lambda_init(l):
    return 0.8 - 0.6 * math.exp(-0.3 * l)


class Sem:
    def __init__(self, h, idx):
        self.h = h
        self.idx = idx
        self.total = 0


class Buf:
    __slots__ = ("name", "w", "r", "sem")

    def __init__(self, name=""):
        self.name = name
        self.w = None
        self.r = {}
        self.sem = None


class Eng:
    def __init__(self, name, eng, sem, is_pe=False):
        self.name = name
        self.eng = eng
        self.sem = sem
        self.seen = {}
        self.is_pe = is_pe

    def wait(self, tok):
        if tok is None:
            return
        s, v, ep = tok
        if ep != EPOCH[0]:
            return
        if self.is_pe and s is self.sem:
            return
        if self.seen.get(s.idx, 0) >= v:
            return
        self.eng.wait_ge(s.h, v)
        self.seen[s.idx] = v


EPOCH = [0]


class K:
    def __init__(self, nc, stack):
        EPOCH[0] = 0
        self.nc = nc
        self.stack = stack
        self.nsem = 0
        self.all_sems = []
        self.PE = Eng("pe", nc.tensor, self.new_sem("pe"), is_pe=True)
        self.ACT = Eng("act", nc.scalar, self.new_sem("act"))
        self.DVE = Eng("dve", nc.vector, self.new_sem("dve"))
        self.POOL = Eng("pool", nc.gpsimd, self.new_sem("pool"))
        self.SP = Eng("sp", nc.sync, self.new_sem("sp"))
        self.engs = [self.PE, self.ACT, self.DVE, self.POOL, self.SP]
        self.free_dma = []
        self.stage_bufs = []

    def new_sem(self, name):
        h = self.stack.enter_context(self.nc.semaphore(f"s_{name}_{self.nsem}"))
        s = Sem(h, self.nsem)
        self.nsem += 1
        self.all_sems.append(s)
        return s

    def _deps(self, E, reads, writes):
        for b in reads:
            E.wait(b.w)
        for b in writes:
            E.wait(b.w)
            for t in b.r.values():
                E.wait(t)

    def _mark(self, tok, reads, writes):
        s = tok[0]
        for b in reads:
            b.r[s.idx] = tok
        for b in writes:
            b.w = tok
            b.r = {}

    def op(self, E, fn, reads=(), writes=()):
        self._deps(E, reads, writes)
        inst = fn()
        E.sem.total += 1
        inst.then_inc(E.sem.h, 1)
        tok = (E.sem, E.sem.total, EPOCH[0])
        self._mark(tok, reads, writes)
        return tok

    def mm(self, fns, reads=(), writes=()):
        E = self.PE
        self._deps(E, reads, writes)
        inst = None
        for f in fns:
            inst = f()
        E.sem.total += 1
        inst.then_inc(E.sem.h, 1)
        tok = (E.sem, E.sem.total, EPOCH[0])
        self._mark(tok, reads, writes)
        return tok

    def dma(self, Q, pairs, sem_buf, reads=(), writes=()):
        self._deps(Q, reads, writes)
        if sem_buf.sem is None:
            if self.free_dma:
                sem_buf.sem = self.free_dma.pop()
            else:
                sem_buf.sem = self.new_sem("dma")
            self.stage_bufs.append(sem_buf)
        s = sem_buf.sem
        s.queue = Q
        for pr in pairs:
            if callable(pr):
                inst = pr()
            else:
                inst = Q.eng.dma_start(out=pr[0], in_=pr[1])
            s.total += 16
            inst.then_inc(s.h, 16)
        tok = (s, s.total, EPOCH[0])
        self._mark(tok, reads, writes)
        return tok

    def snapshot(self):
        return {s_.idx: s_.total for s_ in self.all_sems}

    def compensate(self, snap, dummy_out, dummy_in):
        eng_of = {E.sem.idx: E for E in self.engs}
        for s_ in self.all_sems:
            d = s_.total - snap.get(s_.idx, 0)
            if d <= 0:
                continue
            if s_.idx in eng_of:
                eng_of[s_.idx].eng.sem_inc(s_.h, d)
            else:
                s_.queue.eng.dma_start(out=dummy_out, in_=dummy_in).then_inc(s_.h, d)

    def reset(self):
        return

    def _reset(self):
        if not hasattr(self, "bp"):
            self.bp = [(self.new_sem("hbB"), self.new_sem("hbC")) for _ in range(3)]
            self.bp_ids = {x.idx for p in self.bp for x in p}
            self.hbn = 0
        pair = self.bp[self.hbn % 3]
        nxt = self.bp[(self.hbn + 1) % 3]
        self.hbn += 1
        for E in self.engs:
            E.eng.sem_inc(pair[0].h, 1)
        sp = self.SP.eng
        sp.wait_ge(pair[0].h, len(self.engs))
        for s_ in self.all_sems:
            if s_.idx in self.bp_ids:
                continue
            sp.sem_clear(s_.h)
            s_.total = 0
        sp.sem_clear(nxt[0].h)
        sp.sem_clear(nxt[1].h)
        sp.sem_inc(pair[1].h, 1)
        for E in self.engs:
            E.eng.wait_ge(pair[1].h, 1)
            E.seen = {}
        EPOCH[0] += 1

    def barrier(self):
        for E in self.engs:
            for s in self.all_sems:
                if s.total > 0:
                    E.wait((s, s.total, EPOCH[0]))
        for b in self.stage_bufs:
            self.free_dma.append(b.sem)
            b.sem = None
        self.stage_bufs = []


def _rel_bucket_np(dist):
    d = np.maximum(dist, 0)
    ratio = np.maximum(d, 1).astype(np.float32) / np.float32(16)
    large = 16 + (np.log(ratio).astype(np.float32) / np.float32(math.log(128 / 16)) * np.float32(16)).astype(np.int32)
    large = np.minimum(large, 31)
    return np.where(d < 16, d, large)


def make_consts():
    bf = ml_dtypes.bfloat16
    c = {}
    c["c_identf"] = np.eye(128, dtype=np.float32)
    c["c_identb"] = np.eye(128, dtype=np.float32).astype(bf)
    ob = np.zeros((128, 3, 128), np.float32)
    ob[:, 0, :] = 1.0
    ob[:, 1, :64] = 1.0
    ob[:, 2, 64:] = 1.0
    c["c_onesb"] = ob.astype(bf)
    R = np.zeros((64, 64), np.float32)
    for i in range(32):
        R[2 * i + 1, 2 * i] = -1.0
        R[2 * i, 2 * i + 1] = 1.0
    R128 = np.zeros((128, 128), np.float32)
    R128[:64, :64] = R
    R128[64:, 64:] = R
    c["c_rot"] = R128.astype(bf)
    kk = np.arange(128)[:, None]
    jj = np.arange(256)[None, :]
    mk = np.zeros((128, 2, 256), np.float32)
    mk[:, 0, :] = np.where(jj >= kk, 0.0, NEGM)
    mk[:, 1, :] = np.where((jj - kk >= 0) & (jj - kk < 128), 0.0, NEGM)
    c["c_mask"] = mk
    m = np.arange(384)
    dist = m - 127
    bk = _rel_bucket_np(dist)
    oh = np.zeros((32, 384), np.float32)
    for i in range(384):
        if dist[i] >= 0:
            oh[bk[i], i] = 1.0
    c["c_ohb"] = oh
    hh = np.arange(4, dtype=np.float32)
    log_g = np.log(1.0 - np.exp2(-5.0 - hh)).astype(np.float64)
    pos = np.arange(128)
    dec = np.zeros((128, 4, 128), np.float32)
    for h in range(4):
        rel = pos[None, :] - pos[:, None]
        dec[:, h, :] = np.where(rel >= 0, np.exp(np.maximum(rel, 0) * log_g[h]), 0.0)
    c["c_decay"] = dec
    zeta = np.exp((127 - pos)[:, None] * log_g[None, :]).astype(np.float32)
    c["c_zeta"] = zeta
    xi = np.exp((pos + 1)[:, None] * log_g[None, :])
    ang = np.repeat(1.0 / (10000.0 ** np.linspace(0.0, 1.0, 32, dtype=np.float32)), 2).astype(np.float32)
    ang = np.arange(S, dtype=np.float32)[:, None] * ang[None, :]
    sin, cos = np.sin(ang).astype(np.float32), np.cos(ang).astype(np.float32)
    tab = np.zeros((128, 6, S), np.float32)
    for p in range(128):
        d = p % 64
        tab[p, 0, :] = cos[:, d]
        tab[p, 1, :] = sin[:, d]
        for blk in range(2):
            h = 2 * blk + p // 64
            xs = xi[np.arange(S) % 128, h]
            tab[p, 2 + 2 * blk, :] = cos[:, d] * xs
            tab[p, 3 + 2 * blk, :] = sin[:, d] * xs
    c["c_tab"] = tab
    c["c_gchunk"] = np.exp(128 * log_g).astype(np.float64)
    tt_ = np.arange(128)
    c["c_ust"] = (tt_[:, None] < tt_[None, :]).astype(np.float32).astype(bf)
    c["c_ebase"] = np.broadcast_to((np.arange(8, dtype=np.float32) * 2560.0)[None, :], (128, 8)).copy()
    return c


def build_program(dbg=False, nlayers=DEPTH, stop=None):
    nc = bass.Bass("TRN2", target_bir_lowering=False)
    need_dense = (stop is None) or nlayers > 1
    need_moe = (stop is None and nlayers > 1)
    need_router = nlayers > 1
    consts = make_consts()
    gchunk = consts["c_gchunk"]

    def din(name, shape, dt=F32):
        return nc.dram_tensor(name, list(shape), dt, kind="ExternalInput")

    x_in = din("x", [S, D])
    w_in = din("w_in", [DEPTH, D, PROJ])
    rel_bias = din("rel_bias", [32, 18])
    a_lambda = din("a_lambda", [DEPTH, 4, 64])
    a_subln_g = din("a_subln_g", [DEPTH, 128])
    b_sinks = din("b_sinks", [DEPTH, 12])
    w_out = din("w_out", [DEPTH, D, D])
    ln_mix_g = din("ln_mix_g", [DEPTH, D])
    ln_mix_b = din("ln_mix_b", [DEPTH, D])
    ln_ffn_g = din("ln_ffn_g", [DEPTH, D])
    ln_ffn_b = din("ln_ffn_b", [DEPTH, D])
    if need_dense:
        dense_w_gate = din("dense_w_gate", [1, D, DFF])
        dense_w_up = din("dense_w_up", [1, D, DFF])
        dense_w_down = din("dense_w_down", [1, DFF, D])
    if need_router:
        moe_router = din("moe_router", [1, D, NE])
    if need_moe:
        moe_w_gate = din("moe_w_gate", [1, NE, D, DFF])
        moe_w_up = din("moe_w_up", [1, NE, D, DFF])
        moe_w_down = din("moe_w_down", [1, NE, DFF, D])
    c_identf = din("c_identf", [128, 128])
    c_identb = din("c_identb", [128, 128], BF16)
    c_onesb = din("c_onesb", [128, 3, 128], BF16)
    c_rot = din("c_rot", [128, 128], BF16)
    c_mask = din("c_mask", [128, 2, 256])
    c_ohb = din("c_ohb", [32, 384])
    c_decay = din("c_decay", [128, 4, 128])
    c_zeta = din("c_zeta", [128, 4])
    c_tab = din("c_tab", [128, 6, S])
    c_ust = din("c_ust", [128, 128], BF16)
    c_ebase = din("c_ebase", [128, 8])

    skind = "ExternalOutput" if dbg else "Internal"
    y_out = nc.dram_tensor("y", [S, D], F32, kind="ExternalOutput")
    yT_d = nc.dram_tensor("yT_d", [D, S], BF16, kind=skind)
    x1_d = nc.dram_tensor("x1_d", [S, D], F32, kind=skind)
    x1T_d = nc.dram_tensor("x1T_d", [D, S], BF16, kind=skind)
    x2_d = nc.dram_tensor("x2_d", [S, D], F32, kind=skind)
    z_d = nc.dram_tensor("z_d", [18, 128, 384], F32, kind=skind)
    xg_d = nc.dram_tensor("xg_d", [NE * 2560, D], BF16, kind="Internal")
    ye_d = nc.dram_tensor("ye_d", [NE * 2560, D], F32, kind="Internal")
    cnt_d = nc.dram_tensor("cnt_d", [1, NE], mybir.dt.int32, kind=skind)
    if dbg:
        idx_dbg = nc.dram_tensor("idx_dbg", [128, 16, 2], mybir.dt.int32, kind="ExternalOutput")
        gsel_dbg = nc.dram_tensor("gsel_dbg", [128, 16, 2], F32, kind="ExternalOutput")

    stack = ExitStack()
    stack.enter_context(nc.allow_low_precision("bf16 matmul operands with fp32 accumulation"))
    stack.enter_context(nc.allow_non_contiguous_dma(reason="tiny parameter loads"))
    k = K(nc, stack)
    PE, ACT, DVE, POOL, SP = k.PE, k.ACT, k.DVE, k.POOL, k.SP

    uid = [0]

    def sb(name, shape, dt, st=None):
        uid[0] += 1
        return (st or stack).enter_context(nc.sbuf_tensor(f"{name}_{uid[0]}", list(shape), dt))

    def ps(name, shape, dt, st):
        uid[0] += 1
        return st.enter_context(nc.psum_tensor(f"{name}_{uid[0]}", list(shape), dt))

    identf = sb("identf", [128, 128], F32)
    identb = sb("identb", [128, 128], BF16)
    onesb = sb("onesb", [128, 3, 128], BF16)
    rotb = sb("rotb", [128, 128], BF16)
    TA = sb("TA", [128, 6, 256], BF16)
    TB = sb("TB", [128, 12, 256], BF16)
    cA = sb("cA", [128, 6], F32)
    ust = sb("ust", [128, 128], BF16)
    ebase = sb("ebase", [128, 8], F32)
    idxs = sb("idxs", [128, 16, 2], mybir.dt.int32)
    gsel = sb("gsel", [128, 16, 2], F32)
    carry = sb("carry", [128, 8], F32)
    b_const = Buf("const")
    b_gT = Buf("gT")
    epsc = sb("epsc", [128, 2], F32)
    k.op(DVE, lambda: nc.vector.memset(epsc[:, 0:1], EPS), writes=[b_const])
    k.op(DVE, lambda: nc.vector.memset(epsc[:, 1:2], 128.0 * EPS), writes=[b_const])

    k.dma(SP, [(identf[:], c_identf.ap()), (identb[:], c_identb.ap()), (onesb[:], c_onesb.ap()),
               (rotb[:], c_rot.ap()), (ust[:], c_ust.ap()), (ebase[:], c_ebase.ap())], b_const, writes=[b_const])

    with ExitStack() as st:
        tabs = sb("tabs", [32, 18], F32, st)
        ohb = sb("ohb", [32, 384], F32, st)
        ones32 = sb("ones32", [32, 128], BF16, st)
        oht = sb("oht", [32, 2, 384], BF16, st)
        frep = sb("frep", [128, 18, 384], F32, st)
        traw = sb("traw", [128, 18, 256], F32, st)
        mask = sb("mask", [128, 2, 256], F32, st)
        pf = [ps(f"pf{i}", [128, 512], F32, st) for i in range(2)]
        b_tabs, b_frep, b_traw, b_mask = Buf(), Buf(), Buf(), Buf()
        b_oht = [Buf(), Buf()]
        b_pf = [Buf(), Buf()]
        b_ones32 = Buf()
        k.dma(SP, [(tabs[:], rel_bias.ap()), (ohb[:], c_ohb.ap())], b_tabs, writes=[b_tabs])
        k.dma(SP, [(mask[:], c_mask.ap())], b_mask, writes=[b_mask])
        k.op(DVE, lambda: nc.vector.memset(ones32[:], 1.0), writes=[b_ones32])
        for h in range(18):
            i = h % 2
            k.op(DVE, lambda: nc.vector.tensor_scalar(out=oht[:, i, :], in0=ohb[:], scalar1=tabs[:, h:h + 1], scalar2=None,
                                                      op0=ALU.mult), reads=[b_tabs], writes=[b_oht[i]])
            k.mm([lambda: nc.tensor.matmul(pf[i][:, 0:384], lhsT=ones32[:], rhs=oht[:, i, :], start=True, stop=True)],
                 reads=[b_oht[i], b_ones32], writes=[b_pf[i]])
            k.op(ACT, lambda: nc.scalar.copy(out=frep[:, h, :], in_=pf[i][:, 0:384]), reads=[b_pf[i]], writes=[b_frep])
        k.dma(SP, [(z_d.ap().rearrange("h k m -> k h m"), frep[:])], b_frep, reads=[b_frep])
        k.barrier()
        skew = bass.AP(z_d, 127, [[383, 128], [128 * 384, 18], [1, 256]])
        k.dma(SP, [(traw[:], skew)], b_traw, writes=[b_traw])
        k.op(DVE, lambda: nc.vector.tensor_copy(out=cA[:], in_=frep[:, 0:6, 382]), reads=[b_frep], writes=[b_const])
        for h in range(6):
            k.op(DVE, lambda: nc.vector.scalar_tensor_tensor(out=TA[:, h, :], in0=traw[:, h, :], scalar=cA[:, h:h + 1],
                                                             in1=mask[:, 0, :], op0=ALU.subtract, op1=ALU.add),
                 reads=[b_traw, b_mask, b_const], writes=[b_const])
        for h in range(12):
            k.op(DVE, lambda: nc.vector.tensor_tensor(out=TB[:, h, :], in0=traw[:, 6 + h, :], in1=mask[:, 1, :], op=ALU.add),
                 reads=[b_traw, b_mask], writes=[b_const])
        k.barrier()

    def layer_norm_tile(st_name, r, b_r, stats, mv, sc2, b_small, gb, bb, b_gb, out_t, b_out):
        for j in range(4):
            k.op(DVE, lambda: nc.vector.bn_stats(out=stats[:, j, :], in_=r[:, j * 512:(j + 1) * 512]),
                 reads=[b_r], writes=[b_small])
        k.op(DVE, lambda: nc.vector.bn_aggr(out=mv[:], in_=stats[:]), reads=[b_small], writes=[b_small])
        k.op(ACT, lambda: nc.scalar.activation(out=sc2[:, 0:1], in_=mv[:, 1:2], func=AF.Ln, bias=epsc[:, 0:1], scale=1.0),
             reads=[b_small, b_const], writes=[b_small])
        k.op(ACT, lambda: nc.scalar.activation(out=sc2[:, 0:1], in_=sc2[:, 0:1], func=AF.Exp, scale=-0.5),
             reads=[], writes=[b_small])
        k.op(DVE, lambda: nc.vector.scalar_tensor_tensor(out=sc2[:, 1:2], in0=mv[:, 0:1], scalar=-1.0, in1=sc2[:, 0:1],
                                                         op0=ALU.mult, op1=ALU.mult), reads=[b_small], writes=[b_small])
        k.op(ACT, lambda: nc.scalar.activation(out=out_t, in_=r[:], func=AF.Identity, bias=sc2[:, 1:2], scale=sc2[:, 0:1]),
             reads=[b_r, b_small], writes=[b_out])
        k.op(DVE, lambda: nc.vector.tensor_tensor(out=out_t, in0=out_t, in1=gb[:], op=ALU.mult),
             reads=[b_gb], writes=[b_out])
        k.op(DVE, lambda: nc.vector.tensor_tensor(out=out_t, in0=out_t, in1=bb[:], op=ALU.add),
             reads=[b_gb], writes=[b_out])

    def bcast_row(handle, off, n):
        return bass.AP(handle, off, [[0, 128], [1, n]])

    for l in range(nlayers):
        x_src = x_in if l == 0 else x2_d
        x_dst = y_out if l == nlayers - 1 else x2_d

        with ExitStack() as st:
            xT = sb("xT", [128, 16, S], BF16, st)
            b_xT = [Buf() for _ in range(16)]
            WB = [sb(f"wb{i}", [128, 16, 256], BF16, st) for i in range(2)]
            b_WB = [Buf(), Buf()]
            wb_i = [0]
            PS = [ps(f"psA{i}", [128, 512], F32, st) for i in range(8)]
            b_PS = [Buf() for _ in range(8)]

            with ExitStack() as st2:
                xin = [sb(f"xin{i}", [128, D], F32, st2) for i in range(2)]
                b_xin = [Buf(), Buf()]
                for tt in range(16):
                    i = tt % 2
                    k.dma(SP, [(xin[i][:], x_src.ap()[tt * 128:(tt + 1) * 128, :])], b_xin[i], writes=[b_xin[i]])
                    for cg in range(4):
                        pb = (tt * 4 + cg) % 8
                        k.mm([(lambda c=c: nc.tensor.transpose(PS[pb][:, (c % 4) * 128:(c % 4 + 1) * 128],
                                                               xin[i][:, c * 128:(c + 1) * 128], identf[:]))
                              for c in range(cg * 4, cg * 4 + 4)],
                             reads=[b_xin[i], b_const], writes=[b_PS[pb]])
                        E = ACT if cg % 2 == 0 else DVE
                        src = PS[pb][:].rearrange("p (c t) -> p c t", c=4)
                        dst = xT[:, cg * 4:cg * 4 + 4, tt * 128:(tt + 1) * 128]
                        if E is ACT:
                            k.op(ACT, lambda: nc.scalar.copy(out=dst, in_=src), reads=[b_PS[pb]], writes=[b_xT[tt]])
                        else:
                            k.op(DVE, lambda: nc.vector.tensor_copy(out=dst, in_=src), reads=[b_PS[pb]], writes=[b_xT[tt]])
                k.barrier()

            ps_rr = [0]

            def next_ps():
                i = ps_rr[0] % 8
                ps_rr[0] += 1
                return i

            def load_w(col_pairs, ncols):
                i = wb_i[0] % 2
                wb_i[0] += 1
                wsrc = w_in.ap()[l].rearrange("(c p) f -> p c f", p=128)
                pairs = [(WB[i][:, :, dc:dc + n], wsrc[:, :, sc:sc + n]) for (dc, sc, n) in col_pairs]
                k.dma(POOL, pairs, b_WB[i], writes=[b_WB[i]])
                return WB[i], b_WB[i]

            def proj_fm(col_pairs, nblk, scale, dsts, b_dsts):
                wt, b_wt = load_w(col_pairs, nblk * 128)
                for j in range(nblk):
                    for tb in range(4):
                        pb = next_ps()
                        k.mm([(lambda c=c: nc.tensor.matmul(PS[pb][:], lhsT=wt[:, c, j * 128:(j + 1) * 128],
                                                            rhs=xT[:, c, tb * 512:(tb + 1) * 512],
                                                            start=(c == 0), stop=(c == 15))) for c in range(16)],
                             reads=[b_wt] + b_xT[tb * 4:tb * 4 + 4], writes=[b_PS[pb]])
                        k.op(ACT, lambda: nc.scalar.activation(out=dsts[j][:, tb * 512:(tb + 1) * 512], in_=PS[pb][:],
                                                               func=AF.Copy, scale=float(scale)),
                             reads=[b_PS[pb]], writes=[b_dsts[j]])

            def proj_tm(col0, ncols, evac):
                wt, b_wt = load_w([(0, col0, ncols)], ncols)
                for tt in range(16):
                    pb = next_ps()
                    k.mm([(lambda c=c: nc.tensor.matmul(PS[pb][:, 0:ncols], lhsT=xT[:, c, tt * 128:(tt + 1) * 128],
                                                        rhs=wt[:, c, 0:ncols], start=(c == 0), stop=(c == 15)))
                          for c in range(16)],
                         reads=[b_wt, b_xT[tt]], writes=[b_PS[pb]])
                    evac(tt, PS[pb], b_PS[pb])

            def proj_fm_cols(col0, nblk, scale, dsts, b_dsts):
                for j0 in range(0, nblk, 2):
                    nb = min(2, nblk - j0)
                    proj_fm([(0, col0 + j0 * 128, nb * 128)], nb, scale, dsts[j0:j0 + nb], b_dsts[j0:j0 + nb])

            def proj_tm_cols(col0, ncols, mk_evac):
                for c0 in range(0, ncols, 256):
                    ncl = min(256, ncols - c0)
                    proj_tm(col0 + c0, ncl, mk_evac(c0, ncl))

            lam3 = sb("lam3", [128, 4, 64], F32, st)
            lamt = sb("lamt", [128, 8], F32, st)
            gsub = sb("gsub", [128, 2], F32, st)
            esink = sb("esink", [128, 6], F32, st)
            b_par = Buf()
            k.dma(SP, [(lam3[:].rearrange("p a b -> p (a b)"), bcast_row(a_lambda, l * 256, 256)),
                       (gsub[:, 0:1], bass.AP(a_subln_g, l * 128, [[1, 128], [1, 1]])),
                       (esink[0:64, :], bass.AP(b_sinks, l * 12, [[0, 64], [2, 6]])),
                       (esink[64:128, :], bass.AP(b_sinks, l * 12 + 1, [[0, 64], [2, 6]]))],
                  b_par, writes=[b_par])
            for a in range(2):
                k.op(DVE, lambda: nc.vector.tensor_tensor(out=lam3[:, 2 * a, :], in0=lam3[:, 2 * a, :], in1=lam3[:, 2 * a + 1, :],
                                                          op=ALU.mult), reads=[b_par], writes=[b_par])
                k.op(DVE, lambda: nc.vector.reduce_sum(out=lamt[:, a:a + 1], in_=lam3[:, 2 * a, :], axis=mybir.AxisListType.X),
                     reads=[b_par], writes=[b_par])
            k.op(ACT, lambda: nc.scalar.activation(out=lamt[:, 2:4], in_=lamt[:, 0:2], func=AF.Exp), reads=[b_par], writes=[b_par])
            k.op(DVE, lambda: nc.vector.tensor_tensor(out=lamt[:, 4:5], in0=lamt[:, 3:4], in1=lamt[:, 2:3], op=ALU.subtract),
                 reads=[b_par], writes=[b_par])
            k.op(DVE, lambda: nc.vector.tensor_scalar(out=lamt[:, 4:5], in0=lamt[:, 4:5], scalar1=-lambda_init(l), scalar2=None,
                                                      op0=ALU.add), reads=[b_par], writes=[b_par])
            k.op(DVE, lambda: nc.vector.tensor_scalar(out=gsub[:, 1:2], in0=gsub[:, 0:1],
                                                      scalar1=float(math.sqrt(128.0) * (1.0 - lambda_init(l))), scalar2=None,
                                                      op0=ALU.mult), reads=[b_par], writes=[b_par])
            k.op(ACT, lambda: nc.scalar.activation(out=esink[:], in_=esink[:], func=AF.Exp), reads=[b_par], writes=[b_par])
            neglam = lamt[:, 4:5]

            with ExitStack() as st2:
                Cqf = [sb(f"Cqf{j}", [128, S], BF16, st2) for j in range(2)]
                Cqx = [sb(f"Cqx{j}", [128, S], BF16, st2) for j in range(2)]
                Ckf = [sb(f"Ckf{j}", [128, S], BF16, st2) for j in range(2)]
                Ckt = sb("Ckt", [128, 16, 256], BF16, st2)
                Cv = sb("Cv", [128, 16, 512], BF16, st2)
                Csg = sb("Csg", [128, 16, 512], BF16, st2)
                decay = sb("decay", [128, 4, 128], F32, st2)
                zeta = sb("zeta", [128, 4], F32, st2)
                b_Cqf, b_Cqx, b_Ckf = [Buf(), Buf()], [Buf(), Buf()], [Buf(), Buf()]
                b_Ckt, b_Cv, b_Csg = Buf(), Buf(), Buf()
                b_cc = Buf()
                k.dma(SP, [(decay[:], c_decay.ap()), (zeta[:], c_zeta.ap())], b_cc, writes=[b_cc])

                def mk_evac_cv(c0, ncl):
                    def ev(tt, p, b_p):
                        k.op(ACT, lambda: nc.scalar.copy(out=Cv[:, tt, c0:c0 + ncl], in_=p[:, 0:ncl]), reads=[b_p], writes=[b_Cv])
                    return ev
                proj_tm_cols(O_CV, 512, mk_evac_cv)

                def mk_evac_cg(c0, ncl):
                    def ev(tt, p, b_p):
                        k.op(ACT, lambda: nc.scalar.activation(out=Csg[:, tt, c0:c0 + ncl], in_=p[:, 0:ncl], func=AF.Silu),
                             reads=[b_p], writes=[b_Csg])
                    return ev
                proj_tm_cols(O_CG, 512, mk_evac_cg)

                with ExitStack() as st3:
                    CqT = [sb(f"CqT{j}", [128, S], BF16, st3) for j in range(2)]
                    CkT = [sb(f"CkT{j}", [128, S], BF16, st3) for j in range(2)]
                    tabt = [sb(f"tabt{i}", [128, 6, 512], F32, st3) for i in range(1)]
                    tmp1 = [sb(f"rtmp{i}", [128, 512], F32, st3) for i in range(2)]
                    b_CqT, b_CkT = [Buf(), Buf()], [Buf(), Buf()]
                    b_tabt = [Buf()]
                    b_tmp1 = [Buf(), Buf()]
                    proj_fm_cols(O_CQ, 2, 1.0, CqT, b_CqT)
                    proj_fm_cols(O_CK, 2, 0.125, CkT, b_CkT)
                    for tb in range(4):
                        ti = 0
                        k.dma(SP, [(tabt[ti][:], c_tab.ap()[:, :, tb * 512:(tb + 1) * 512])], b_tabt[ti], writes=[b_tabt[ti]])
                        for j in range(2):
                            for (src, b_src, outs) in ((CqT[j], b_CqT[j], [(Cqf[j], b_Cqf[j], 0, 1), (Cqx[j], b_Cqx[j], 2 + 2 * j, 3 + 2 * j)]),
                                                       (CkT[j], b_CkT[j], [(Ckf[j], b_Ckf[j], 0, 1)])):
                                pb = next_ps()
                                sl = slice(tb * 512, (tb + 1) * 512)
                                k.mm([lambda: nc.tensor.matmul(PS[pb][:], lhsT=rotb[:], rhs=src[:, sl], start=True, stop=True)],
                                     reads=[b_src, b_const], writes=[b_PS[pb]])
                                for (dst, b_dst, ic, isn) in outs:
                                    t1 = tmp1[0]
                                    t2 = tmp1[1]
                                    k.op(DVE, lambda: nc.vector.tensor_tensor(out=t1[:], in0=src[:, sl], in1=tabt[ti][:, ic, :], op=ALU.mult),
                                         reads=[b_src, b_tabt[ti]], writes=[b_tmp1[0]])
                                    k.op(DVE, lambda: nc.vector.tensor_tensor(out=t2[:], in0=PS[pb][:], in1=tabt[ti][:, isn, :], op=ALU.mult),
                                         reads=[b_PS[pb], b_tabt[ti]], writes=[b_tmp1[1]])
                                    k.op(DVE, lambda: nc.vector.tensor_tensor(out=dst[:, sl], in0=t1[:], in1=t2[:], op=ALU.add),
                                         reads=[b_tmp1[0], b_tmp1[1]], writes=[b_dst])
                    k.barrier()
                PSb = [PS[i][:].bitcast(BF16) if hasattr(PS[i][:], "bitcast") else None for i in range(8)]
                for tt in range(16):
                    pb = next_ps()
                    pview = PSb[pb]
                    k.mm([(lambda j=j: nc.tensor.transpose(pview[:, j * 128:(j + 1) * 128], Ckf[j][:, tt * 128:(tt + 1) * 128], identb[:]))
                          for j in range(2)], reads=[b_Ckf[0], b_Ckf[1], b_const], writes=[b_PS[pb]])
                    for h in range(4):
                        k.op(DVE, lambda: nc.vector.tensor_scalar(out=Ckt[:, tt, h * 64:(h + 1) * 64], in0=pview[:, h * 64:(h + 1) * 64],
                                                                  scalar1=zeta[:, h:h + 1], scalar2=None, op0=ALU.mult),
                             reads=[b_PS[pb], b_cc], writes=[b_Ckt])

                ST = sb("ST", [128, 2, 128], F32, st2)
                STb = [sb(f"STb{i}", [128, 2, 128], BF16, st2) for i in range(2)]
                iT = [sb(f"iT{i}", [128, 128], BF16, st2) for i in range(4)]
                ycm = [sb(f"ycm{i}", [128, 512], BF16, st2) for i in range(2)]
                ycT = [sb(f"ycT{i}", [128, 4, 128], BF16, st2) for i in range(2)]
                cst = sb("cst", [128, 4, 6], F32, st2)
                cmv = sb("cmv", [128, 4, 2], F32, st2)
                crs = sb("crs", [128, 4], F32, st2)
                ctmp = sb("ctmp", [128, 512], F32, st2)
                b_ST, b_STb, b_iT = Buf(), [Buf(), Buf()], [Buf() for _ in range(4)]
                b_ycm, b_ycT, b_cs, b_ctmp = [Buf(), Buf()], [Buf(), Buf()], Buf(), Buf()
                k.op(DVE, lambda: nc.vector.memset(ST[:], 0.0), writes=[b_ST])
                for n in range(16):
                    cs = slice(n * 128, (n + 1) * 128)
                    po = next_ps()
                    for h in range(4):
                        blk, p0 = h // 2, 64 * (h % 2)
                        pi = next_ps()
                        if pi == po:
                            pi = next_ps()
                        k.mm([lambda: nc.tensor.matmul(PS[pi][:, 0:128], lhsT=Ckf[blk][p0:p0 + 64, cs], rhs=Cqf[blk][p0:p0 + 64, cs],
                                                       start=True, stop=True)],
                             reads=[b_Ckf[blk], b_Cqf[blk]], writes=[b_PS[pi]])
                        it = (n * 4 + h) % 4
                        k.op(DVE, lambda: nc.vector.tensor_tensor(out=iT[it][:], in0=PS[pi][:, 0:128], in1=decay[:, h, :], op=ALU.mult),
                             reads=[b_PS[pi], b_cc], writes=[b_iT[it]])
                        fns = [lambda: nc.tensor.matmul(PS[po][:, h * 128:(h + 1) * 128], lhsT=iT[it][:], rhs=Cv[:, n, h * 128:(h + 1) * 128],
                                                        start=True, stop=(n == 0))]
                        rds = [b_iT[it], b_Cv]
                        if n > 0:
                            sbf = STb[n % 2]
                            fns.append(lambda: nc.tensor.matmul(PS[po][:, h * 128:(h + 1) * 128], lhsT=Cqx[blk][p0:p0 + 64, cs],
                                                                rhs=sbf[p0:p0 + 64, blk, :], start=False, stop=True))
                            rds += [b_Cqx[blk], b_STb[n % 2]]
                        k.mm(fns, reads=rds, writes=[b_PS[po]])
                    if n < 15:
                        for blk in range(2):
                            pk = next_ps()
                            if pk == po:
                                pk = next_ps()
                            k.mm([lambda: nc.tensor.matmul(PS[pk][:, 0:256], lhsT=Ckt[:, n, blk * 128:(blk + 1) * 128],
                                                           rhs=Cv[:, n, blk * 256:(blk + 1) * 256], start=True, stop=True)],
                                 reads=[b_Ckt, b_Cv], writes=[b_PS[pk]])
                            for par in range(2):
                                h = 2 * blk + par
                                pr = slice(64 * par, 64 * par + 64)
                                k.op(DVE, lambda: nc.vector.scalar_tensor_tensor(out=ST[pr, blk, :], in0=ST[pr, blk, :],
                                                                                 scalar=float(gchunk[h]),
                                                                                 in1=PS[pk][pr, par * 128:(par + 1) * 128],
                                                                                 op0=ALU.mult, op1=ALU.add),
                                     reads=[b_PS[pk]], writes=[b_ST])
                        nb = (n + 1) % 2
                        k.op(ACT, lambda: nc.scalar.copy(out=STb[nb][:], in_=ST[:]), reads=[b_ST], writes=[b_STb[nb]])
                    yi = n % 2
                    for h in range(4):
                        k.op(DVE, lambda: nc.vector.bn_stats(out=cst[:, h, :], in_=PS[po][:, h * 128:(h + 1) * 128]),
                             reads=[b_PS[po]], writes=[b_cs])
                        k.op(DVE, lambda: nc.vector.bn_aggr(out=cmv[:, h, :], in_=cst[:, h, :]), reads=[b_cs], writes=[b_cs])
                    k.op(ACT, lambda: nc.scalar.activation(out=crs[:], in_=cmv[:, :, 1], func=AF.Ln, bias=epsc[:, 0:1], scale=1.0),
                         reads=[b_cs, b_const], writes=[b_cs])
                    k.op(ACT, lambda: nc.scalar.activation(out=crs[:], in_=crs[:], func=AF.Exp, scale=-0.5),
                         reads=[], writes=[b_cs])
                    for h in range(4):
                        hs = slice(h * 128, (h + 1) * 128)
                        k.op(DVE, lambda: nc.vector.tensor_scalar(out=ctmp[:, hs], in0=PS[po][:, hs], scalar1=cmv[:, h, 0:1],
                                                                  scalar2=crs[:, h:h + 1], op0=ALU.subtract, op1=ALU.mult),
                             reads=[b_PS[po], b_cs], writes=[b_ctmp])
                    k.op(DVE, lambda: nc.vector.tensor_tensor(out=ycm[yi][:], in0=ctmp[:], in1=Csg[:, n, :], op=ALU.mult),
                         reads=[b_ctmp, b_Csg], writes=[b_ycm[yi]])
                    pt = next_ps()
                    pview = PSb[pt]
                    k.mm([(lambda h=h: nc.tensor.transpose(pview[:, h * 128:(h + 1) * 128], ycm[yi][:, h * 128:(h + 1) * 128], identb[:]))
                          for h in range(4)], reads=[b_ycm[yi], b_const], writes=[b_PS[pt]])
                    k.op(ACT, lambda: nc.scalar.copy(out=ycT[yi][:].rearrange("p h q -> p (h q)"), in_=pview[:, 0:512]),
                         reads=[b_PS[pt]], writes=[b_ycT[yi]])
                    k.dma(SP, [(yT_d.ap()[1536:2048, cs].rearrange("(h e) q -> e h q", e=128), ycT[yi][:])], b_ycT[yi],
                          reads=[b_ycT[yi]])
                k.barrier()
            if stop == "C" and l == nlayers - 1:
                break

            with ExitStack() as st2:
                AqT = [sb(f"AqT{j}", [128, S], BF16, st2) for j in range(6)]
                AkT = [sb(f"AkT{j}", [128, S], BF16, st2) for j in range(6)]
                Av = sb("Av", [128, 16, 768], BF16, st2)
                b_AqT = [Buf() for _ in range(6)]
                b_AkT = [Buf() for _ in range(6)]
                b_Av = Buf()
                proj_fm_cols(O_AQ, 6, 0.125, AqT, b_AqT)
                proj_fm_cols(O_AK, 6, 1.0, AkT, b_AkT)

                def mk_evac_av(c0, ncol):
                    def ev(tt, p, b_p):
                        k.op(ACT, lambda: nc.scalar.copy(out=Av[:, tt, c0:c0 + ncol], in_=p[:, 0:ncol]), reads=[b_p], writes=[b_Av])
                    return ev
                proj_tm_cols(O_AV, 768, mk_evac_av)

                PT = [sb(f"PT{i}", [128, 512], BF16, st2) for i in range(4)]
                b_PT = [Buf() for _ in range(4)]
                fa = [sb(f"fa{i}", [128, 512], F32, st2) for i in range(4)]
                b_fa = [Buf() for _ in range(4)]
                sqb = sb("sqb", [128, 512], BF16, st2)
                b_sq = Buf()
                yst = [sb(f"yst{i}", [128, 512], BF16, st2) for i in range(2)]
                b_yst = [Buf(), Buf()]
                step = [0]
                blkc = 0
                for h in range(6):
                    for qb in range(4):
                        nkt = 4 * qb + 4
                        pend = []
                        q0 = qb * 512

                        def emit_pv(item):
                            kt, m, c0, pti = item
                            Ob, Sb = 2 * m, 2 * m + 1
                            k.mm([lambda: nc.tensor.matmul(PS[Ob][:, c0:512], lhsT=Av[:, kt, h * 128:(h + 1) * 128], rhs=PT[pti][:, c0:512],
                                                           start=(kt == 0), stop=(kt == nkt - 1)),
                                  lambda: nc.tensor.matmul(PS[Sb][:, c0:512], lhsT=onesb[:, 0, :], rhs=PT[pti][:, c0:512],
                                                           start=(kt == 0), stop=(kt == nkt - 1))],
                                 reads=[b_Av, b_PT[pti], b_const], writes=[b_PS[Ob], b_PS[Sb]])

                        for kt in range(nkt):
                            j0 = q0 - 128 * kt
                            c0 = max(0, -j0)
                            for m in range(2):
                                sci = 4 + step[0] % 4
                                pti = step[0] % 4
                                step[0] += 1
                                pr = slice(64 * m, 64 * m + 64)
                                fns = [lambda: nc.tensor.matmul(PS[sci][:, c0:512], lhsT=AkT[h][pr, kt * 128:(kt + 1) * 128],
                                                                rhs=AqT[h][pr, q0 + c0:q0 + 512], start=True, stop=(j0 >= 256))]
                                if j0 < 256:
                                    jA = max(j0, 0)
                                    cA_, cB_ = jA - j0, min(512, 256 - j0)
                                    jB = cB_ + j0
                                    fns.append(lambda: nc.tensor.matmul(PS[sci][:, cA_:cB_], lhsT=identb[:], rhs=TA[:, h, jA:jB],
                                                                        start=False, stop=True))
                                k.mm(fns, reads=[b_AkT[h], b_AqT[h], b_const], writes=[b_PS[sci]])
                                k.op(ACT, lambda: nc.scalar.activation(out=PT[pti][:, c0:512], in_=PS[sci][:, c0:512], func=AF.Exp,
                                                                       bias=cA[:, h:h + 1], scale=1.0),
                                     reads=[b_PS[sci], b_const], writes=[b_PT[pti]])
                                pend.append((kt, m, c0, pti))
                                if len(pend) > 2:
                                    emit_pv(pend.pop(0))
                        while pend:
                            emit_pv(pend.pop(0))
                        k.op(DVE, lambda: nc.vector.reciprocal(out=fa[0][:], in_=PS[1][:]), reads=[b_PS[1]], writes=[b_fa[0]])
                        k.op(DVE, lambda: nc.vector.tensor_tensor(out=fa[0][:], in0=fa[0][:], in1=PS[0][:], op=ALU.mult),
                             reads=[b_PS[0]], writes=[b_fa[0]])
                        k.op(DVE, lambda: nc.vector.reciprocal(out=fa[1][:], in_=PS[3][:]), reads=[b_PS[3]], writes=[b_fa[1]])
                        k.op(DVE, lambda: nc.vector.tensor_tensor(out=fa[1][:], in0=fa[1][:], in1=PS[2][:], op=ALU.mult),
                             reads=[b_PS[2]], writes=[b_fa[1]])
                        k.op(DVE, lambda: nc.vector.scalar_tensor_tensor(out=fa[2][:], in0=fa[1][:], scalar=neglam, in1=fa[0][:],
                                                                         op0=ALU.mult, op1=ALU.add),
                             reads=[b_fa[0], b_fa[1], b_par], writes=[b_fa[2]])
                        k.op(DVE, lambda: nc.vector.tensor_tensor(out=sqb[:], in0=fa[2][:], in1=fa[2][:], op=ALU.mult),
                             reads=[b_fa[2]], writes=[b_sq])
                        sci = 4 + step[0] % 4
                        step[0] += 1
                        k.mm([lambda: nc.tensor.matmul(PS[sci][:], lhsT=onesb[:, 0, :], rhs=sqb[:], start=True, stop=True)],
                             reads=[b_sq, b_const], writes=[b_PS[sci]])
                        k.op(ACT, lambda: nc.scalar.activation(out=fa[3][:], in_=PS[sci][:], func=AF.Ln, bias=epsc[:, 1:2], scale=1.0),
                             reads=[b_PS[sci], b_const], writes=[b_fa[3]])
                        k.op(ACT, lambda: nc.scalar.activation(out=fa[3][:], in_=fa[3][:], func=AF.Exp, scale=-0.5),
                             reads=[], writes=[b_fa[3]])
                        yi = blkc % 2
                        blkc += 1
                        k.op(DVE, lambda: nc.vector.scalar_tensor_tensor(out=yst[yi][:], in0=fa[2][:], scalar=gsub[:, 1:2], in1=fa[3][:],
                                                                         op0=ALU.mult, op1=ALU.mult),
                             reads=[b_fa[2], b_fa[3], b_par], writes=[b_yst[yi]])
                        k.dma(SP, [(yT_d.ap()[h * 128:(h + 1) * 128, q0:q0 + 512], yst[yi][:])], b_yst[yi], reads=[b_yst[yi]])
                k.barrier()
            if stop == "A" and l == nlayers - 1:
                break

            with ExitStack() as st2:
                BqT = [sb(f"BqT{j}", [128, S], BF16, st2) for j in range(6)]
                BkT = [sb(f"BkT{j}", [128, S], BF16, st2) for j in range(3)]
                Bvp = sb("Bvp", [128, 16, 3, 2, 128], BF16, st2)
                b_BqT = [Buf() for _ in range(6)]
                b_BkT = [Buf() for _ in range(3)]
                b_Bvp = Buf()
                k.op(DVE, lambda: nc.vector.memset(Bvp[:].rearrange("p a b c d -> p (a b c d)"), 0.0), writes=[b_Bvp])
                proj_fm_cols(O_BQ, 6, 0.125, BqT, b_BqT)
                proj_fm([(0, O_BK, 64), (64, O_BK, 64), (128, O_BK + 64, 64), (192, O_BK + 64, 64)], 2, 1.0, BkT[0:2], b_BkT[0:2])
                proj_fm([(0, O_BK + 128, 64), (64, O_BK + 128, 64)], 1, 1.0, BkT[2:3], b_BkT[2:3])

                def evac_bv(tt, p, b_p):
                    src = p[:, 0:192].rearrange("p (g e) -> p g e", g=3)
                    k.op(ACT, lambda: nc.scalar.copy(out=Bvp[:, tt, :, 0, 0:64], in_=src), reads=[b_p], writes=[b_Bvp])
                    k.op(DVE, lambda: nc.vector.tensor_copy(out=Bvp[:, tt, :, 1, 64:128], in_=src), reads=[b_p], writes=[b_Bvp])
                proj_tm(O_BV, 192, evac_bv)

                PT = [sb(f"PTb{i}", [128, 256], BF16, st2) for i in range(4)]
                b_PT = [Buf() for _ in range(4)]
                fb_ = [sb(f"fb{i}", [128, 512], F32, st2) for i in range(2)]
                b_fb = [Buf(), Buf()]
                yst = [sb(f"ystb{i}", [128, 512], BF16, st2) for i in range(2)]
                b_yst = [Buf(), Buf()]
                step = [0]
                blkc = 0
                for i in range(6):
                    g = i // 2
                    for qb in range(4):
                        q0 = qb * 512
                        kts = list(range(max(0, 4 * qb - 1), 4 * qb + 4))
                        for kt in kts:
                            cs_ = max(0, 128 * kt - q0)
                            ce_ = min(512, 128 * kt + 256 - q0)
                            N = ce_ - cs_
                            jA = q0 + cs_ - 128 * kt
                            for par in range(2):
                                pr = slice(64 * par, 64 * par + 64)
                                sci = 4 + step[0] % 4
                                pti = step[0] % 4
                                step[0] += 1
                                hb = 2 * i + par
                                k.mm([lambda: nc.tensor.matmul(PS[sci][:, 0:N], lhsT=BkT[g][pr, kt * 128:(kt + 1) * 128],
                                                               rhs=BqT[i][pr, q0 + cs_:q0 + ce_], start=True, stop=False),
                                      lambda: nc.tensor.matmul(PS[sci][:, 0:N], lhsT=identb[:], rhs=TB[:, hb, jA:jA + N],
                                                               start=False, stop=True)],
                                     reads=[b_BkT[g], b_BqT[i], b_const], writes=[b_PS[sci]])
                                k.op(ACT, lambda: nc.scalar.activation(out=PT[pti][:, 0:N], in_=PS[sci][:, 0:N], func=AF.Exp),
                                     reads=[b_PS[sci]], writes=[b_PT[pti]])
                                fns = []
                                wr = set()
                                for sub in range(N // 128):
                                    cc = cs_ + sub * 128
                                    qt = (q0 + cc) // 128
                                    qtl = cc // 128
                                    bank = qtl % 2
                                    col = (qtl // 2) * 128
                                    first = (kt == max(0, qt - 1)) and par == 0
                                    last = (kt == qt) and par == 1
                                    fns.append(lambda bank=bank, col=col, sub=sub, first=first, last=last:
                                               nc.tensor.matmul(PS[bank][:, col:col + 128], lhsT=Bvp[:, kt, g, par, :],
                                                                rhs=PT[pti][:, sub * 128:(sub + 1) * 128], start=first, stop=last))
                                    fns.append(lambda bank=bank, col=col, sub=sub, first=first, last=last:
                                               nc.tensor.matmul(PS[2 + bank][:, col:col + 128], lhsT=onesb[:, 1 + par, :],
                                                                rhs=PT[pti][:, sub * 128:(sub + 1) * 128], start=first, stop=last))
                                    wr.add(bank)
                                    wr.add(2 + bank)
                                k.mm(fns, reads=[b_Bvp, b_PT[pti], b_const], writes=[b_PS[w] for w in sorted(wr)])
                        yi = blkc % 2
                        blkc += 1
                        for bank in range(2):
                            dstv = fb_[0][:].rearrange("p (a b c) -> p a b c", a=2, b=2)[:, :, bank, :]
                            k.op(DVE, lambda: nc.vector.tensor_scalar(out=dstv, in0=PS[2 + bank][:, 0:256].rearrange("p (a c) -> p a c", a=2),
                                                                      scalar1=esink[:, i:i + 1], scalar2=None, op0=ALU.add),
                                 reads=[b_PS[2 + bank], b_par], writes=[b_fb[0]])
                        k.op(DVE, lambda: nc.vector.reciprocal(out=fb_[1][:], in_=fb_[0][:]), reads=[b_fb[0]], writes=[b_fb[1]])
                        for bank in range(2):
                            dstv = yst[yi][:].rearrange("p (a b c) -> p a b c", a=2, b=2)[:, :, bank, :]
                            rv = fb_[1][:].rearrange("p (a b c) -> p a b c", a=2, b=2)[:, :, bank, :]
                            k.op(DVE, lambda: nc.vector.tensor_tensor(out=dstv, in0=PS[bank][:, 0:256].rearrange("p (a c) -> p a c", a=2),
                                                                      in1=rv, op=ALU.mult),
                                 reads=[b_PS[bank], b_fb[1]], writes=[b_yst[yi]])
                        k.dma(SP, [(yT_d.ap()[768 + i * 128:768 + (i + 1) * 128, q0:q0 + 512], yst[yi][:])], b_yst[yi],
                              reads=[b_yst[yi]])
                k.barrier()
        if stop == "B" and l == nlayers - 1:
            break

        is_moe = (l % 2 == 1)
        with ExitStack() as st:
            Wo = sb("Wo", [128, 16, D], BF16, st)
            b_Wo = Buf()
            wsrc = w_out.ap()[l].rearrange("(c p) f -> p c f", p=128)
            k.dma(POOL, [(Wo[:, :, j * 512:(j + 1) * 512], wsrc[:, :, j * 512:(j + 1) * 512]) for j in range(4)], b_Wo, writes=[b_Wo])
            gb = sb("gb", [128, D], F32, st)
            bb = sb("bb", [128, D], F32, st)
            b_gb = Buf()
            k.dma(SP, [(gb[:], bcast_row(ln_mix_g, l * D, D)), (bb[:], bcast_row(ln_mix_b, l * D, D))], b_gb, writes=[b_gb])
            yt = [sb(f"yt{i}", [128, 16, 128], BF16, st) for i in range(2)]
            xr = [sb(f"xr{i}", [128, D], F32, st) for i in range(2)]
            rr = [sb(f"rr{i}", [128, D], F32, st) for i in range(2)]
            x1 = [sb(f"x1{i}", [128, D], F32, st) for i in range(2)]
            x1Tb = [sb(f"x1Tb{i}", [128, 16, 128], BF16, st) for i in range(2)]
            stats = sb("stats", [128, 4, 6], F32, st)
            mv = sb("mv", [128, 2], F32, st)
            sc2 = sb("sc2", [128, 2], F32, st)
            b_yt, b_xr, b_rr, b_x1, b_x1Tb = [Buf(), Buf()], [Buf(), Buf()], [Buf(), Buf()], [Buf(), Buf()], [Buf(), Buf()]
            b_small = Buf()
            PS = [ps(f"psB{i}", [128, 512], F32, st) for i in range(8)]
            b_PS = [Buf() for _ in range(8)]
            if is_moe:
                Wr = sb("Wr", [128, 16, NE], F32, st)
                Wrh = sb("Wrh", [128, 16, NE], BF16, st)
                Wrl = sb("Wrl", [128, 16, NE], BF16, st)
                x1Tf = sb("x1Tl", [128, 16, 128], BF16, st)
                gl = sb("gl", [128, 6, NE], F32, st)
                gsm = sb("gsm", [128, 4], F32, st)
                SELb = sb("SELb", [128, NE], BF16, st)
                rk = sb("rk", [128, NE], F32, st)
                tmp8 = sb("tmp8", [128, NE], F32, st)
                sfl = sb("sfl", [128, 2], F32, st)
                cnti = sb("cnti", [128, NE], mybir.dt.int32, st)
                x1bf = [sb(f"x1bf{i}", [128, D], BF16, st) for i in range(2)]
                b_x1bf = [Buf(), Buf()]
                b_idx = Buf()
                b_Wr, b_x1Tf, b_gl = Buf(), Buf(), Buf()
                k.op(DVE, lambda: nc.vector.memset(carry[:], 0.0), writes=[b_gT])
                k.dma(SP, [(Wr[:], moe_router.ap()[0].rearrange("(c p) e -> p c e", p=128))], b_Wr, writes=[b_Wr])
                k.op(DVE, lambda: nc.vector.tensor_copy(out=Wrh[:], in_=Wr[:]), reads=[b_Wr], writes=[b_Wr])
                k.op(DVE, lambda: nc.vector.tensor_tensor(out=Wrl[:], in0=Wr[:], in1=Wrh[:], op=ALU.subtract), reads=[b_Wr], writes=[b_Wr])
            ysrc = yT_d.ap().rearrange("(c p) t -> p c t", p=128)
            x1Tdst = x1T_d.ap().rearrange("(c p) t -> p c t", p=128)
            for tt in range(16):
                i = tt % 2
                ts_ = slice(tt * 128, (tt + 1) * 128)
                k.dma(SP, [(yt[i][:], ysrc[:, :, ts_])], b_yt[i], writes=[b_yt[i]])
                k.dma(SP, [(xr[i][:], x_src.ap()[ts_, :])], b_xr[i], writes=[b_xr[i]])
                for cg in range(4):
                    k.mm([(lambda c=c: nc.tensor.matmul(PS[cg][:], lhsT=yt[i][:, c, :], rhs=Wo[:, c, cg * 512:(cg + 1) * 512],
                                                        start=(c == 0), stop=(c == 15))) for c in range(16)],
                         reads=[b_yt[i], b_Wo], writes=[b_PS[cg]])
                    k.op(DVE, lambda: nc.vector.scalar_tensor_tensor(out=rr[i][:, cg * 512:(cg + 1) * 512], in0=xr[i][:, cg * 512:(cg + 1) * 512],
                                                                     scalar=float(ALPHA), in1=PS[cg][:], op0=ALU.mult, op1=ALU.add),
                         reads=[b_xr[i], b_PS[cg]], writes=[b_rr[i]])
                layer_norm_tile("mix", rr[i], b_rr[i], stats, mv, sc2, b_small, gb, bb, b_gb, x1[i][:], b_x1[i])
                k.dma(SP, [(x1_d.ap()[ts_, :], x1[i][:])], b_x1[i], reads=[b_x1[i]])
                for cg in range(4):
                    pb = 4 + cg
                    k.mm([(lambda c=c: nc.tensor.transpose(PS[pb][:, (c % 4) * 128:(c % 4 + 1) * 128],
                                                           x1[i][:, c * 128:(c + 1) * 128], identf[:]))
                          for c in range(cg * 4, cg * 4 + 4)], reads=[b_x1[i], b_const], writes=[b_PS[pb]])
                    src = PS[pb][:].rearrange("p (c t) -> p c t", c=4)
                    k.op(ACT, lambda: nc.scalar.copy(out=x1Tb[i][:, cg * 4:cg * 4 + 4, :], in_=src), reads=[b_PS[pb]], writes=[b_x1Tb[i]])
                    if is_moe:
                        k.op(DVE, lambda: nc.vector.tensor_tensor(out=x1Tf[:, cg * 4:cg * 4 + 4, :], in0=src, in1=x1Tb[i][:, cg * 4:cg * 4 + 4, :],
                                                                  op=ALU.subtract), reads=[b_PS[pb], b_x1Tb[i]], writes=[b_x1Tf])
                k.dma(SP, [(x1Tdst[:, :, ts_], x1Tb[i][:])], b_x1Tb[i], reads=[b_x1Tb[i]])
                if is_moe:
                    rfn = []
                    for (xa, wa) in ((x1Tb[i], Wrh), (x1Tf, Wrh), (x1Tb[i], Wrl)):
                        for c in range(16):
                            rfn.append(lambda c=c, xa=xa, wa=wa: nc.tensor.matmul(PS[0][:, 0:NE], lhsT=xa[:, c, :], rhs=wa[:, c, :],
                                                                                  start=(len(rfn_i) == 0), stop=(len(rfn_i) == 47)))
                    rfn_i = []

                    def _run(f):
                        r = f()
                        rfn_i.append(1)
                        return r
                    k.mm([(lambda f=f: _run(f)) for f in rfn], reads=[b_x1Tf, b_x1Tb[i], b_Wr], writes=[b_PS[0]])
                    L, L2, SELm, Wg_, G = gl[:, 0, :], gl[:, 1, :], gl[:, 2, :], gl[:, 3, :], gl[:, 4, :]
                    AX = mybir.AxisListType.X
                    ops = [
                        (DVE, lambda: nc.vector.tensor_copy(out=L, in_=PS[0][:, 0:NE]), [b_PS[0]]),
                        (DVE, lambda: nc.vector.reduce_max(out=gsm[:, 0:1], in_=L, axis=AX), []),
                        (DVE, lambda: nc.vector.tensor_scalar(out=L2, in0=L, scalar1=gsm[:, 0:1], scalar2=-1e30, op0=ALU.is_equal, op1=ALU.mult), []),
                        (DVE, lambda: nc.vector.tensor_tensor(out=L2, in0=L2, in1=L, op=ALU.add), []),
                        (DVE, lambda: nc.vector.reduce_max(out=gsm[:, 1:2], in_=L2, axis=AX), []),
                        (DVE, lambda: nc.vector.tensor_scalar(out=SELm, in0=L, scalar1=gsm[:, 1:2], scalar2=None, op0=ALU.is_ge), []),
                        (DVE, lambda: nc.vector.tensor_scalar(out=gsm[:, 2:3], in0=gsm[:, 0:1], scalar1=-1.0, scalar2=None, op0=ALU.mult), []),
                        (ACT, lambda: nc.scalar.activation(out=Wg_, in_=L, func=AF.Exp, bias=gsm[:, 2:3], scale=1.0), []),
                        (DVE, lambda: nc.vector.tensor_tensor(out=Wg_, in0=Wg_, in1=SELm, op=ALU.mult), []),
                        (DVE, lambda: nc.vector.reduce_sum(out=gsm[:, 3:4], in_=Wg_, axis=AX), []),
                        (DVE, lambda: nc.vector.reciprocal(out=gsm[:, 3:4], in_=gsm[:, 3:4]), []),
                        (DVE, lambda: nc.vector.tensor_scalar(out=G, in0=Wg_, scalar1=gsm[:, 3:4], scalar2=None, op0=ALU.mult), []),
                    ]
                    for (E, f, rd) in ops:
                        k.op(E, f, reads=rd + [b_gl], writes=[b_gl])
                    eq1, eq2 = gl[:, 5, :], gl[:, 1, :]
                    for f in (lambda: nc.vector.tensor_scalar(out=eq1, in0=L, scalar1=gsm[:, 0:1], scalar2=None, op0=ALU.is_equal),
                              lambda: nc.vector.tensor_tensor(out=eq2, in0=SELm, in1=eq1, op=ALU.subtract),
                              lambda: nc.vector.tensor_copy(out=SELb[:], in_=SELm)):
                        k.op(DVE, f, reads=[b_gl], writes=[b_gl])
                    k.mm([lambda: nc.tensor.matmul(PS[1][:, 0:NE], lhsT=ust[:], rhs=SELb[:], start=True, stop=True),
                          lambda: nc.tensor.matmul(PS[1][:, NE:2 * NE], lhsT=onesb[:, 0, :], rhs=SELb[:], start=True, stop=True)],
                         reads=[b_gl, b_const], writes=[b_PS[1]])
                    for f in (lambda: nc.vector.tensor_tensor(out=rk[:], in0=PS[1][:, 0:NE], in1=carry[:], op=ALU.add),
                              lambda: nc.vector.tensor_tensor(out=rk[:], in0=rk[:], in1=ebase[:], op=ALU.add),
                              lambda: nc.vector.tensor_tensor(out=carry[:], in0=carry[:], in1=PS[1][:, NE:2 * NE], op=ALU.add),
                              lambda: nc.vector.tensor_tensor(out=tmp8[:], in0=eq1, in1=rk[:], op=ALU.mult),
                              lambda: nc.vector.reduce_sum(out=sfl[:, 0:1], in_=tmp8[:], axis=AX),
                              lambda: nc.vector.tensor_tensor(out=tmp8[:], in0=eq2, in1=rk[:], op=ALU.mult),
                              lambda: nc.vector.reduce_sum(out=sfl[:, 1:2], in_=tmp8[:], axis=AX),
                              lambda: nc.vector.tensor_tensor(out=tmp8[:], in0=eq1, in1=G, op=ALU.mult),
                              lambda: nc.vector.reduce_sum(out=gsel[:, tt, 0:1], in_=tmp8[:], axis=AX),
                              lambda: nc.vector.tensor_tensor(out=tmp8[:], in0=eq2, in1=G, op=ALU.mult),
                              lambda: nc.vector.reduce_sum(out=gsel[:, tt, 1:2], in_=tmp8[:], axis=AX)):
                        k.op(DVE, f, reads=[b_gl, b_PS[1], b_const], writes=[b_gl, b_gT])
                    k.op(DVE, lambda: nc.vector.tensor_copy(out=idxs[:, tt, :], in_=sfl[:]), reads=[b_gl], writes=[b_idx])
                    k.op(ACT, lambda: nc.scalar.copy(out=x1bf[i][:], in_=x1[i][:]), reads=[b_x1[i]], writes=[b_x1bf[i]])
                    k.dma(POOL, [(lambda kk_=kk_: nc.gpsimd.indirect_dma_start(
                        out=xg_d.ap(), out_offset=bass.IndirectOffsetOnAxis(idxs[:, tt, kk_:kk_ + 1], 0),
                        in_=x1bf[i][:], in_offset=None)) for kk_ in range(2)],
                          b_x1bf[i], reads=[b_x1bf[i], b_idx])
            if is_moe:
                k.op(DVE, lambda: nc.vector.tensor_copy(out=cnti[:], in_=carry[:]), reads=[b_gT, b_gl], writes=[b_gT])
                k.dma(SP, [(cnt_d.ap(), cnti[0:1, :])], b_gT, reads=[b_gT])
                if dbg:
                    k.dma(SP, [(idx_dbg.ap(), idxs[:]), (gsel_dbg.ap(), gsel[:])], b_gT, reads=[b_gT, b_idx])
            k.barrier()
        if stop == "O" and l == nlayers - 1:
            break

        def ffn_unit(xb, b_xb, hT, b_hT, WG, WU, WD, b_WG, b_WU, b_WD, sgt, b_sgt, PS, b_PS, wcnt, pcnt,
                     wg_src, wu_src, wd_src, down_evac, NS):
            rem = NS - 512
            for fg in range(NFB // 2):
                wi = wcnt[0] % 2
                wcnt[0] += 1
                k.dma(POOL, [(WG[wi][:], wg_src[:, :, fg * 256:(fg + 1) * 256])], b_WG[wi], writes=[b_WG[wi]])
                k.dma(POOL, [(WU[wi][:], wu_src[:, :, fg * 256:(fg + 1) * 256])], b_WU[wi], writes=[b_WU[wi]])
                for j in range(2):
                    fbk = fg * 2 + j
                    if rem:
                        pg, pu, pr = (2, 3, 4) if pcnt[0] % 2 == 0 else (5, 6, 7)
                    else:
                        pg = (pcnt[0] * 2) % 6
                        pu, pr = pg + 1, None
                    pcnt[0] += 1
                    for (Wt, b_Wt, pm, roff) in ((WG[wi], b_WG[wi], pg, 0), (WU[wi], b_WU[wi], pu, 128)):
                        fns = []
                        for c in range(16):
                            fns.append(lambda c=c, Wt=Wt, pm=pm: nc.tensor.matmul(PS[pm][:], lhsT=Wt[:, c, j * 128:(j + 1) * 128], rhs=xb[:, c, 0:512],
                                                                                 start=(c == 0), stop=(c == 15)))
                            if rem:
                                fns.append(lambda c=c, Wt=Wt, roff=roff: nc.tensor.matmul(PS[pr][:, roff:roff + rem], lhsT=Wt[:, c, j * 128:(j + 1) * 128],
                                                                                       rhs=xb[:, c, 512:NS], start=(c == 0), stop=(c == 15)))
                        k.mm(fns, reads=[b_Wt, b_xb], writes=[b_PS[pm]] + ([b_PS[pr]] if rem else []))
                    si = fbk % 2
                    k.op(ACT, lambda: nc.scalar.activation(out=sgt[si][:, 0:512], in_=PS[pg][:], func=AF.Silu), reads=[b_PS[pg]], writes=[b_sgt[si]])
                    k.op(DVE, lambda: nc.vector.tensor_tensor(out=hT[:, fbk, 0:512], in0=sgt[si][:, 0:512], in1=PS[pu][:], op=ALU.mult),
                         reads=[b_sgt[si], b_PS[pu]], writes=[b_hT[fbk]])
                    if rem:
                        k.op(ACT, lambda: nc.scalar.activation(out=sgt[si][:, 512:NS], in_=PS[pr][:, 0:rem], func=AF.Silu),
                             reads=[b_PS[pr]], writes=[b_sgt[si]])
                        k.op(DVE, lambda: nc.vector.tensor_tensor(out=hT[:, fbk, 512:NS], in0=sgt[si][:, 512:NS], in1=PS[pr][:, 128:128 + rem], op=ALU.mult),
                             reads=[b_sgt[si], b_PS[pr]], writes=[b_hT[fbk]])
            for dc in range(16):
                wis = []
                for hf in range(2):
                    wi = wcnt[1] % 3
                    wcnt[1] += 1
                    wis.append(wi)
                    k.dma(POOL, [(WD[wi][:], wd_src[:, hf * 22:(hf + 1) * 22, dc * 128:(dc + 1) * 128])], b_WD[wi], writes=[b_WD[wi]])
                if rem:
                    po, pq = dc % 2, 2 + dc % 2
                else:
                    po, pq = 6 + dc % 2, None
                fns = []
                for f in range(NFB):
                    fns.append(lambda f=f: nc.tensor.matmul(PS[po][:], lhsT=WD[wis[f // 22]][:, f % 22, :], rhs=hT[:, f, 0:512],
                                                            start=(f == 0), stop=(f == NFB - 1)))
                    if rem:
                        fns.append(lambda f=f: nc.tensor.matmul(PS[pq][:, 0:rem], lhsT=WD[wis[f // 22]][:, f % 22, :], rhs=hT[:, f, 512:NS],
                                                                start=(f == 0), stop=(f == NFB - 1)))
                k.mm(fns, reads=[b_WD[wis[0]], b_WD[wis[1]]] + b_hT, writes=[b_PS[po]] + ([b_PS[pq]] if rem else []))
                down_evac(dc, PS[po], b_PS[po], PS[pq] if rem else None, b_PS[pq] if rem else None)

        if not is_moe:
            with ExitStack() as st:
                xb = sb("xb", [128, 16, 512], BF16, st)
                hT = sb("hT", [128, NFB, 512], BF16, st)
                acc = sb("acc", [128, 16, 512], F32, st)
                WG = [sb(f"wg{i}", [128, 16, 256], BF16, st) for i in range(2)]
                WU = [sb(f"wu{i}", [128, 16, 256], BF16, st) for i in range(2)]
                WD = [sb(f"wd{i}", [128, NFB // 2, 128], BF16, st) for i in range(3)]
                sgt = [sb(f"sgt{i}", [128, 512], BF16, st) for i in range(2)]
                gb = sb("gb2", [128, D], F32, st)
                bb = sb("bb2", [128, D], F32, st)
                xr = sb("xr2", [128, D], F32, st)
                rr = sb("rr2", [128, D], F32, st)
                stats = sb("stats2", [128, 4, 6], F32, st)
                mv = sb("mv2", [128, 2], F32, st)
                sc2 = sb("sc22", [128, 2], F32, st)
                b_xb, b_hT, b_acc = Buf(), [Buf() for _ in range(NFB)], [Buf() for _ in range(16)]
                b_WG, b_WU, b_WD, b_sgt = [Buf(), Buf()], [Buf(), Buf()], [Buf(), Buf(), Buf()], [Buf(), Buf()]
                b_gb, b_xr, b_rr, b_small = Buf(), Buf(), Buf(), Buf()
                PS = [ps(f"psC{i}", [128, 512], F32, st) for i in range(8)]
                b_PS = [Buf() for _ in range(8)]
                k.dma(SP, [(gb[:], bcast_row(ln_ffn_g, l * D, D)), (bb[:], bcast_row(ln_ffn_b, l * D, D))], b_gb, writes=[b_gb])
                x1Tsrc = x1T_d.ap().rearrange("(c p) t -> p c t", p=128)
                wcnt = [0, 0]
                pcnt = [0]
                wg_src = dense_w_gate.ap()[0].rearrange("(c p) f -> p c f", p=128)
                wu_src = dense_w_up.ap()[0].rearrange("(c p) f -> p c f", p=128)
                wd_src = dense_w_down.ap()[0].rearrange("(c p) n -> p c n", p=128)
                for tb in range(4):
                    k.dma(SP, [(xb[:], x1Tsrc[:, :, tb * 512:(tb + 1) * 512])], b_xb, writes=[b_xb])

                    def evac_dense(dc, p, b_p, p2, b_p2):
                        k.op(ACT, lambda: nc.scalar.copy(out=acc[:, dc, :], in_=p[:]), reads=[b_p], writes=[b_acc[dc]])
                    ffn_unit(xb, b_xb, hT, b_hT, WG, WU, WD, b_WG, b_WU, b_WD, sgt, b_sgt, PS, b_PS, wcnt, pcnt,
                             wg_src, wu_src, wd_src, evac_dense, 512)
                    for t4 in range(4):
                        tt = tb * 4 + t4
                        ts_ = slice(tt * 128, (tt + 1) * 128)
                        k.dma(SP, [(xr[:], x1_d.ap()[ts_, :])], b_xr, writes=[b_xr])
                        for cg in range(4):
                            pb = cg
                            k.mm([(lambda c=c: nc.tensor.transpose(PS[pb][:, (c % 4) * 128:(c % 4 + 1) * 128],
                                                                   acc[:, c, t4 * 128:(t4 + 1) * 128], identf[:]))
                                  for c in range(cg * 4, cg * 4 + 4)], reads=b_acc[cg * 4:cg * 4 + 4] + [b_const], writes=[b_PS[pb]])
                            k.op(DVE, lambda: nc.vector.scalar_tensor_tensor(out=rr[:, cg * 512:(cg + 1) * 512], in0=xr[:, cg * 512:(cg + 1) * 512],
                                                                             scalar=float(ALPHA), in1=PS[pb][:], op0=ALU.mult, op1=ALU.add),
                                 reads=[b_xr, b_PS[pb]], writes=[b_rr])
                        layer_norm_tile("ffn", rr, b_rr, stats, mv, sc2, b_small, gb, bb, b_gb, rr[:], b_rr)
                        k.dma(SP, [(x_dst.ap()[ts_, :], rr[:])], b_rr, reads=[b_rr])
                k.barrier()
        else:
            with ExitStack() as st:
                NS, NT, EST = 640, 5, 2560
                xgr = sb("xgr", [128, NT, D], BF16, st)
                xb = sb("xbm", [128, 16, NS], BF16, st)
                hT = sb("hTm", [128, NFB, NS], BF16, st)
                WG = [sb(f"wgm{i}", [128, 16, 256], BF16, st) for i in range(2)]
                WU = [sb(f"wum{i}", [128, 16, 256], BF16, st) for i in range(2)]
                WD = [sb(f"wdm{i}", [128, NFB // 2, 128], BF16, st) for i in range(3)]
                sgt = [sb(f"sgtm{i}", [128, NS], BF16, st) for i in range(2)]
                ysb = [sb(f"ysb{i}", [128, NS], F32, st) for i in range(2)]
                yes = sb("yes", [128, NT, D], F32, st)
                PS = [ps(f"psM{i}", [128, 512], F32, st) for i in range(8)]
                dscr = sb("dscr", [1, NE], mybir.dt.int32, st)
                cregs = [nc.alloc_registers(f"cnt{l}_{e}", mybir.ALL_ENGINES) for e in range(NE)]
                for e in range(NE):
                    for r in cregs[e]:
                        nc.reg_load(r, cnt_d.ap()[0:1, e:e + 1])
                for e in range(NE):
                    wg_src = moe_w_gate.ap()[0, e].rearrange("(c p) f -> p c f", p=128)
                    wu_src = moe_w_up.ap()[0, e].rearrange("(c p) f -> p c f", p=128)
                    wd_src = moe_w_down.ap()[0, e].rearrange("(c p) n -> p c n", p=128)
                    for j in range(4):
                        snap_ = k.snapshot()
                        with nc.If_cmp(cregs[e], NS * j, "IS_GT"):
                            b_xgr, b_xb, b_hT = Buf(), Buf(), [Buf() for _ in range(NFB)]
                            b_WG, b_WU, b_WD, b_sgt = [Buf(), Buf()], [Buf(), Buf()], [Buf(), Buf(), Buf()], [Buf(), Buf()]
                            b_ysb, b_yes = [Buf(), Buf()], Buf()
                            b_PS = [Buf() for _ in range(8)]
                            base = e * EST + NS * j
                            k.dma(SP, [(xgr[:], xg_d.ap()[base:base + NS, :].rearrange("(s p) d -> p s d", p=128))], b_xgr, writes=[b_xgr])
                            for c in range(16):
                                pb = c % 8
                                pv = PS[pb][:].bitcast(BF16)
                                k.mm([(lambda s_=s_: nc.tensor.transpose(pv[:, s_ * 128:(s_ + 1) * 128], xgr[:, s_, c * 128:(c + 1) * 128], identb[:]))
                                      for s_ in range(NT)], reads=[b_xgr, b_const], writes=[b_PS[pb]])
                                if c % 2 == 0:
                                    k.op(ACT, lambda: nc.scalar.copy(out=xb[:, c, :], in_=pv[:, 0:NS]), reads=[b_PS[pb]], writes=[b_xb])
                                else:
                                    k.op(DVE, lambda: nc.vector.tensor_copy(out=xb[:, c, :], in_=pv[:, 0:NS]), reads=[b_PS[pb]], writes=[b_xb])

                            def evac_moe(dc, p, b_p, p2, b_p2):
                                si = dc % 2
                                pt, pt2 = 4 + dc % 2, 6 + dc % 2
                                k.op(ACT, lambda: nc.scalar.copy(out=ysb[si][:, 0:512], in_=p[:]), reads=[b_p], writes=[b_ysb[si]])
                                k.op(ACT, lambda: nc.scalar.copy(out=ysb[si][:, 512:NS], in_=p2[:, 0:NS - 512]), reads=[b_p2], writes=[b_ysb[si]])
                                k.mm([(lambda s_=s_: nc.tensor.transpose(PS[pt][:, s_ * 128:(s_ + 1) * 128], ysb[si][:, s_ * 128:(s_ + 1) * 128], identf[:]))
                                      for s_ in range(4)] +
                                     [lambda: nc.tensor.transpose(PS[pt2][:, 0:128], ysb[si][:, 512:NS], identf[:])],
                                     reads=[b_ysb[si], b_const], writes=[b_PS[pt], b_PS[pt2]])
                                k.op(DVE, lambda: nc.vector.tensor_copy(out=yes[:, 0:4, dc * 128:(dc + 1) * 128],
                                                                        in_=PS[pt][:].rearrange("p (s c) -> p s c", s=4)),
                                     reads=[b_PS[pt]], writes=[b_yes])
                                k.op(DVE, lambda: nc.vector.tensor_copy(out=yes[:, 4, dc * 128:(dc + 1) * 128], in_=PS[pt2][:, 0:128]),
                                     reads=[b_PS[pt2]], writes=[b_yes])
                            ffn_unit(xb, b_xb, hT, b_hT, WG, WU, WD, b_WG, b_WU, b_WD, sgt, b_sgt, PS, b_PS, [0, 0], [0],
                                     wg_src, wu_src, wd_src, evac_moe, NS)
                            k.dma(SP, [(ye_d.ap()[base:base + NS, :].rearrange("(s p) d -> p s d", p=128), yes[:])], b_yes, reads=[b_yes])
                            k.barrier()
                        with nc.Else():
                            k.compensate(snap_, dscr[0:1, :], cnt_d.ap())
            with ExitStack() as st:
                gb = sb("gb3", [128, D], F32, st)
                bb = sb("bb3", [128, D], F32, st)
                r1 = [sb(f"r1_{i}", [128, D], F32, st) for i in range(2)]
                r2 = [sb(f"r2_{i}", [128, D], F32, st) for i in range(2)]
                xr = [sb(f"xr3_{i}", [128, D], F32, st) for i in range(2)]
                stats = sb("stats3", [128, 4, 6], F32, st)
                mv = sb("mv3", [128, 2], F32, st)
                sc2 = sb("sc23", [128, 2], F32, st)
                b_gb, b_small = Buf(), Buf()
                b_r1, b_r2, b_xr = [Buf(), Buf()], [Buf(), Buf()], [Buf(), Buf()]
                k.dma(SP, [(gb[:], bcast_row(ln_ffn_g, l * D, D)), (bb[:], bcast_row(ln_ffn_b, l * D, D))], b_gb, writes=[b_gb])
                for tt in range(16):
                    i = tt % 2
                    ts_ = slice(tt * 128, (tt + 1) * 128)
                    k.dma(POOL, [lambda: nc.gpsimd.indirect_dma_start(out=r1[i][:], out_offset=None, in_=ye_d.ap(),
                                                                      in_offset=bass.IndirectOffsetOnAxis(idxs[:, tt, 0:1], 0))],
                          b_r1[i], writes=[b_r1[i]])
                    k.dma(POOL, [lambda: nc.gpsimd.indirect_dma_start(out=r2[i][:], out_offset=None, in_=ye_d.ap(),
                                                                      in_offset=bass.IndirectOffsetOnAxis(idxs[:, tt, 1:2], 0))],
                          b_r2[i], writes=[b_r2[i]])
                    k.dma(SP, [(xr[i][:], x1_d.ap()[ts_, :])], b_xr[i], writes=[b_xr[i]])
                    k.op(DVE, lambda: nc.vector.tensor_scalar(out=r1[i][:], in0=r1[i][:], scalar1=gsel[:, tt, 0:1], scalar2=None, op0=ALU.mult),
                         reads=[], writes=[b_r1[i]])
                    k.op(DVE, lambda: nc.vector.scalar_tensor_tensor(out=r1[i][:], in0=r2[i][:], scalar=gsel[:, tt, 1:2], in1=r1[i][:],
                                                                     op0=ALU.mult, op1=ALU.add), reads=[b_r2[i]], writes=[b_r1[i]])
                    k.op(DVE, lambda: nc.vector.scalar_tensor_tensor(out=r1[i][:], in0=xr[i][:], scalar=float(ALPHA), in1=r1[i][:],
                                                                     op0=ALU.mult, op1=ALU.add), reads=[b_xr[i]], writes=[b_r1[i]])
                    layer_norm_tile("ffn", r1[i], b_r1[i], stats, mv, sc2, b_small, gb, bb, b_gb, r1[i][:], b_r1[i])
                    k.dma(SP, [(x_dst.ap()[ts_, :], r1[i][:])], b_r1[i], reads=[b_r1[i]])
                k.barrier()

    k.barrier()
    stack.close()
    return nc, consts


_CACHE = {}


def kernel(**inputs):
    n = 8
    if "prog" not in _CACHE:
        _CACHE["prog"] = build_program()
    nc, consts = _CACHE["prog"]
    shared = {kk: np.ascontiguousarray(v) for kk, v in inputs.items() if kk != "x"}
    for kk, v in consts.items():
        if kk == "c_gchunk":
            continue
        shared[kk] = v
    x = np.ascontiguousarray(inputs["x"])
    in_maps = []
    for b in range(n):
        m = dict(shared)
        m["x"] = x[b]
        in_maps.append(m)
    res = run_bass_kernel_spmd(nc, in_maps, core_ids=list(range(n)))
    return np.stack([np.asarray(r["y"]) for r in res.results], axis=0).astype(np.float32)
```

```python
import os
import math
from contextlib import ExitStack
import numpy as np
import ml_dtypes
import concourse.bass as bass
import concourse.mybir as mybir
from concourse.bass_utils import run_bass_kernel_spmd

F32 = mybir.dt.float32
BF16 = mybir.dt.bfloat16
ALU = mybir.AluOpType
AF = mybir.ActivationFunctionType

S = 2048
D = 2048
DEPTH = 2
PROJ = 4992
DFF = 5632
NFB = DFF // 128
NE = 8
ALPHA = (2.0 * DEPTH) ** 0.25
EPS = 1e-5
NEGM = -30000.0
O_AQ, O_AK, O_AV, O_BQ, O_BK, O_BV, O_CQ, O_CK, O_CV, O_CG = 0, 768, 1536, 2304, 3072, 3264, 3456, 3712, 3968, 4480


def lambda_init(l):
    return 0.8 - 0.6 * math.exp(-0.3 * l)


class Sem:
    def __init__(self, h, idx):
        self.h = h
        self.idx = idx
        self.total = 0


class Buf:
    __slots__ = ("name", "w", "r", "sem")

    def __init__(self, name=""):
        self.name = name
        self.w = None
        self.r = {}
        self.sem = None


class Eng:
    def __init__(self, name, eng, sem, is_pe=False):
        self.name = name
        self.eng = eng
        self.sem = sem
        self.seen = {}
        self.is_pe = is_pe

    def wait(self, tok):
        if tok is None:
            return
        s, v, ep = tok
        if ep != EPOCH[0]:
            return
        if self.is_pe and s is self.sem:
            return
        if self.seen.get(s.idx, 0) >= v:
            return
        self.eng.wait_ge(s.h, v)
        self.seen[s.idx] = v


EPOCH = [0]


class K:
    def __init__(self, nc, stack):
        EPOCH[0] = 0
        self.nc = nc
        self.stack = stack
        self.nsem = 0
        self.all_sems = []
        self.PE = Eng("pe", nc.tensor, self.new_sem("pe"), is_pe=True)
        self.ACT = Eng("act", nc.scalar, self.new_sem("act"))
        self.DVE = Eng("dve", nc.vector, self.new_sem("dve"))
        self.POOL = Eng("pool", nc.gpsimd, self.new_sem("pool"))
        self.SP = Eng("sp", nc.sync, self.new_sem("sp"))
        self.engs = [self.PE, self.ACT, self.DVE, self.POOL, self.SP]
        self.free_dma = []
        self.stage_bufs = []

    def new_sem(self, name):
        h = self.stack.enter_context(self.nc.semaphore(f"s_{name}_{self.nsem}"))
        s = Sem(h, self.nsem)
        self.nsem += 1
        self.all_sems.append(s)
        return s

    def _deps(self, E, reads, writes):
        for b in reads:
            E.wait(b.w)
        for b in writes:
            E.wait(b.w)
            for t in b.r.values():
                E.wait(t)

    def _mark(self, tok, reads, writes):
        s = tok[0]
        for b in reads:
            b.r[s.idx] = tok
        for b in writes:
            b.w = tok
            b.r = {}

    def op(self, E, fn, reads=(), writes=()):
        self._deps(E, reads, writes)
        inst = fn()
        E.sem.total += 1
        inst.then_inc(E.sem.h, 1)
        tok = (E.sem, E.sem.total, EPOCH[0])
        self._mark(tok, reads, writes)
        return tok

    def mm(self, fns, reads=(), writes=()):
        E = self.PE
        self._deps(E, reads, writes)
        inst = None
        for f in fns:
            inst = f()
        E.sem.total += 1
        inst.then_inc(E.sem.h, 1)
        tok = (E.sem, E.sem.total, EPOCH[0])
        self._mark(tok, reads, writes)
        return tok

    def dma(self, Q, pairs, sem_buf, reads=(), writes=()):
        self._deps(Q, reads, writes)
        if sem_buf.sem is None:
            if self.free_dma:
                sem_buf.sem = self.free_dma.pop()
            else:
                sem_buf.sem = self.new_sem("dma")
            self.stage_bufs.append(sem_buf)
        s = sem_buf.sem
        s.queue = Q
        for pr in pairs:
            if callable(pr):
                inst = pr()
            else:
                inst = Q.eng.dma_start(out=pr[0], in_=pr[1])
            s.total += 16
            inst.then_inc(s.h, 16)
        tok = (s, s.total, EPOCH[0])
        self._mark(tok, reads, writes)
        return tok

    def snapshot(self):
        return {s_.idx: s_.total for s_ in self.all_sems}

    def compensate(self, snap, dummy_out, dummy_in):
        eng_of = {E.sem.idx: E for E in self.engs}
        for s_ in self.all_sems:
            d = s_.total - snap.get(s_.idx, 0)
            if d <= 0:
                continue
            if s_.idx in eng_of:
                eng_of[s_.idx].eng.sem_inc(s_.h, d)
            else:
                s_.queue.eng.dma_start(out=dummy_out, in_=dummy_in).then_inc(s_.h, d)

    def reset(self):
        return

    def _reset(self):
        if not hasattr(self, "bp"):
            self.bp = [(self.new_sem("hbB"), self.new_sem("hbC")) for _ in range(3)]
            self.bp_ids = {x.idx for p in self.bp for x in p}
            self.hbn = 0
        pair = self.bp[self.hbn % 3]
        nxt = self.bp[(self.hbn + 1) % 3]
        self.hbn += 1
        for E in self.engs:
            E.eng.sem_inc(pair[0].h, 1)
        sp = self.SP.eng
        sp.wait_ge(pair[0].h, len(self.engs))
        for s_ in self.all_sems:
            if s_.idx in self.bp_ids:
                continue
            sp.sem_clear(s_.h)
            s_.total = 0
        sp.sem_clear(nxt[0].h)
        sp.sem_clear(nxt[1].h)
        sp.sem_inc(pair[1].h, 1)
        for E in self.engs:
            E.eng.wait_ge(pair[1].h, 1)
            E.seen = {}
        EPOCH[0] += 1

    def barrier(self):
        for E in self.engs:
            for s in self.all_sems:
                if s.total > 0:
                    E.wait((s, s.total, EPOCH[0]))
        for b in self.stage_bufs:
            self.free_dma.append(b.sem)
            b.sem = None
        self.stage_bufs = []


def _rel_bucket_np(dist):
    d = np.maximum(dist, 0)
    ratio = np.maximum(d, 1).astype(np.float32) / np.float32(16)
    large = 16 + (np.log(ratio).astype(np.float32) / np.float32(math.log(128 / 16)) * np.float32(16)).astype(np.int32)
    large = np.minimum(large, 31)
    return np.where(d < 16, d, large)


def make_consts():
    bf = ml_dtypes.bfloat16
    c = {}
    c["c_identf"] = np.eye(128, dtype=np.float32)
    c["c_identb"] = np.eye(128, dtype=np.float32).astype(bf)
    ob = np.zeros((128, 3, 128), np.float32)
    ob[:, 0, :] = 1.0
    ob[:, 1, :64] = 1.0
    ob[:, 2, 64:] = 1.0
    c["c_onesb"] = ob.astype(bf)
    R = np.zeros((64, 64), np.float32)
    for i in range(32):
        R[2 * i + 1, 2 * i] = -1.0
        R[2 * i, 2 * i + 1] = 1.0
    R128 = np.zeros((128, 128), np.float32)
    R128[:64, :64] = R
    R128[64:, 64:] = R
    c["c_rot"] = R128.astype(bf)
    kk = np.arange(128)[:, None]
    jj = np.arange(256)[None, :]
    mk = np.zeros((128, 2, 256), np.float32)
    mk[:, 0, :] = np.where(jj >= kk, 0.0, NEGM)
    mk[:, 1, :] = np.where((jj - kk >= 0) & (jj - kk < 128), 0.0, NEGM)
    c["c_mask"] = mk
    m = np.arange(384)
    dist = m - 127
    bk = _rel_bucket_np(dist)
    oh = np.zeros((32, 384), np.float32)
    for i in range(384):
        if dist[i] >= 0:
            oh[bk[i], i] = 1.0
    c["c_ohb"] = oh
    hh = np.arange(4, dtype=np.float32)
    log_g = np.log(1.0 - np.exp2(-5.0 - hh)).astype(np.float64)
    pos = np.arange(128)
    dec = np.zeros((128, 4, 128), np.float32)
    for h in range(4):
        rel = pos[None, :] - pos[:, None]
        dec[:, h, :] = np.where(rel >= 0, np.exp(np.maximum(rel, 0) * log_g[h]), 0.0)
    c["c_decay"] = dec
    zeta = np.exp((127 - pos)[:, None] * log_g[None, :]).astype(np.float32)
    c["c_zeta"] = zeta
    xi = np.exp((pos + 1)[:, None] * log_g[None, :])
    ang = np.repeat(1.0 / (10000.0 ** np.linspace(0.0, 1.0, 32, dtype=np.float32)), 2).astype(np.float32)
    ang = np.arange(S, dtype=np.float32)[:, None] * ang[None, :]
    sin, cos = np.sin(ang).astype(np.float32), np.cos(ang).astype(np.float32)
    tab = np.zeros((128, 6, S), np.float32)
    for p in range(128):
        d = p % 64
        tab[p, 0, :] = cos[:, d]
        tab[p, 1, :] = sin[:, d]
        for blk in range(2):
            h = 2 * blk + p // 64
            xs = xi[np.arange(S) % 128, h]
            tab[p, 2 + 2 * blk, :] = cos[:, d] * xs
            tab[p, 3 + 2 * blk, :] = sin[:, d] * xs
    c["c_tab"] = tab
    c["c_gchunk"] = np.exp(128 * log_g).astype(np.float64)
    tt_ = np.arange(128)
    c["c_ust"] = (tt_[:, None] < tt_[None, :]).astype(np.float32).astype(bf)
    c["c_ebase"] = np.broadcast_to((np.arange(8, dtype=np.float32) * 2560.0)[None, :], (128, 8)).copy()
    return c


def build_program(dbg=False, nlayers=DEPTH, stop=None):
    nc = bass.Bass("TRN2", target_bir_lowering=False)
    need_dense = (stop is None) or nlayers > 1
    need_moe = (stop is None and nlayers > 1)
    need_router = nlayers > 1
    consts = make_consts()
    gchunk = consts["c_gchunk"]

    def din(name, shape, dt=F32):
        return nc.dram_tensor(name, list(shape), dt, kind="ExternalInput")

    x_in = din("x", [S, D])
    w_in = din("w_in", [DEPTH, D, PROJ])
    rel_bias = din("rel_bias", [32, 18])
    a_lambda = din("a_lambda", [DEPTH, 4, 64])
    a_subln_g = din("a_subln_g", [DEPTH, 128])
    b_sinks = din("b_sinks", [DEPTH, 12])
    w_out = din("w_out", [DEPTH, D, D])
    ln_mix_g = din("ln_mix_g", [DEPTH, D])
    ln_mix_b = din("ln_mix_b", [DEPTH, D])
    ln_ffn_g = din("ln_ffn_g", [DEPTH, D])
    ln_ffn_b = din("ln_ffn_b", [DEPTH, D])
    if need_dense:
        dense_w_gate = din("dense_w_gate", [1, D, DFF])
        dense_w_up = din("dense_w_up", [1, D, DFF])
        dense_w_down = din("dense_w_down", [1, DFF, D])
    if need_router:
        moe_router = din("moe_router", [1, D, NE])
    if need_moe:
        moe_w_gate = din("moe_w_gate", [1, NE, D, DFF])
        moe_w_up = din("moe_w_up", [1, NE, D, DFF])
        moe_w_down = din("moe_w_down", [1, NE, DFF, D])
    c_identf = din("c_identf", [128, 128])
    c_identb = din("c_identb", [128, 128], BF16)
    c_onesb = din("c_onesb", [128, 3, 128], BF16)
    c_rot = din("c_rot", [128, 128], BF16)
    c_mask = din("c_mask", [128, 2, 256])
    c_ohb = din("c_ohb", [32, 384])
    c_decay = din("c_decay", [128, 4, 128])
    c_zeta = din("c_zeta", [128, 4])
    c_tab = din("c_tab", [128, 6, S])
    c_ust = din("c_ust", [128, 128], BF16)
    c_ebase = din("c_ebase", [128, 8])

    skind = "ExternalOutput" if dbg else "Internal"
    y_out = nc.dram_tensor("y", [S, D], F32, kind="ExternalOutput")
    yT_d = nc.dram_tensor("yT_d", [D, S], BF16, kind=skind)
    x1_d = nc.dram_tensor("x1_d", [S, D], F32, kind=skind)
    x1T_d = nc.dram_tensor("x1T_d", [D, S], BF16, kind=skind)
    x2_d = nc.dram_tensor("x2_d", [S, D], F32, kind=skind)
    z_d = nc.dram_tensor("z_d", [18, 128, 384], F32, kind=skind)
    xg_d = nc.dram_tensor("xg_d", [NE * 2560, D], BF16, kind="Internal")
    ye_d = nc.dram_tensor("ye_d", [NE * 2560, D], F32, kind="Internal")
    cnt_d = nc.dram_tensor("cnt_d", [1, NE], mybir.dt.int32, kind=skind)
    if dbg:
        idx_dbg = nc.dram_tensor("idx_dbg", [128, 16, 2], mybir.dt.int32, kind="ExternalOutput")
        gsel_dbg = nc.dram_tensor("gsel_dbg", [128, 16, 2], F32, kind="ExternalOutput")

    stack = ExitStack()
    stack.enter_context(nc.allow_low_precision("bf16 matmul operands with fp32 accumulation"))
    stack.enter_context(nc.allow_non_contiguous_dma(reason="tiny parameter loads"))
    k = K(nc, stack)
    PE, ACT, DVE, POOL, SP = k.PE, k.ACT, k.DVE, k.POOL, k.SP

    uid = [0]

    def sb(name, shape, dt, st=None):
        uid[0] += 1
        return (st or stack).enter_context(nc.sbuf_tensor(f"{name}_{uid[0]}", list(shape), dt))

    def ps(name, shape, dt, st):
        uid[0] += 1
        return st.enter_context(nc.psum_tensor(f"{name}_{uid[0]}", list(shape), dt))

    identf = sb("identf", [128, 128], F32)
    identb = sb("identb", [128, 128], BF16)
    onesb = sb("onesb", [128, 3, 128], BF16)
    rotb = sb("rotb", [128, 128], BF16)
    TA = sb("TA", [128, 6, 256], BF16)
    TB = sb("TB", [128, 12, 256], BF16)
    cA = sb("cA", [128, 6], F32)
    ust = sb("ust", [128, 128], BF16)
    ebase = sb("ebase", [128, 8], F32)
    idxs = sb("idxs", [128, 16, 2], mybir.dt.int32)
    gsel = sb("gsel", [128, 16, 2], F32)
    carry = sb("carry", [128, 8], F32)
    b_const = Buf("const")
    b_gT = Buf("gT")
    epsc = sb("epsc", [128, 2], F32)
    k.op(DVE, lambda: nc.vector.memset(epsc[:, 0:1], EPS), writes=[b_const])
    k.op(DVE, lambda: nc.vector.memset(epsc[:, 1:2], 128.0 * EPS), writes=[b_const])

    k.dma(SP, [(identf[:], c_identf.ap()), (identb[:], c_identb.ap()), (onesb[:], c_onesb.ap()),
               (rotb[:], c_rot.ap()), (ust[:], c_ust.ap()), (ebase[:], c_ebase.ap())], b_const, writes=[b_const])

    with ExitStack() as st:
        tabs = sb("tabs", [32, 18], F32, st)
        ohb = sb("ohb", [32, 384], F32, st)
        ones32 = sb("ones32", [32, 128], BF16, st)
        oht = sb("oht", [32, 2, 384], BF16, st)
        frep = sb("frep", [128, 18, 384], F32, st)
        traw = sb("traw", [128, 18, 256], F32, st)
        mask = sb("mask", [128, 2, 256], F32, st)
        pf = [ps(f"pf{i}", [128, 512], F32, st) for i in range(2)]
        b_tabs, b_frep, b_traw, b_mask = Buf(), Buf(), Buf(), Buf()
        b_oht = [Buf(), Buf()]
        b_pf = [Buf(), Buf()]
        b_ones32 = Buf()
        k.dma(SP, [(tabs[:], rel_bias.ap()), (ohb[:], c_ohb.ap())], b_tabs, writes=[b_tabs])
        k.dma(SP, [(mask[:], c_mask.ap())], b_mask, writes=[b_mask])
        k.op(DVE, lambda: nc.vector.memset(ones32[:], 1.0), writes=[b_ones32])
        for h in range(18):
            i = h % 2
            k.op(DVE, lambda: nc.vector.tensor_scalar(out=oht[:, i, :], in0=ohb[:], scalar1=tabs[:, h:h + 1], scalar2=None,
                                                      op0=ALU.mult), reads=[b_tabs], writes=[b_oht[i]])
            k.mm([lambda: nc.tensor.matmul(pf[i][:, 0:384], lhsT=ones32[:], rhs=oht[:, i, :], start=True, stop=True)],
                 reads=[b_oht[i], b_ones32], writes=[b_pf[i]])
            k.op(ACT, lambda: nc.scalar.copy(out=frep[:, h, :], in_=pf[i][:, 0:384]), reads=[b_pf[i]], writes=[b_frep])
        k.dma(SP, [(z_d.ap().rearrange("h k m -> k h m"), frep[:])], b_frep, reads=[b_frep])
        k.barrier()
        skew = bass.AP(z_d, 127, [[383, 128], [128 * 384, 18], [1, 256]])
        k.dma(SP, [(traw[:], skew)], b_traw, writes=[b_traw])
        k.op(DVE, lambda: nc.vector.tensor_copy(out=cA[:], in_=frep[:, 0:6, 382]), reads=[b_frep], writes=[b_const])
        for h in range(6):
            k.op(DVE, lambda: nc.vector.scalar_tensor_tensor(out=TA[:, h, :], in0=traw[:, h, :], scalar=cA[:, h:h + 1],
                                                             in1=mask[:, 0, :], op0=ALU.subtract, op1=ALU.add),
                 reads=[b_traw, b_mask, b_const], writes=[b_const])
        for h in range(12):
            k.op(DVE, lambda: nc.vector.tensor_tensor(out=TB[:, h, :], in0=traw[:, 6 + h, :], in1=mask[:, 1, :], op=ALU.add),
                 reads=[b_traw, b_mask], writes=[b_const])
        k.barrier()

    def layer_norm_tile(st_name, r, b_r, stats, mv, sc2, b_small, gb, bb, b_gb, out_t, b_out):
        for j in range(4):
            k.op(DVE, lambda: nc.vector.bn_stats(out=stats[:, j, :], in_=r[:, j * 512:(j + 1) * 512]),
                 reads=[b_r], writes=[b_small])
        k.op(DVE, lambda: nc.vector.bn_aggr(out=mv[:], in_=stats[:]), reads=[b_small], writes=[b_small])
        k.op(ACT, lambda: nc.scalar.activation(out=sc2[:, 0:1], in_=mv[:, 1:2], func=AF.Ln, bias=epsc[:, 0:1], scale=1.0),
             reads=[b_small, b_const], writes=[b_small])
        k.op(ACT, lambda: nc.scalar.activation(out=sc2[:, 0:1], in_=sc2[:, 0:1], func=AF.Exp, scale=-0.5),
             reads=[], writes=[b_small])
        k.op(DVE, lambda: nc.vector.scalar_tensor_tensor(out=sc2[:, 1:2], in0=mv[:, 0:1], scalar=-1.0, in1=sc2[:, 0:1],
                                                         op0=ALU.mult, op1=ALU.mult), reads=[b_small], writes=[b_small])
        k.op(ACT, lambda: nc.scalar.activation(out=out_t, in_=r[:], func=AF.Identity, bias=sc2[:, 1:2], scale=sc2[:, 0:1]),
             reads=[b_r, b_small], writes=[b_out])
        k.op(DVE, lambda: nc.vector.tensor_tensor(out=out_t, in0=out_t, in1=gb[:], op=ALU.mult),
             reads=[b_gb], writes=[b_out])
        k.op(DVE, lambda: nc.vector.tensor_tensor(out=out_t, in0=out_t, in1=bb[:], op=ALU.add),
             reads=[b_gb], writes=[b_out])

    def bcast_row(handle, off, n):
        return bass.AP(handle, off, [[0, 128], [1, n]])

    for l in range(nlayers):
        x_src = x_in if l == 0 else x2_d
        x_dst = y_out if l == nlayers - 1 else x2_d

        with ExitStack() as st:
            xT = sb("xT", [128, 16, S], BF16, st)
            b_xT = [Buf() for _ in range(16)]
            WB = [sb(f"wb{i}", [128, 16, 256], BF16, st) for i in range(2)]
            b_WB = [Buf(), Buf()]
            wb_i = [0]
            PS = [ps(f"psA{i}", [128, 512], F32, st) for i in range(8)]
            b_PS = [Buf() for _ in range(8)]

            with ExitStack() as st2:
                xin = [sb(f"xin{i}", [128, D], F32, st2) for i in range(2)]
                b_xin = [Buf(), Buf()]
                for tt in range(16):
                    i = tt % 2
                    k.dma(SP, [(xin[i][:], x_src.ap()[tt * 128:(tt + 1) * 128, :])], b_xin[i], writes=[b_xin[i]])
                    for cg in range(4):
                        pb = (tt * 4 + cg) % 8
                        k.mm([(lambda c=c: nc.tensor.transpose(PS[pb][:, (c % 4) * 128:(c % 4 + 1) * 128],
                                                               xin[i][:, c * 128:(c + 1) * 128], identf[:]))
                              for c in range(cg * 4, cg * 4 + 4)],
                             reads=[b_xin[i], b_const], writes=[b_PS[pb]])
                        E = ACT if cg % 2 == 0 else DVE
                        src = PS[pb][:].rearrange("p (c t) -> p c t", c=4)
                        dst = xT[:, cg * 4:cg * 4 + 4, tt * 128:(tt + 1) * 128]
                        if E is ACT:
                            k.op(ACT, lambda: nc.scalar.copy(out=dst, in_=src), reads=[b_PS[pb]], writes=[b_xT[tt]])
                        else:
                            k.op(DVE, lambda: nc.vector.tensor_copy(out=dst, in_=src), reads=[b_PS[pb]], writes=[b_xT[tt]])
                k.barrier()

            ps_rr = [0]

            def next_ps():
                i = ps_rr[0] % 8
                ps_rr[0] += 1
                return i

            def load_w(col_pairs, ncols):
                i = wb_i[0] % 2
                wb_i[0] += 1
                wsrc = w_in.ap()[l].rearrange("(c p) f -> p c f", p=128)
                pairs = [(WB[i][:, :, dc:dc + n], wsrc[:, :, sc:sc + n]) for (dc, sc, n) in col_pairs]
                k.dma(POOL, pairs, b_WB[i], writes=[b_WB[i]])
                return WB[i], b_WB[i]

            def proj_fm(col_pairs, nblk, scale, dsts, b_dsts):
                wt, b_wt = load_w(col_pairs, nblk * 128)
                for j in range(nblk):
                    for tb in range(4):
                        pb = next_ps()
                        k.mm([(lambda c=c: nc.tensor.matmul(PS[pb][:], lhsT=wt[:, c, j * 128:(j + 1) * 128],
                                                            rhs=xT[:, c, tb * 512:(tb + 1) * 512],
                                                            start=(c == 0), stop=(c == 15))) for c in range(16)],
                             reads=[b_wt] + b_xT[tb * 4:tb * 4 + 4], writes=[b_PS[pb]])
                        k.op(ACT, lambda: nc.scalar.activation(out=dsts[j][:, tb * 512:(tb + 1) * 512], in_=PS[pb][:],
                                                               func=AF.Copy, scale=float(scale)),
                             reads=[b_PS[pb]], writes=[b_dsts[j]])

            def proj_tm(col0, ncols, evac):
                wt, b_wt = load_w([(0, col0, ncols)], ncols)
                for tt in range(16):
                    pb = next_ps()
                    k.mm([(lambda c=c: nc.tensor.matmul(PS[pb][:, 0:ncols], lhsT=xT[:, c, tt * 128:(tt + 1) * 128],
                                                        rhs=wt[:, c, 0:ncols], start=(c == 0), stop=(c == 15)))
                          for c in range(16)],
                         reads=[b_wt, b_xT[tt]], writes=[b_PS[pb]])
                    evac(tt, PS[pb], b_PS[pb])

            def proj_fm_cols(col0, nblk, scale, dsts, b_dsts):
                for j0 in range(0, nblk, 2):
                    nb = min(2, nblk - j0)
                    proj_fm([(0, col0 + j0 * 128, nb * 128)], nb, scale, dsts[j0:j0 + nb], b_dsts[j0:j0 + nb])

            def proj_tm_cols(col0, ncols, mk_evac):
                for c0 in range(0, ncols, 256):
                    ncl = min(256, ncols - c0)
                    proj_tm(col0 + c0, ncl, mk_evac(c0, ncl))

            lam3 = sb("lam3", [128, 4, 64], F32, st)
            lamt = sb("lamt", [128, 8], F32, st)
            gsub = sb("gsub", [128, 2], F32, st)
            esink = sb("esink", [128, 6], F32, st)
            b_par = Buf()
            k.dma(SP, [(lam3[:].rearrange("p a b -> p (a b)"), bcast_row(a_lambda, l * 256, 256)),
                       (gsub[:, 0:1], bass.AP(a_subln_g, l * 128, [[1, 128], [1, 1]])),
                       (esink[0:64, :], bass.AP(b_sinks, l * 12, [[0, 64], [2, 6]])),
                       (esink[64:128, :], bass.AP(b_sinks, l * 12 + 1, [[0, 64], [2, 6]]))],
                  b_par, writes=[b_par])
            for a in range(2):
                k.op(DVE, lambda: nc.vector.tensor_tensor(out=lam3[:, 2 * a, :], in0=lam3[:, 2 * a, :], in1=lam3[:, 2 * a + 1, :],
                                                          op=ALU.mult), reads=[b_par], writes=[b_par])
                k.op(DVE, lambda: nc.vector.reduce_sum(out=lamt[:, a:a + 1], in_=lam3[:, 2 * a, :], axis=mybir.AxisListType.X),
                     reads=[b_par], writes=[b_par])
            k.op(ACT, lambda: nc.scalar.activation(out=lamt[:, 2:4], in_=lamt[:, 0:2], func=AF.Exp), reads=[b_par], writes=[b_par])
            k.op(DVE, lambda: nc.vector.tensor_tensor(out=lamt[:, 4:5], in0=lamt[:, 3:4], in1=lamt[:, 2:3], op=ALU.subtract),
                 reads=[b_par], writes=[b_par])
            k.op(DVE, lambda: nc.vector.tensor_scalar(out=lamt[:, 4:5], in0=lamt[:, 4:5], scalar1=-lambda_init(l), scalar2=None,
                                                      op0=ALU.add), reads=[b_par], writes=[b_par])
            k.op(DVE, lambda: nc.vector.tensor_scalar(out=gsub[:, 1:2], in0=gsub[:, 0:1],
                                                      scalar1=float(math.sqrt(128.0) * (1.0 - lambda_init(l))), scalar2=None,
                                                      op0=ALU.mult), reads=[b_par], writes=[b_par])
            k.op(ACT, lambda: nc.scalar.activation(out=esink[:], in_=esink[:], func=AF.Exp), reads=[b_par], writes=[b_par])
            neglam = lamt[:, 4:5]

            with ExitStack() as st2:
                Cqf = [sb(f"Cqf{j}", [128, S], BF16, st2) for j in range(2)]
                Cqx = [sb(f"Cqx{j}", [128, S], BF16, st2) for j in range(2)]
                Ckf = [sb(f"Ckf{j}", [128, S], BF16, st2) for j in range(2)]
                Ckt = sb("Ckt", [128, 16, 256], BF16, st2)
                Cv = sb("Cv", [128, 16, 512], BF16, st2)
                Csg = sb("Csg", [128, 16, 512], BF16, st2)
                decay = sb("decay", [128, 4, 128], F32, st2)
                zeta = sb("zeta", [128, 4], F32, st2)
                b_Cqf, b_Cqx, b_Ckf = [Buf(), Buf()], [Buf(), Buf()], [Buf(), Buf()]
                b_Ckt, b_Cv, b_Csg = Buf(), Buf(), Buf()
                b_cc = Buf()
                k.dma(SP, [(decay[:], c_decay.ap()), (zeta[:], c_zeta.ap())], b_cc, writes=[b_cc])

                def mk_evac_cv(c0, ncl):
                    def ev(tt, p, b_p):
                        k.op(ACT, lambda: nc.scalar.copy(out=Cv[:, tt, c0:c0 + ncl], in_=p[:, 0:ncl]), reads=[b_p], writes=[b_Cv])
                    return ev
                proj_tm_cols(O_CV, 512, mk_evac_cv)

                def mk_evac_cg(c0, ncl):
                    def ev(tt, p, b_p):
                        k.op(ACT, lambda: nc.scalar.activation(out=Csg[:, tt, c0:c0 + ncl], in_=p[:, 0:ncl], func=AF.Silu),
                             reads=[b_p], writes=[b_Csg])
                    return ev
                proj_tm_cols(O_CG, 512, mk_evac_cg)

                with ExitStack() as st3:
                    CqT = [sb(f"CqT{j}", [128, S], BF16, st3) for j in range(2)]
                    CkT = [sb(f"CkT{j}", [128, S], BF16, st3) for j in range(2)]
                    tabt = [sb(f"tabt{i}", [128, 6, 512], F32, st3) for i in range(1)]
                    tmp1 = [sb(f"rtmp{i}", [128, 512], F32, st3) for i in range(2)]
                    b_CqT, b_CkT = [Buf(), Buf()], [Buf(), Buf()]
                    b_tabt = [Buf()]
                    b_tmp1 = [Buf(), Buf()]
                    proj_fm_cols(O_CQ, 2, 1.0, CqT, b_CqT)
                    proj_fm_cols(O_CK, 2, 0.125, CkT, b_CkT)
                    for tb in range(4):
                        ti = 0
                        k.dma(SP, [(tabt[ti][:], c_tab.ap()[:, :, tb * 512:(tb + 1) * 512])], b_tabt[ti], writes=[b_tabt[ti]])
                        for j in range(2):
                            for (src, b_src, outs) in ((CqT[j], b_CqT[j], [(Cqf[j], b_Cqf[j], 0, 1), (Cqx[j], b_Cqx[j], 2 + 2 * j, 3 + 2 * j)]),
                                                       (CkT[j], b_CkT[j], [(Ckf[j], b_Ckf[j], 0, 1)])):
                                pb = next_ps()
                                sl = slice(tb * 512, (tb + 1) * 512)
                                k.mm([lambda: nc.tensor.matmul(PS[pb][:], lhsT=rotb[:], rhs=src[:, sl], start=True, stop=True)],
                                     reads=[b_src, b_const], writes=[b_PS[pb]])
                                for (dst, b_dst, ic, isn) in outs:
                                    t1 = tmp1[0]
                                    t2 = tmp1[1]
                                    k.op(DVE, lambda: nc.vector.tensor_tensor(out=t1[:], in0=src[:, sl], in1=tabt[ti][:, ic, :], op=ALU.mult),
                                         reads=[b_src, b_tabt[ti]], writes=[b_tmp1[0]])
                                    k.op(DVE, lambda: nc.vector.tensor_tensor(out=t2[:], in0=PS[pb][:], in1=tabt[ti][:, isn, :], op=ALU.mult),
                                         reads=[b_PS[pb], b_tabt[ti]], writes=[b_tmp1[1]])
                                    k.op(DVE, lambda: nc.vector.tensor_tensor(out=dst[:, sl], in0=t1[:], in1=t2[:], op=ALU.add),
                                         reads=[b_tmp1[0], b_tmp1[1]], writes=[b_dst])
                    k.barrier()
                PSb = [PS[i][:].bitcast(BF16) if hasattr(PS[i][:], "bitcast") else None for i in range(8)]
                for tt in range(16):
                    pb = next_ps()
                    pview = PSb[pb]
                    k.mm([(lambda j=j: nc.tensor.transpose(pview[:, j * 128:(j + 1) * 128], Ckf[j][:, tt * 128:(tt + 1) * 128], identb[:]))
                          for j in range(2)], reads=[b_Ckf[0], b_Ckf[1], b_const], writes=[b_PS[pb]])
                    for h in range(4):
                        k.op(DVE, lambda: nc.vector.tensor_scalar(out=Ckt[:, tt, h * 64:(h + 1) * 64], in0=pview[:, h * 64:(h + 1) * 64],
                                                                  scalar1=zeta[:, h:h + 1], scalar2=None, op0=ALU.mult),
                             reads=[b_PS[pb], b_cc], writes=[b_Ckt])

                ST = sb("ST", [128, 2, 128], F32, st2)
                STb = [sb(f"STb{i}", [128, 2, 128], BF16, st2) for i in range(2)]
                iT = [sb(f"iT{i}", [128, 128], BF16, st2) for i in range(4)]
                ycm = [sb(f"ycm{i}", [128, 512], BF16, st2) for i in range(2)]
                ycT = [sb(f"ycT{i}", [128, 4, 128], BF16, st2) for i in range(2)]
                cst = sb("cst", [128, 4, 6], F32, st2)
                cmv = sb("cmv", [128, 4, 2], F32, st2)
                crs = sb("crs", [128, 4], F32, st2)
                ctmp = sb("ctmp", [128, 512], F32, st2)
                b_ST, b_STb, b_iT = Buf(), [Buf(), Buf()], [Buf() for _ in range(4)]
                b_ycm, b_ycT, b_cs, b_ctmp = [Buf(), Buf()], [Buf(), Buf()], Buf(), Buf()
                k.op(DVE, lambda: nc.vector.memset(ST[:], 0.0), writes=[b_ST])
                for n in range(16):
                    cs = slice(n * 128, (n + 1) * 128)
                    po = next_ps()
                    for h in range(4):
                        blk, p0 = h // 2, 64 * (h % 2)
                        pi = next_ps()
                        if pi == po:
                            pi = next_ps()
                        k.mm([lambda: nc.tensor.matmul(PS[pi][:, 0:128], lhsT=Ckf[blk][p0:p0 + 64, cs], rhs=Cqf[blk][p0:p0 + 64, cs],
                                                       start=True, stop=True)],
                             reads=[b_Ckf[blk], b_Cqf[blk]], writes=[b_PS[pi]])
                        it = (n * 4 + h) % 4
                        k.op(DVE, lambda: nc.vector.tensor_tensor(out=iT[it][:], in0=PS[pi][:, 0:128], in1=decay[:, h, :], op=ALU.mult),
                             reads=[b_PS[pi], b_cc], writes=[b_iT[it]])
                        fns = [lambda: nc.tensor.matmul(PS[po][:, h * 128:(h + 1) * 128], lhsT=iT[it][:], rhs=Cv[:, n, h * 128:(h + 1) * 128],
                                                        start=True, stop=(n == 0))]
                        rds = [b_iT[it], b_Cv]
                        if n > 0:
                            sbf = STb[n % 2]
                            fns.append(lambda: nc.tensor.matmul(PS[po][:, h * 128:(h + 1) * 128], lhsT=Cqx[blk][p0:p0 + 64, cs],
                                                                rhs=sbf[p0:p0 + 64, blk, :], start=False, stop=True))
                            rds += [b_Cqx[blk], b_STb[n % 2]]
                        k.mm(fns, reads=rds, writes=[b_PS[po]])
                    if n < 15:
                        for blk in range(2):
                            pk = next_ps()
                            if pk == po:
                                pk = next_ps()
                            k.mm([lambda: nc.tensor.matmul(PS[pk][:, 0:256], lhsT=Ckt[:, n, blk * 128:(blk + 1) * 128],
                                                           rhs=Cv[:, n, blk * 256:(blk + 1) * 256], start=True, stop=True)],
                                 reads=[b_Ckt, b_Cv], writes=[b_PS[pk]])
                            for par in range(2):
                                h = 2 * blk + par
                                pr = slice(64 * par, 64 * par + 64)
                                k.op(DVE, lambda: nc.vector.scalar_tensor_tensor(out=ST[pr, blk, :], in0=ST[pr, blk, :],
                                                                                 scalar=float(gchunk[h]),
                                                                                 in1=PS[pk][pr, par * 128:(par + 1) * 128],
                                                                                 op0=ALU.mult, op1=ALU.add),
                                     reads=[b_PS[pk]], writes=[b_ST])
                        nb = (n + 1) % 2
                        k.op(ACT, lambda: nc.scalar.copy(out=STb[nb][:], in_=ST[:]), reads=[b_ST], writes=[b_STb[nb]])
                    yi = n % 2
                    for h in range(4):
                        k.op(DVE, lambda: nc.vector.bn_stats(out=cst[:, h, :], in_=PS[po][:, h * 128:(h + 1) * 128]),
                             reads=[b_PS[po]], writes=[b_cs])
                        k.op(DVE, lambda: nc.vector.bn_aggr(out=cmv[:, h, :], in_=cst[:, h, :]), reads=[b_cs], writes=[b_cs])
                    k.op(ACT, lambda: nc.scalar.activation(out=crs[:], in_=cmv[:, :, 1], func=AF.Ln, bias=epsc[:, 0:1], scale=1.0),
                         reads=[b_cs, b_const], writes=[b_cs])
                    k.op(ACT, lambda: nc.scalar.activation(out=crs[:], in_=crs[:], func=AF.Exp, scale=-0.5),
                         reads=[], writes=[b_cs])
                    for h in range(4):
                        hs = slice(h * 128, (h + 1) * 128)
                        k.op(DVE, lambda: nc.vector.tensor_scalar(out=ctmp[:, hs], in0=PS[po][:, hs], scalar1=cmv[:, h, 0:1],
                                                                  scalar2=crs[:, h:h + 1], op0=ALU.subtract, op1=ALU.mult),
                             reads=[b_PS[po], b_cs], writes=[b_ctmp])
                    k.op(DVE, lambda: nc.vector.tensor_tensor(out=ycm[yi][:], in0=ctmp[:], in1=Csg[:, n, :], op=ALU.mult),
                         reads=[b_ctmp, b_Csg], writes=[b_ycm[yi]])
                    pt = next_ps()
                    pview = PSb[pt]
                    k.mm([(lambda h=h: nc.tensor.transpose(pview[:, h * 128:(h + 1) * 128], ycm[yi][:, h * 128:(h + 1) * 128], identb[:]))
                          for h in range(4)], reads=[b_ycm[yi], b_const], writes=[b_PS[pt]])
                    k.op(ACT, lambda: nc.scalar.copy(out=ycT[yi][:].rearrange("p h q -> p (h q)"), in_=pview[:, 0:512]),
                         reads=[b_PS[pt]], writes=[b_ycT[yi]])
                    k.dma(SP, [(yT_d.ap()[1536:2048, cs].rearrange("(h e) q -> e h q", e=128), ycT[yi][:])], b_ycT[yi],
                          reads=[b_ycT[yi]])
                k.barrier()
            if stop == "C" and l == nlayers - 1:
                break

            with ExitStack() as st2:
                AqT = [sb(f"AqT{j}", [128, S], BF16, st2) for j in range(6)]
                AkT = [sb(f"AkT{j}", [128, S], BF16, st2) for j in range(6)]
                Av = sb("Av", [128, 16, 768], BF16, st2)
                b_AqT = [Buf() for _ in range(6)]
                b_AkT = [Buf() for _ in range(6)]
                b_Av = Buf()
                proj_fm_cols(O_AQ, 6, 0.125, AqT, b_AqT)
                proj_fm_cols(O_AK, 6, 1.0, AkT, b_AkT)

                def mk_evac_av(c0, ncol):
                    def ev(tt, p, b_p):
                        k.op(ACT, lambda: nc.scalar.copy(out=Av[:, tt, c0:c0 + ncol], in_=p[:, 0:ncol]), reads=[b_p], writes=[b_Av])
                    return ev
                proj_tm_cols(O_AV, 768, mk_evac_av)

                PT = [sb(f"PT{i}", [128, 512], BF16, st2) for i in range(4)]
                b_PT = [Buf() for _ in range(4)]
                fa = [sb(f"fa{i}", [128, 512], F32, st2) for i in range(4)]
                b_fa = [Buf() for _ in range(4)]
                sqb = sb("sqb", [128, 512], BF16, st2)
                b_sq = Buf()
                yst = [sb(f"yst{i}", [128, 512], BF16, st2) for i in range(2)]
                b_yst = [Buf(), Buf()]
                step = [0]
                blkc = 0
                for h in range(6):
                    for qb in range(4):
                        nkt = 4 * qb + 4
                        pend = []
                        q0 = qb * 512

                        def emit_pv(item):
                            kt, m, c0, pti = item
                            Ob, Sb = 2 * m, 2 * m + 1
                            k.mm([lambda: nc.tensor.matmul(PS[Ob][:, c0:512], lhsT=Av[:, kt, h * 128:(h + 1) * 128], rhs=PT[pti][:, c0:512],
                                                           start=(kt == 0), stop=(kt == nkt - 1)),
                                  lambda: nc.tensor.matmul(PS[Sb][:, c0:512], lhsT=onesb[:, 0, :], rhs=PT[pti][:, c0:512],
                                                           start=(kt == 0), stop=(kt == nkt - 1))],
                                 reads=[b_Av, b_PT[pti], b_const], writes=[b_PS[Ob], b_PS[Sb]])

                        for kt in range(nkt):
                            j0 = q0 - 128 * kt
                            c0 = max(0, -j0)
                            for m in range(2):
                                sci = 4 + step[0] % 4
                                pti = step[0] % 4
                                step[0] += 1
                                pr = slice(64 * m, 64 * m + 64)
                                fns = [lambda: nc.tensor.matmul(PS[sci][:, c0:512], lhsT=AkT[h][pr, kt * 128:(kt + 1) * 128],
                                                                rhs=AqT[h][pr, q0 + c0:q0 + 512], start=True, stop=(j0 >= 256))]
                                if j0 < 256:
                                    jA = max(j0, 0)
                                    cA_, cB_ = jA - j0, min(512, 256 - j0)
                                    jB = cB_ + j0
                                    fns.append(lambda: nc.tensor.matmul(PS[sci][:, cA_:cB_], lhsT=identb[:], rhs=TA[:, h, jA:jB],
                                                                        start=False, stop=True))
                                k.mm(fns, reads=[b_AkT[h], b_AqT[h], b_const], writes=[b_PS[sci]])
                                k.op(ACT, lambda: nc.scalar.activation(out=PT[pti][:, c0:512], in_=PS[sci][:, c0:512], func=AF.Exp,
                                                                       bias=cA[:, h:h + 1], scale=1.0),
                                     reads=[b_PS[sci], b_const], writes=[b_PT[pti]])
                                pend.append((kt, m, c0, pti))
                                if len(pend) > 2:
                                    emit_pv(pend.pop(0))
                        while pend:
                            emit_pv(pend.pop(0))
                        k.op(DVE, lambda: nc.vector.reciprocal(out=fa[0][:], in_=PS[1][:]), reads=[b_PS[1]], writes=[b_fa[0]])
                        k.op(DVE, lambda: nc.vector.tensor_tensor(out=fa[0][:], in0=fa[0][:], in1=PS[0][:], op=ALU.mult),
                             reads=[b_PS[0]], writes=[b_fa[0]])
                        k.op(DVE, lambda: nc.vector.reciprocal(out=fa[1][:], in_=PS[3][:]), reads=[b_PS[3]], writes=[b_fa[1]])
                        k.op(DVE, lambda: nc.vector.tensor_tensor(out=fa[1][:], in0=fa[1][:], in1=PS[2][:], op=ALU.mult),
                             reads=[b_PS[2]], writes=[b_fa[1]])
                        k.op(DVE, lambda: nc.vector.scalar_tensor_tensor(out=fa[2][:], in0=fa[1][:], scalar=neglam, in1=fa[0][:],
                                                                         op0=ALU.mult, op1=ALU.add),
                             reads=[b_fa[0], b_fa[1], b_par], writes=[b_fa[2]])
                        k.op(DVE, lambda: nc.vector.tensor_tensor(out=sqb[:], in0=fa[2][:], in1=fa[2][:], op=ALU.mult),
                             reads=[b_fa[2]], writes=[b_sq])
                        sci = 4 + step[0] % 4
                        step[0] += 1
                        k.mm([lambda: nc.tensor.matmul(PS[sci][:], lhsT=onesb[:, 0, :], rhs=sqb[:], start=True, stop=True)],
                             reads=[b_sq, b_const], writes=[b_PS[sci]])
                        k.op(ACT, lambda: nc.scalar.activation(out=fa[3][:], in_=PS[sci][:], func=AF.Ln, bias=epsc[:, 1:2], scale=1.0),
                             reads=[b_PS[sci], b_const], writes=[b_fa[3]])
                        k.op(ACT, lambda: nc.scalar.activation(out=fa[3][:], in_=fa[3][:], func=AF.Exp, scale=-0.5),
                             reads=[], writes=[b_fa[3]])
                        yi = blkc % 2
                        blkc += 1
                        k.op(DVE, lambda: nc.vector.scalar_tensor_tensor(out=yst[yi][:], in0=fa[2][:], scalar=gsub[:, 1:2], in1=fa[3][:],
                                                                         op0=ALU.mult, op1=ALU.mult),
                             reads=[b_fa[2], b_fa[3], b_par], writes=[b_yst[yi]])
                        k.dma(SP, [(yT_d.ap()[h * 128:(h + 1) * 128, q0:q0 + 512], yst[yi][:])], b_yst[yi], reads=[b_yst[yi]])
                k.barrier()
            if stop == "A" and l == nlayers - 1:
                break

            with ExitStack() as st2:
                BqT = [sb(f"BqT{j}", [128, S], BF16, st2) for j in range(6)]
                BkT = [sb(f"BkT{j}", [128, S], BF16, st2) for j in range(3)]
                Bvp = sb("Bvp", [128, 16, 3, 2, 128], BF16, st2)
                b_BqT = [Buf() for _ in range(6)]
                b_BkT = [Buf() for _ in range(3)]
                b_Bvp = Buf()
                k.op(DVE, lambda: nc.vector.memset(Bvp[:].rearrange("p a b c d -> p (a b c d)"), 0.0), writes=[b_Bvp])
                proj_fm_cols(O_BQ, 6, 0.125, BqT, b_BqT)
                proj_fm([(0, O_BK, 64), (64, O_BK, 64), (128, O_BK + 64, 64), (192, O_BK + 64, 64)], 2, 1.0, BkT[0:2], b_BkT[0:2])
                proj_fm([(0, O_BK + 128, 64), (64, O_BK + 128, 64)], 1, 1.0, BkT[2:3], b_BkT[2:3])

                def evac_bv(tt, p, b_p):
                    src = p[:, 0:192].rearrange("p (g e) -> p g e", g=3)
                    k.op(ACT, lambda: nc.scalar.copy(out=Bvp[:, tt, :, 0, 0:64], in_=src), reads=[b_p], writes=[b_Bvp])
                    k.op(DVE, lambda: nc.vector.tensor_copy(out=Bvp[:, tt, :, 1, 64:128], in_=src), reads=[b_p], writes=[b_Bvp])
                proj_tm(O_BV, 192, evac_bv)

                PT = [sb(f"PTb{i}", [128, 256], BF16, st2) for i in range(4)]
                b_PT = [Buf() for _ in range(4)]
                fb_ = [sb(f"fb{i}", [128, 512], F32, st2) for i in range(2)]
                b_fb = [Buf(), Buf()]
                yst = [sb(f"ystb{i}", [128, 512], BF16, st2) for i in range(2)]
                b_yst = [Buf(), Buf()]
                step = [0]
                blkc = 0
                for i in range(6):
                    g = i // 2
                    for qb in range(4):
                        q0 = qb * 512
                        kts = list(range(max(0, 4 * qb - 1), 4 * qb + 4))
                        for kt in kts:
                            cs_ = max(0, 128 * kt - q0)
                            ce_ = min(512, 128 * kt + 256 - q0)
                            N = ce_ - cs_
                            jA = q0 + cs_ - 128 * kt
                            for par in range(2):
                                pr = slice(64 * par, 64 * par + 64)
                                sci = 4 + step[0] % 4
                                pti = step[0] % 4
                                step[0] += 1
                                hb = 2 * i + par
                                k.mm([lambda: nc.tensor.matmul(PS[sci][:, 0:N], lhsT=BkT[g][pr, kt * 128:(kt + 1) * 128],
                                                               rhs=BqT[i][pr, q0 + cs_:q0 + ce_], start=True, stop=False),
                                      lambda: nc.tensor.matmul(PS[sci][:, 0:N], lhsT=identb[:], rhs=TB[:, hb, jA:jA + N],
                                                               start=False, stop=True)],
                                     reads=[b_BkT[g], b_BqT[i], b_const], writes=[b_PS[sci]])
                                k.op(ACT, lambda: nc.scalar.activation(out=PT[pti][:, 0:N], in_=PS[sci][:, 0:N], func=AF.Exp),
                                     reads=[b_PS[sci]], writes=[b_PT[pti]])
                                fns = []
                                wr = set()
                                for sub in range(N // 128):
                                    cc = cs_ + sub * 128
                                    qt = (q0 + cc) // 128
                                    qtl = cc // 128
                                    bank = qtl % 2
                                    col = (qtl // 2) * 128
                                    first = (kt == max(0, qt - 1)) and par == 0
                                    last = (kt == qt) and par == 1
                                    fns.append(lambda bank=bank, col=col, sub=sub, first=first, last=last:
                                               nc.tensor.matmul(PS[bank][:, col:col + 128], lhsT=Bvp[:, kt, g, par, :],
                                                                rhs=PT[pti][:, sub * 128:(sub + 1) * 128], start=first, stop=last))
                                    fns.append(lambda bank=bank, col=col, sub=sub, first=first, last=last:
                                               nc.tensor.matmul(PS[2 + bank][:, col:col + 128], lhsT=onesb[:, 1 + par, :],
                                                                rhs=PT[pti][:, sub * 128:(sub + 1) * 128], start=first, stop=last))
                                    wr.add(bank)
                                    wr.add(2 + bank)
                                k.mm(fns, reads=[b_Bvp, b_PT[pti], b_const], writes=[b_PS[w] for w in sorted(wr)])
                        yi = blkc % 2
                        blkc += 1
                        for bank in range(2):
                            dstv = fb_[0][:].rearrange("p (a b c) -> p a b c", a=2, b=2)[:, :, bank, :]
                            k.op(DVE, lambda: nc.vector.tensor_scalar(out=dstv, in0=PS[2 + bank][:, 0:256].rearrange("p (a c) -> p a c", a=2),
                                                                      scalar1=esink[:, i:i + 1], scalar2=None, op0=ALU.add),
                                 reads=[b_PS[2 + bank], b_par], writes=[b_fb[0]])
                        k.op(DVE, lambda: nc.vector.reciprocal(out=fb_[1][:], in_=fb_[0][:]), reads=[b_fb[0]], writes=[b_fb[1]])
                        for bank in range(2):
                            dstv = yst[yi][:].rearrange("p (a b c) -> p a b c", a=2, b=2)[:, :, bank, :]
                            rv = fb_[1][:].rearrange("p (a b c) -> p a b c", a=2, b=2)[:, :, bank, :]
                            k.op(DVE, lambda: nc.vector.tensor_tensor(out=dstv, in0=PS[bank][:, 0:256].rearrange("p (a c) -> p a c", a=2),
                                                                      in1=rv, op=ALU.mult),
                                 reads=[b_PS[bank], b_fb[1]], writes=[b_yst[yi]])
                        k.dma(SP, [(yT_d.ap()[768 + i * 128:768 + (i + 1) * 128, q0:q0 + 512], yst[yi][:])], b_yst[yi],
                              reads=[b_yst[yi]])
                k.barrier()
        if stop == "B" and l == nlayers - 1:
            break

        is_moe = (l % 2 == 1)
        with ExitStack() as st:
            Wo = sb("Wo", [128, 16, D], BF16, st)
            b_Wo = Buf()
            wsrc = w_out.ap()[l].rearrange("(c p) f -> p c f", p=128)
            k.dma(POOL, [(Wo[:, :, j * 512:(j + 1) * 512], wsrc[:, :, j * 512:(j + 1) * 512]) for j in range(4)], b_Wo, writes=[b_Wo])
            gb = sb("gb", [128, D], F32, st)
            bb = sb("bb", [128, D], F32, st)
            b_gb = Buf()
            k.dma(SP, [(gb[:], bcast_row(ln_mix_g, l * D, D)), (bb[:], bcast_row(ln_mix_b, l * D, D))], b_gb, writes=[b_gb])
            yt = [sb(f"yt{i}", [128, 16, 128], BF16, st) for i in range(2)]
            xr = [sb(f"xr{i}", [128, D], F32, st) for i in range(2)]
            rr = [sb(f"rr{i}", [128, D], F32, st) for i in range(2)]
            x1 = [sb(f"x1{i}", [128, D], F32, st) for i in range(2)]
            x1Tb = [sb(f"x1Tb{i}", [128, 16, 128], BF16, st) for i in range(2)]
            stats = sb("stats", [128, 4, 6], F32, st)
            mv = sb("mv", [128, 2], F32, st)
            sc2 = sb("sc2", [128, 2], F32, st)
            b_yt, b_xr, b_rr, b_x1, b_x1Tb = [Buf(), Buf()], [Buf(), Buf()], [Buf(), Buf()], [Buf(), Buf()], [Buf(), Buf()]
            b_small = Buf()
            PS = [ps(f"psB{i}", [128, 512], F32, st) for i in range(8)]
            b_PS = [Buf() for _ in range(8)]
            if is_moe:
                Wr = sb("Wr", [128, 16, NE], F32, st)
                Wrh = sb("Wrh", [128, 16, NE], BF16, st)
                Wrl = sb("Wrl", [128, 16, NE], BF16, st)
                x1Tf = sb("x1Tl", [128, 16, 128], BF16, st)
                gl = sb("gl", [128, 6, NE], F32, st)
                gsm = sb("gsm", [128, 4], F32, st)
                SELb = sb("SELb", [128, NE], BF16, st)
                rk = sb("rk", [128, NE], F32, st)
                tmp8 = sb("tmp8", [128, NE], F32, st)
                sfl = sb("sfl", [128, 2], F32, st)
                cnti = sb("cnti", [128, NE], mybir.dt.int32, st)
                x1bf = [sb(f"x1bf{i}", [128, D], BF16, st) for i in range(2)]
                b_x1bf = [Buf(), Buf()]
                b_idx = Buf()
                b_Wr, b_x1Tf, b_gl = Buf(), Buf(), Buf()
                k.op(DVE, lambda: nc.vector.memset(carry[:], 0.0), writes=[b_gT])
                k.dma(SP, [(Wr[:], moe_router.ap()[0].rearrange("(c p) e -> p c e", p=128))], b_Wr, writes=[b_Wr])
                k.op(DVE, lambda: nc.vector.tensor_copy(out=Wrh[:], in_=Wr[:]), reads=[b_Wr], writes=[b_Wr])
                k.op(DVE, lambda: nc.vector.tensor_tensor(out=Wrl[:], in0=Wr[:], in1=Wrh[:], op=ALU.subtract), reads=[b_Wr], writes=[b_Wr])
            ysrc = yT_d.ap().rearrange("(c p) t -> p c t", p=128)
            x1Tdst = x1T_d.ap().rearrange("(c p) t -> p c t", p=128)
            for tt in range(16):
                i = tt % 2
                ts_ = slice(tt * 128, (tt + 1) * 128)
                k.dma(SP, [(yt[i][:], ysrc[:, :, ts_])], b_yt[i], writes=[b_yt[i]])
                k.dma(SP, [(xr[i][:], x_src.ap()[ts_, :])], b_xr[i], writes=[b_xr[i]])
                for cg in range(4):
                    k.mm([(lambda c=c: nc.tensor.matmul(PS[cg][:], lhsT=yt[i][:, c, :], rhs=Wo[:, c, cg * 512:(cg + 1) * 512],
                                                        start=(c == 0), stop=(c == 15))) for c in range(16)],
                         reads=[b_yt[i], b_Wo], writes=[b_PS[cg]])
                    k.op(DVE, lambda: nc.vector.scalar_tensor_tensor(out=rr[i][:, cg * 512:(cg + 1) * 512], in0=xr[i][:, cg * 512:(cg + 1) * 512],
                                                                     scalar=float(ALPHA), in1=PS[cg][:], op0=ALU.mult, op1=ALU.add),
                         reads=[b_xr[i], b_PS[cg]], writes=[b_rr[i]])
                layer_norm_tile("mix", rr[i], b_rr[i], stats, mv, sc2, b_small, gb, bb, b_gb, x1[i][:], b_x1[i])
                k.dma(SP, [(x1_d.ap()[ts_, :], x1[i][:])], b_x1[i], reads=[b_x1[i]])
                for cg in range(4):
                    pb = 4 + cg
                    k.mm([(lambda c=c: nc.tensor.transpose(PS[pb][:, (c % 4) * 128:(c % 4 + 1) * 128],
                                                           x1[i][:, c * 128:(c + 1) * 128], identf[:]))
                          for c in range(cg * 4, cg * 4 + 4)], reads=[b_x1[i], b_const], writes=[b_PS[pb]])
                    src = PS[pb][:].rearrange("p (c t) -> p c t", c=4)
                    k.op(ACT, lambda: nc.scalar.copy(out=x1Tb[i][:, cg * 4:cg * 4 + 4, :], in_=src), reads=[b_PS[pb]], writes=[b_x1Tb[i]])
                    if is_moe:
                        k.op(DVE, lambda: nc.vector.tensor_tensor(out=x1Tf[:, cg * 4:cg * 4 + 4, :], in0=src, in1=x1Tb[i][:, cg * 4:cg * 4 + 4, :],
                                                                  op=ALU.subtract), reads=[b_PS[pb], b_x1Tb[i]], writes=[b_x1Tf])
                k.dma(SP, [(x1Tdst[:, :, ts_], x1Tb[i][:])], b_x1Tb[i], reads=[b_x1Tb[i]])
                if is_moe:
                    rfn = []
                    for (xa, wa) in ((x1Tb[i], Wrh), (x1Tf, Wrh), (x1Tb[i], Wrl)):
                        for c in range(16):
                            rfn.append(lambda c=c, xa=xa, wa=wa: nc.tensor.matmul(PS[0][:, 0:NE], lhsT=xa[:, c, :], rhs=wa[:, c, :],
                                                                                  start=(len(rfn_i) == 0), stop=(len(rfn_i) == 47)))
                    rfn_i = []

                    def _run(f):
                        r = f()
                        rfn_i.append(1)
                        return r
                    k.mm([(lambda f=f: _run(f)) for f in rfn], reads=[b_x1Tf, b_x1Tb[i], b_Wr], writes=[b_PS[0]])
                    L, L2, SELm, Wg_, G = gl[:, 0, :], gl[:, 1, :], gl[:, 2, :], gl[:, 3, :], gl[:, 4, :]
                    AX = mybir.AxisListType.X
                    ops = [
                        (DVE, lambda: nc.vector.tensor_copy(out=L, in_=PS[0][:, 0:NE]), [b_PS[0]]),
                        (DVE, lambda: nc.vector.reduce_max(out=gsm[:, 0:1], in_=L, axis=AX), []),
                        (DVE, lambda: nc.vector.tensor_scalar(out=L2, in0=L, scalar1=gsm[:, 0:1], scalar2=-1e30, op0=ALU.is_equal, op1=ALU.mult), []),
                        (DVE, lambda: nc.vector.tensor_tensor(out=L2, in0=L2, in1=L, op=ALU.add), []),
                        (DVE, lambda: nc.vector.reduce_max(out=gsm[:, 1:2], in_=L2, axis=AX), []),
                        (DVE, lambda: nc.vector.tensor_scalar(out=SELm, in0=L, scalar1=gsm[:, 1:2], scalar2=None, op0=ALU.is_ge), []),
                        (DVE, lambda: nc.vector.tensor_scalar(out=gsm[:, 2:3], in0=gsm[:, 0:1], scalar1=-1.0, scalar2=None, op0=ALU.mult), []),
                        (ACT, lambda: nc.scalar.activation(out=Wg_, in_=L, func=AF.Exp, bias=gsm[:, 2:3], scale=1.0), []),
                        (DVE, lambda: nc.vector.tensor_tensor(out=Wg_, in0=Wg_, in1=SELm, op=ALU.mult), []),
                        (DVE, lambda: nc.vector.reduce_sum(out=gsm[:, 3:4], in_=Wg_, axis=AX), []),
                        (DVE, lambda: nc.vector.reciprocal(out=gsm[:, 3:4], in_=gsm[:, 3:4]), []),
                        (DVE, lambda: nc.vector.tensor_scalar(out=G, in0=Wg_, scalar1=gsm[:, 3:4], scalar2=None, op0=ALU.mult), []),
                    ]
                    for (E, f, rd) in ops:
                        k.op(E, f, reads=rd + [b_gl], writes=[b_gl])
                    eq1, eq2 = gl[:, 5, :], gl[:, 1, :]
                    for f in (lambda: nc.vector.tensor_scalar(out=eq1, in0=L, scalar1=gsm[:, 0:1], scalar2=None, op0=ALU.is_equal),
                              lambda: nc.vector.tensor_tensor(out=eq2, in0=SELm, in1=eq1, op=ALU.subtract),
                              lambda: nc.vector.tensor_copy(out=SELb[:], in_=SELm)):
                        k.op(DVE, f, reads=[b_gl], writes=[b_gl])
                    k.mm([lambda: nc.tensor.matmul(PS[1][:, 0:NE], lhsT=ust[:], rhs=SELb[:], start=True, stop=True),
                          lambda: nc.tensor.matmul(PS[1][:, NE:2 * NE], lhsT=onesb[:, 0, :], rhs=SELb[:], start=True, stop=True)],
                         reads=[b_gl, b_const], writes=[b_PS[1]])
                    for f in (lambda: nc.vector.tensor_tensor(out=rk[:], in0=PS[1][:, 0:NE], in1=carry[:], op=ALU.add),
                              lambda: nc.vector.tensor_tensor(out=rk[:], in0=rk[:], in1=ebase[:], op=ALU.add),
                              lambda: nc.vector.tensor_tensor(out=carry[:], in0=carry[:], in1=PS[1][:, NE:2 * NE], op=ALU.add),
                              lambda: nc.vector.tensor_tensor(out=tmp8[:], in0=eq1, in1=rk[:], op=ALU.mult),
                              lambda: nc.vector.reduce_sum(out=sfl[:, 0:1], in_=tmp8[:], axis=AX),
                              lambda: nc.vector.tensor_tensor(out=tmp8[:], in0=eq2, in1=rk[:], op=ALU.mult),
                              lambda: nc.vector.reduce_sum(out=sfl[:, 1:2], in_=tmp8[:], axis=AX),
                              lambda: nc.vector.tensor_tensor(out=tmp8[:], in0=eq1, in1=G, op=ALU.mult),
                              lambda: nc.vector.reduce_sum(out=gsel[:, tt, 0:1], in_=tmp8[:], axis=AX),
                              lambda: nc.vector.tensor_tensor(out=tmp8[:], in0=eq2, in1=G, op=ALU.mult),
                              lambda: nc.vector.reduce_sum(out=gsel[:, tt, 1:2], in_=tmp8[:], axis=AX)):
                        k.op(DVE, f, reads=[b_gl, b_PS[1], b_const], writes=[b_gl, b_gT])
                    k.op(DVE, lambda: nc.vector.tensor_copy(out=idxs[:, tt, :], in_=sfl[:]), reads=[b_gl], writes=[b_idx])
                    k.op(ACT, lambda: nc.scalar.copy(out=x1bf[i][:], in_=x1[i][:]), reads=[b_x1[i]], writes=[b_x1bf[i]])
                    k.dma(POOL, [(lambda kk_=kk_: nc.gpsimd.indirect_dma_start(
                        out=xg_d.ap(), out_offset=bass.IndirectOffsetOnAxis(idxs[:, tt, kk_:kk_ + 1], 0),
                        in_=x1bf[i][:], in_offset=None)) for kk_ in range(2)],
                          b_x1bf[i], reads=[b_x1bf[i], b_idx])
            if is_moe:
                k.op(DVE, lambda: nc.vector.tensor_copy(out=cnti[:], in_=carry[:]), reads=[b_gT, b_gl], writes=[b_gT])
                k.dma(SP, [(cnt_d.ap(), cnti[0:1, :])], b_gT, reads=[b_gT])
                if dbg:
                    k.dma(SP, [(idx_dbg.ap(), idxs[:]), (gsel_dbg.ap(), gsel[:])], b_gT, reads=[b_gT, b_idx])
            k.barrier()
        if stop == "O" and l == nlayers - 1:
            break

        def ffn_unit(xb, b_xb, hT, b_hT, WG, WU, WD, b_WG, b_WU, b_WD, sgt, b_sgt, PS, b_PS, wcnt, pcnt,
                     wg_src, wu_src, wd_src, down_evac, NS):
            rem = NS - 512
            for fg in range(NFB // 2):
                wi = wcnt[0] % 2
                wcnt[0] += 1
                k.dma(POOL, [(WG[wi][:], wg_src[:, :, fg * 256:(fg + 1) * 256])], b_WG[wi], writes=[b_WG[wi]])
                k.dma(POOL, [(WU[wi][:], wu_src[:, :, fg * 256:(fg + 1) * 256])], b_WU[wi], writes=[b_WU[wi]])
                for j in range(2):
                    fbk = fg * 2 + j
                    if rem:
                        pg, pu, pr = (2, 3, 4) if pcnt[0] % 2 == 0 else (5, 6, 7)
                    else:
                        pg = (pcnt[0] * 2) % 6
                        pu, pr = pg + 1, None
                    pcnt[0] += 1
                    for (Wt, b_Wt, pm, roff) in ((WG[wi], b_WG[wi], pg, 0), (WU[wi], b_WU[wi], pu, 128)):
                        fns = []
                        for c in range(16):
                            fns.append(lambda c=c, Wt=Wt, pm=pm: nc.tensor.matmul(PS[pm][:], lhsT=Wt[:, c, j * 128:(j + 1) * 128], rhs=xb[:, c, 0:512],
                                                                                 start=(c == 0), stop=(c == 15)))
                        for c in range(16):
                            if rem:
                                fns.append(lambda c=c, Wt=Wt, roff=roff: nc.tensor.matmul(PS[pr][:, roff:roff + rem], lhsT=Wt[:, c, j * 128:(j + 1) * 128],
                                                                                       rhs=xb[:, c, 512:NS], start=(c == 0), stop=(c == 15)))
                        k.mm(fns, reads=[b_Wt, b_xb], writes=[b_PS[pm]] + ([b_PS[pr]] if rem else []))
                    si = fbk % 2
                    k.op(ACT, lambda: nc.scalar.activation(out=sgt[si][:, 0:512], in_=PS[pg][:], func=AF.Silu), reads=[b_PS[pg]], writes=[b_sgt[si]])
                    k.op(DVE, lambda: nc.vector.tensor_tensor(out=hT[:, fbk, 0:512], in0=sgt[si][:, 0:512], in1=PS[pu][:], op=ALU.mult),
                         reads=[b_sgt[si], b_PS[pu]], writes=[b_hT[fbk]])
                    if rem:
                        k.op(ACT, lambda: nc.scalar.activation(out=sgt[si][:, 512:NS], in_=PS[pr][:, 0:rem], func=AF.Silu),
                             reads=[b_PS[pr]], writes=[b_sgt[si]])
                        k.op(DVE, lambda: nc.vector.tensor_tensor(out=hT[:, fbk, 512:NS], in0=sgt[si][:, 512:NS], in1=PS[pr][:, 128:128 + rem], op=ALU.mult),
                             reads=[b_sgt[si], b_PS[pr]], writes=[b_hT[fbk]])
            for dc in range(16):
                wis = []
                for hf in range(2):
                    wi = wcnt[1] % 3
                    wcnt[1] += 1
                    wis.append(wi)
                    k.dma(POOL, [(WD[wi][:], wd_src[:, hf * 22:(hf + 1) * 22, dc * 128:(dc + 1) * 128])], b_WD[wi], writes=[b_WD[wi]])
                if rem:
                    po, pq = dc % 2, 2 + dc % 2
                else:
                    po, pq = 6 + dc % 2, None
                fns = []
                for f in range(NFB):
                    fns.append(lambda f=f: nc.tensor.matmul(PS[po][:], lhsT=WD[wis[f // 22]][:, f % 22, :], rhs=hT[:, f, 0:512],
                                                            start=(f == 0), stop=(f == NFB - 1)))
                for f in range(NFB):
                    if rem:
                        fns.append(lambda f=f: nc.tensor.matmul(PS[pq][:, 0:rem], lhsT=WD[wis[f // 22]][:, f % 22, :], rhs=hT[:, f, 512:NS],
                                                                start=(f == 0), stop=(f == NFB - 1)))
                k.mm(fns, reads=[b_WD[wis[0]], b_WD[wis[1]]] + b_hT, writes=[b_PS[po]] + ([b_PS[pq]] if rem else []))
                down_evac(dc, PS[po], b_PS[po], PS[pq] if rem else None, b_PS[pq] if rem else None)

        if not is_moe:
            with ExitStack() as st:
                xb = sb("xb", [128, 16, 512], BF16, st)
                hT = sb("hT", [128, NFB, 512], BF16, st)
                acc = sb("acc", [128, 16, 512], F32, st)
                WG = [sb(f"wg{i}", [128, 16, 256], BF16, st) for i in range(2)]
                WU = [sb(f"wu{i}", [128, 16, 256], BF16, st) for i in range(2)]
                WD = [sb(f"wd{i}", [128, NFB // 2, 128], BF16, st) for i in range(3)]
                sgt = [sb(f"sgt{i}", [128, 512], BF16, st) for i in range(2)]
                gb = sb("gb2", [128, D], F32, st)
                bb = sb("bb2", [128, D], F32, st)
                xr = sb("xr2", [128, D], F32, st)
                rr = sb("rr2", [128, D], F32, st)
                stats = sb("stats2", [128, 4, 6], F32, st)
                mv = sb("mv2", [128, 2], F32, st)
                sc2 = sb("sc22", [128, 2], F32, st)
                b_xb, b_hT, b_acc = Buf(), [Buf() for _ in range(NFB)], [Buf() for _ in range(16)]
                b_WG, b_WU, b_WD, b_sgt = [Buf(), Buf()], [Buf(), Buf()], [Buf(), Buf(), Buf()], [Buf(), Buf()]
                b_gb, b_xr, b_rr, b_small = Buf(), Buf(), Buf(), Buf()
                PS = [ps(f"psC{i}", [128, 512], F32, st) for i in range(8)]
                b_PS = [Buf() for _ in range(8)]
                k.dma(SP, [(gb[:], bcast_row(ln_ffn_g, l * D, D)), (bb[:], bcast_row(ln_ffn_b, l * D, D))], b_gb, writes=[b_gb])
                x1Tsrc = x1T_d.ap().rearrange("(c p) t -> p c t", p=128)
                wcnt = [0, 0]
                pcnt = [0]
                wg_src = dense_w_gate.ap()[0].rearrange("(c p) f -> p c f", p=128)
                wu_src = dense_w_up.ap()[0].rearrange("(c p) f -> p c f", p=128)
                wd_src = dense_w_down.ap()[0].rearrange("(c p) n -> p c n", p=128)
                for tb in range(4):
                    k.dma(SP, [(xb[:], x1Tsrc[:, :, tb * 512:(tb + 1) * 512])], b_xb, writes=[b_xb])

                    def evac_dense(dc, p, b_p, p2, b_p2):
                        k.op(ACT, lambda: nc.scalar.copy(out=acc[:, dc, :], in_=p[:]), reads=[b_p], writes=[b_acc[dc]])
                    ffn_unit(xb, b_xb, hT, b_hT, WG, WU, WD, b_WG, b_WU, b_WD, sgt, b_sgt, PS, b_PS, wcnt, pcnt,
                             wg_src, wu_src, wd_src, evac_dense, 512)
                    for t4 in range(4):
                        tt = tb * 4 + t4
                        ts_ = slice(tt * 128, (tt + 1) * 128)
                        k.dma(SP, [(xr[:], x1_d.ap()[ts_, :])], b_xr, writes=[b_xr])
                        for cg in range(4):
                            pb = cg
                            k.mm([(lambda c=c: nc.tensor.transpose(PS[pb][:, (c % 4) * 128:(c % 4 + 1) * 128],
                                                                   acc[:, c, t4 * 128:(t4 + 1) * 128], identf[:]))
                                  for c in range(cg * 4, cg * 4 + 4)], reads=b_acc[cg * 4:cg * 4 + 4] + [b_const], writes=[b_PS[pb]])
                            k.op(DVE, lambda: nc.vector.scalar_tensor_tensor(out=rr[:, cg * 512:(cg + 1) * 512], in0=xr[:, cg * 512:(cg + 1) * 512],
                                                                             scalar=float(ALPHA), in1=PS[pb][:], op0=ALU.mult, op1=ALU.add),
                                 reads=[b_xr, b_PS[pb]], writes=[b_rr])
                        layer_norm_tile("ffn", rr, b_rr, stats, mv, sc2, b_small, gb, bb, b_gb, rr[:], b_rr)
                        k.dma(SP, [(x_dst.ap()[ts_, :], rr[:])], b_rr, reads=[b_rr])
                k.barrier()
        else:
            with ExitStack() as st:
                NS, NT, EST = 640, 5, 2560
                xgr = sb("xgr", [128, NT, D], BF16, st)
                xb = sb("xbm", [128, 16, NS], BF16, st)
                hT = sb("hTm", [128, NFB, NS], BF16, st)
                WG = [sb(f"wgm{i}", [128, 16, 256], BF16, st) for i in range(2)]
                WU = [sb(f"wum{i}", [128, 16, 256], BF16, st) for i in range(2)]
                WD = [sb(f"wdm{i}", [128, NFB // 2, 128], BF16, st) for i in range(3)]
                sgt = [sb(f"sgtm{i}", [128, NS], BF16, st) for i in range(2)]
                ysb = [sb(f"ysb{i}", [128, NS], F32, st) for i in range(2)]
                yes = sb("yes", [128, NT, D], F32, st)
                PS = [ps(f"psM{i}", [128, 512], F32, st) for i in range(8)]
                dscr = sb("dscr", [1, NE], mybir.dt.int32, st)
                cregs = [nc.alloc_registers(f"cnt{l}_{e}", mybir.ALL_ENGINES) for e in range(NE)]
                for e in range(NE):
                    for r in cregs[e]:
                        nc.reg_load(r, cnt_d.ap()[0:1, e:e + 1])
                for e in range(NE):
                    wg_src = moe_w_gate.ap()[0, e].rearrange("(c p) f -> p c f", p=128)
                    wu_src = moe_w_up.ap()[0, e].rearrange("(c p) f -> p c f", p=128)
                    wd_src = moe_w_down.ap()[0, e].rearrange("(c p) n -> p c n", p=128)
                    for j in range(4):
                        snap_ = k.snapshot()
                        with nc.If_cmp(cregs[e], NS * j, "IS_GT"):
                            b_xgr, b_xb, b_hT = Buf(), Buf(), [Buf() for _ in range(NFB)]
                            b_WG, b_WU, b_WD, b_sgt = [Buf(), Buf()], [Buf(), Buf()], [Buf(), Buf(), Buf()], [Buf(), Buf()]
                            b_ysb, b_yes = [Buf(), Buf()], Buf()
                            b_PS = [Buf() for _ in range(8)]
                            base = e * EST + NS * j
                            k.dma(SP, [(xgr[:], xg_d.ap()[base:base + NS, :].rearrange("(s p) d -> p s d", p=128))], b_xgr, writes=[b_xgr])
                            for c in range(16):
                                pb = c % 8
                                pv = PS[pb][:].bitcast(BF16)
                                k.mm([(lambda s_=s_: nc.tensor.transpose(pv[:, s_ * 128:(s_ + 1) * 128], xgr[:, s_, c * 128:(c + 1) * 128], identb[:]))
                                      for s_ in range(NT)], reads=[b_xgr, b_const], writes=[b_PS[pb]])
                                if c % 2 == 0:
                                    k.op(ACT, lambda: nc.scalar.copy(out=xb[:, c, :], in_=pv[:, 0:NS]), reads=[b_PS[pb]], writes=[b_xb])
                                else:
                                    k.op(DVE, lambda: nc.vector.tensor_copy(out=xb[:, c, :], in_=pv[:, 0:NS]), reads=[b_PS[pb]], writes=[b_xb])

                            def evac_moe(dc, p, b_p, p2, b_p2):
                                si = dc % 2
                                pt, pt2 = 4 + dc % 2, 6 + dc % 2
                                k.op(ACT, lambda: nc.scalar.copy(out=ysb[si][:, 0:512], in_=p[:]), reads=[b_p], writes=[b_ysb[si]])
                                k.op(ACT, lambda: nc.scalar.copy(out=ysb[si][:, 512:NS], in_=p2[:, 0:NS - 512]), reads=[b_p2], writes=[b_ysb[si]])
                                k.mm([(lambda s_=s_: nc.tensor.transpose(PS[pt][:, s_ * 128:(s_ + 1) * 128], ysb[si][:, s_ * 128:(s_ + 1) * 128], identf[:]))
                                      for s_ in range(4)] +
                                     [lambda: nc.tensor.transpose(PS[pt2][:, 0:128], ysb[si][:, 512:NS], identf[:])],
                                     reads=[b_ysb[si], b_const], writes=[b_PS[pt], b_PS[pt2]])
                                k.op(DVE, lambda: nc.vector.tensor_copy(out=yes[:, 0:4, dc * 128:(dc + 1) * 128],
                                                                        in_=PS[pt][:].rearrange("p (s c) -> p s c", s=4)),
                                     reads=[b_PS[pt]], writes=[b_yes])
                                k.op(DVE, lambda: nc.vector.tensor_copy(out=yes[:, 4, dc * 128:(dc + 1) * 128], in_=PS[pt2][:, 0:128]),
                                     reads=[b_PS[pt2]], writes=[b_yes])
                            ffn_unit(xb, b_xb, hT, b_hT, WG, WU, WD, b_WG, b_WU, b_WD, sgt, b_sgt, PS, b_PS, [0, 0], [0],
                                     wg_src, wu_src, wd_src, evac_moe, NS)
                            k.dma(SP, [(ye_d.ap()[base:base + NS, :].rearrange("(s p) d -> p s d", p=128), yes[:])], b_yes, reads=[b_yes])
                            k.barrier()
                        with nc.Else():
                            k.compensate(snap_, dscr[0:1, :], cnt_d.ap())
            with ExitStack() as st:
                gb = sb("gb3", [128, D], F32, st)
                bb = sb("bb3", [128, D], F32, st)
                r1 = [sb(f"r1_{i}", [128, D], F32, st) for i in range(2)]
                r2 = [sb(f"r2_{i}", [128, D], F32, st) for i in range(2)]
                xr = [sb(f"xr3_{i}", [128, D], F32, st) for i in range(2)]
                stats = sb("stats3", [128, 4, 6], F32, st)
                mv = sb("mv3", [128, 2], F32, st)
                sc2 = sb("sc23", [128, 2], F32, st)
                b_gb, b_small = Buf(), Buf()
                b_r1, b_r2, b_xr = [Buf(), Buf()], [Buf(), Buf()], [Buf(), Buf()]
                k.dma(SP, [(gb[:], bcast_row(ln_ffn_g, l * D, D)), (bb[:], bcast_row(ln_ffn_b, l * D, D))], b_gb, writes=[b_gb])
                for tt in range(16):
                    i = tt % 2
                    ts_ = slice(tt * 128, (tt + 1) * 128)
                    k.dma(POOL, [lambda: nc.gpsimd.indirect_dma_start(out=r1[i][:], out_offset=None, in_=ye_d.ap(),
                                                                      in_offset=bass.IndirectOffsetOnAxis(idxs[:, tt, 0:1], 0))],
                          b_r1[i], writes=[b_r1[i]])
                    k.dma(POOL, [lambda: nc.gpsimd.indirect_dma_start(out=r2[i][:], out_offset=None, in_=ye_d.ap(),
                                                                      in_offset=bass.IndirectOffsetOnAxis(idxs[:, tt, 1:2], 0))],
                          b_r2[i], writes=[b_r2[i]])
                    k.dma(SP, [(xr[i][:], x1_d.ap()[ts_, :])], b_xr[i], writes=[b_xr[i]])
                    k.op(DVE, lambda: nc.vector.tensor_scalar(out=r1[i][:], in0=r1[i][:], scalar1=gsel[:, tt, 0:1], scalar2=None, op0=ALU.mult),
                         reads=[], writes=[b_r1[i]])
                    k.op(DVE, lambda: nc.vector.scalar_tensor_tensor(out=r1[i][:], in0=r2[i][:], scalar=gsel[:, tt, 1:2], in1=r1[i][:],
                                                                     op0=ALU.mult, op1=ALU.add), reads=[b_r2[i]], writes=[b_r1[i]])
                    k.op(DVE, lambda: nc.vector.scalar_tensor_tensor(out=r1[i][:], in0=xr[i][:], scalar=float(ALPHA), in1=r1[i][:],
                                                                     op0=ALU.mult, op1=ALU.add), reads=[b_xr[i]], writes=[b_r1[i]])
                    layer_norm_tile("ffn", r1[i], b_r1[i], stats, mv, sc2, b_small, gb, bb, b_gb, r1[i][:], b_r1[i])
                    k.dma(SP, [(x_dst.ap()[ts_, :], r1[i][:])], b_r1[i], reads=[b_r1[i]])
                k.barrier()

    k.barrier()
    stack.close()
    return nc, consts


_CACHE = {}


def kernel(**inputs):
    n = 8
    if "prog" not in _CACHE:
        _CACHE["prog"] = build_program()
    nc, consts = _CACHE["prog"]
    shared = {kk: np.ascontiguousarray(v) for kk, v in inputs.items() if kk != "x"}
    for kk, v in consts.items():
        if kk == "c_gchunk":
            continue
        shared[kk] = v
    x = np.ascontiguousarray(inputs["x"])
    in_maps = []
    for b in range(n):
        m = dict(shared)
        m["x"] = x[b]
        in_maps.append(m)
    res = run_bass_kernel_spmd(nc, in_maps, core_ids=list(range(n)))
    return np.stack([np.asarray(r["y"]) for r in res.results], axis=0).astype(np.float32)
```
